# Optimizing a Trainium2 kernel written in Bass

```python
import math
import jax
import jax.numpy as jnp
from jax import lax
import numpy as np


D_MODEL = 4096
BATCH = 8
SEQ = 2048
DEPTH = 2

GRID_W = 64
CTX_LEN = 256
NORM_EPS = 1e-6
ROPE_THETA = 10000.0
Q_BLOCK = 128

N_EVEN = (DEPTH + 1) // 2
N_ODD = DEPTH // 2

MLA_HEADS = 16
MLA_Q_RANK = 1024
MLA_KV_RANK = 512
MLA_NOPE_DIM = 128
MLA_ROPE_DIM = 64
MLA_V_DIM = 128
MLA_QK_DIM = MLA_NOPE_DIM + MLA_ROPE_DIM
MLA_OUT = MLA_HEADS * MLA_V_DIM
SC_WIDTH = D_MODEL - MLA_OUT
EVEN_IN = MLA_Q_RANK + MLA_KV_RANK + MLA_ROPE_DIM + 3 * SC_WIDTH

DIFF_HEADS = 16
DIFF_HEAD_DIM = 64
DIFF_QK = DIFF_HEADS * 2 * DIFF_HEAD_DIM
DIFF_V = DIFF_HEADS * 2 * DIFF_HEAD_DIM
HY_WIDTH = D_MODEL - DIFF_V
HY_ORDER = 2
HY_BANDS = 16
HY_EMB = 1 + 2 * HY_BANDS
HY_FFN = 64
HY_SHIFT = 0.05
HY_MIN_DECAY = math.log(1e-2) / 1.5
HY_MAX_DECAY = math.log(1e-2) / 0.3
ODD_IN = 2 * DIFF_QK + DIFF_V + 3 * HY_WIDTH

N_EXPERTS = 32
TOP_K = 4
D_EXPERT = 640
SWIGLU_LIMIT = 7.0
SWIGLU_ALPHA = 1.702
MOE_BLOCK = 128

kernel_name = 'hybrid_mla_hyena_diffattn_moe_dit'


def rms_norm(x, gain=None):
    xf = x.astype(jnp.float32)
    y = xf * lax.rsqrt(jnp.mean(xf * xf, -1, keepdims=True) + NORM_EPS)
    if gain is not None:
        y = y * gain.astype(jnp.float32)
    return y.astype(x.dtype)


def adaln(cond, w, b):
    m = jax.nn.silu(cond) @ w + b
    return jnp.split(m[:, None, :], 6, -1)


def modulate(x, shift, scale):
    return rms_norm(x) * (1.0 + scale) + shift


def axial_rope(n_tok, rot_dim):
    rows = n_tok // GRID_W
    row = jnp.repeat(jnp.arange(rows), GRID_W).astype(jnp.float32)
    col = jnp.tile(jnp.arange(GRID_W), rows).astype(jnp.float32)
    quarter = rot_dim // 4
    inv = 1.0 / (ROPE_THETA ** (jnp.arange(quarter, dtype=jnp.float32) / quarter))
    ang = jnp.concatenate([row[:, None] * inv, col[:, None] * inv], -1)
    return jnp.cos(ang), jnp.sin(ang)


def apply_rope(x, cos, sin):
    shape = (x.shape[1],) + (1,) * (x.ndim - 3) + (cos.shape[-1],)
    cos = cos.reshape(shape).astype(x.dtype)
    sin = sin.reshape(shape).astype(x.dtype)
    x1, x2 = jnp.split(x, 2, axis=-1)
    return jnp.concatenate([x1 * cos - x2 * sin, x1 * sin + x2 * cos], -1)


def conv3(u, w):
    up = jnp.pad(u, ((0, 0), (1, 1), (0, 0)))
    return up[:, :-2] * w[0] + up[:, 1:-1] * w[1] + up[:, 2:] * w[2]


def sweep_query_blocks(block_fn, q):
    b, n = q.shape[:2]
    qb = jnp.moveaxis(q.reshape((b, n // Q_BLOCK, Q_BLOCK) + q.shape[2:]), 1, 0)
    out = jnp.moveaxis(lax.map(block_fn, qb), 0, 1)
    return out.reshape((b, n) + out.shape[3:])


def mla_q(cq, q_norm, w_uq, rope):
    b, n = cq.shape[:2]
    q = (rms_norm(cq, q_norm) @ w_uq).reshape(b, n, MLA_HEADS, MLA_QK_DIM)
    if rope is None:
        return q
    q_nope, q_pe = jnp.split(q, [MLA_NOPE_DIM], -1)
    return jnp.concatenate([q_nope, apply_rope(q_pe, *rope)], -1)


def mla_kv(ckv_pe, kv_norm, w_ukv, rope):
    b, n = ckv_pe.shape[:2]
    ckv, k_pe = jnp.split(ckv_pe, [MLA_KV_RANK], -1)
    kv = (rms_norm(ckv, kv_norm) @ w_ukv).reshape(b, n, MLA_HEADS, MLA_NOPE_DIM + MLA_V_DIM)
    k_nope, v = jnp.split(kv, [MLA_NOPE_DIM], -1)
    if rope is not None:
        k_pe = apply_rope(k_pe, *rope)
    k_pe = jnp.broadcast_to(k_pe[:, :, None, :], (b, n, MLA_HEADS, MLA_ROPE_DIM))
    return jnp.concatenate([k_nope, k_pe], -1), v


def mla_attend(q, k, v):
    scale = MLA_QK_DIM ** -0.5

    def blk(qb):
        s = jnp.einsum('bqhd,bkhd->bhqk', qb, k).astype(jnp.float32) * scale
        p = jax.nn.softmax(s, -1).astype(v.dtype)
        return jnp.einsum('bhqk,bkhd->bqhd', p, v)

    return sweep_query_blocks(blk, q)


def even_out(attn, sc_in, conv_w, out_w):
    b, n = attn.shape[:2]
    gb, gc, hh = jnp.split(sc_in, 3, -1)
    sc = gb * conv3(gc * hh, conv_w)
    return jnp.concatenate([attn.reshape(b, n, MLA_OUT), sc], -1) @ out_w


def even_mixer(u, uc, in_w, q_norm, kv_norm, w_uq, w_ukv, conv_w, out_w, rope, need_ctx):
    kv0, kv1 = MLA_Q_RANK, MLA_Q_RANK + MLA_KV_RANK + MLA_ROPE_DIM
    hl = u @ in_w
    hc = uc @ (in_w if need_ctx else in_w[:, kv0:kv1])
    hc_kv = hc[..., kv0:kv1] if need_ctx else hc
    k_ctx, v_ctx = mla_kv(hc_kv, kv_norm, w_ukv, None)
    k_lat, v_lat = mla_kv(hl[..., kv0:kv1], kv_norm, w_ukv, rope)
    q_lat = mla_q(hl[..., :kv0], q_norm, w_uq, rope)
    attn = mla_attend(q_lat, jnp.concatenate([k_lat, k_ctx], 1), jnp.concatenate([v_lat, v_ctx], 1))
    y = even_out(attn, hl[..., kv1:], conv_w, out_w)
    yc = None
    if need_ctx:
        q_ctx = mla_q(hc[..., :kv0], q_norm, w_uq, None)
        yc = even_out(mla_attend(q_ctx, k_ctx, v_ctx), hc[..., kv1:], conv_w, out_w)
    return y, yc


def diff_q(hq, rope):
    b, n = hq.shape[:2]
    q = hq.reshape(b, n, DIFF_HEADS, 2, DIFF_HEAD_DIM)
    return q if rope is None else apply_rope(q, *rope)


def diff_kv(hkv, rope):
    b, n = hkv.shape[:2]
    k, v = jnp.split(hkv, [DIFF_QK], -1)
    k = k.reshape(b, n, DIFF_HEADS, 2, DIFF_HEAD_DIM)
    if rope is not None:
        k = apply_rope(k, *rope)
    return k, v.reshape(b, n, DIFF_HEADS, 2 * DIFF_HEAD_DIM)


def diff_attend(q, k, v, lam):
    scale = DIFF_HEAD_DIM ** -0.5

    def blk(qb):
        s = jnp.einsum('bqhcd,bkhcd->bhcqk', qb, k).astype(jnp.float32) * scale
        p = jax.nn.softmax(s, -1)
        pd = (p[:, :, 0] - lam * p[:, :, 1]).astype(v.dtype)
        return jnp.einsum('bhqk,bkhe->bqhe', pd, v)

    return sweep_query_blocks(blk, q)


def hyena_filters(n, w1, b1, w2, b2, w3):
    f32 = jnp.float32
    t = jnp.linspace(0.0, 1.0, n, dtype=f32)[:, None]
    ang = (2.0 * math.pi / n) * jnp.arange(n, dtype=f32)[:, None] * jnp.linspace(1e-4, HY_BANDS - 1, HY_BANDS, dtype=f32)[None, :]
    z = jnp.concatenate([t, jnp.cos(ang), -jnp.sin(ang)], -1)
    hid = jnp.sin(z @ w1.astype(f32) + b1.astype(f32))
    hid = jnp.sin(hid @ w2.astype(f32) + b2.astype(f32))
    k = (hid @ w3.astype(f32)).reshape(n, HY_ORDER, 2, HY_WIDTH)
    decay = jnp.abs(jnp.linspace(HY_MIN_DECAY, HY_MAX_DECAY, HY_WIDTH, dtype=f32))
    k = k * (jnp.exp(-t[:, :, None, None] * decay) + HY_SHIFT)
    fwd, bwd = k[:, :, 0], k[:, :, 1]
    full = jnp.concatenate([fwd[:1] + bwd[:1], fwd[1:], jnp.zeros_like(fwd[:1]), bwd[:0:-1]], 0)
    full = full / jnp.sum(jnp.abs(full), 0, keepdims=True)
    return jnp.moveaxis(full, 1, 0)


def fft_conv(u, filt):
    n = u.shape[1]
    uf = jnp.fft.rfft(u.astype(jnp.float32), n=2 * n, axis=1)
    hf = jnp.fft.rfft(filt, axis=0)
    return jnp.fft.irfft(uf * hf[None], n=2 * n, axis=1)[:, :n]


def hyena(proj, conv_w, filt, skip):
    dt = proj.dtype
    v, x1, x2 = jnp.split(conv3(proj, conv_w), 3, -1)
    z = v
    for o, gate in enumerate((x1, x2)):
        z = gate * (fft_conv(z, filt[o]).astype(dt) + z * skip[o])
    return z


def odd_out(attn, hy_in, subln, lam_init, conv_w, filt, skip, out_w):
    b, n = attn.shape[:2]
    a = (rms_norm(attn, subln) * (1.0 - lam_init)).reshape(b, n, DIFF_V)
    return jnp.concatenate([a, hyena(hy_in, conv_w, filt, skip)], -1) @ out_w


def odd_mixer(u, uc, in_w, lam_p, subln, conv_w, filt_p, skip, out_w, rope, lam_init, need_ctx):
    kv0, kv1 = DIFF_QK, 2 * DIFF_QK + DIFF_V
    hl = u @ in_w
    hc = uc @ (in_w if need_ctx else in_w[:, kv0:kv1])
    hc_kv = hc[..., kv0:kv1] if need_ctx else hc
    lq1, lk1, lq2, lk2 = lam_p.astype(jnp.float32)
    lam = jnp.exp(jnp.sum(lq1 * lk1)) - jnp.exp(jnp.sum(lq2 * lk2)) + lam_init
    k_ctx, v_ctx = diff_kv(hc_kv, None)
    k_lat, v_lat = diff_kv(hl[..., kv0:kv1], rope)
    q_lat = diff_q(hl[..., :kv0], rope)
    attn = diff_attend(q_lat, jnp.concatenate([k_lat, k_ctx], 1), jnp.concatenate([v_lat, v_ctx], 1), lam)
    filt_lat = hyena_filters(u.shape[1], *filt_p)
    y = odd_out(attn, hl[..., kv1:], subln, lam_init, conv_w, filt_lat, skip, out_w)
    yc = None
    if need_ctx:
        attn_c = diff_attend(diff_q(hc[..., :kv0], None), k_ctx, v_ctx, lam)
        filt_ctx = hyena_filters(uc.shape[1], *filt_p)
        yc = odd_out(attn_c, hc[..., kv1:], subln, lam_init, conv_w, filt_ctx, skip, out_w)
    return y, yc


def moe(h, router_w, router_b, w_gu, b_gu, w_down, b_down):
    t_tok, d = h.shape
    tk = t_tok * TOP_K
    logits = (h @ router_w).astype(jnp.float32) + router_b.astype(jnp.float32)
    top_v, top_i = lax.top_k(logits, TOP_K)
    gates = jax.nn.softmax(top_v, -1)
    flat_e = top_i.reshape(-1)
    order = jnp.argsort(flat_e)
    sorted_e = flat_e[order]
    sorted_tok = (order // TOP_K).astype(jnp.int32)
    sorted_gate = gates.reshape(-1)[order]
    counts = jnp.bincount(flat_e, length=N_EXPERTS)
    padded = (counts + MOE_BLOCK - 1) // MOE_BLOCK * MOE_BLOCK
    pad_end = jnp.cumsum(padded)
    pad_start = pad_end - padded
    start = jnp.cumsum(counts) - counts
    dest = pad_start[sorted_e] + jnp.arange(tk) - start[sorted_e]
    n_blocks = -(-tk // MOE_BLOCK) + N_EXPERTS
    n_slots = n_blocks * MOE_BLOCK
    slot_tok = jnp.full((n_slots,), t_tok, jnp.int32).at[dest].set(sorted_tok)
    slot_gate = jnp.zeros((n_slots,), jnp.float32).at[dest].set(sorted_gate)
    block_e = jnp.minimum(jnp.searchsorted(pad_end, jnp.arange(n_blocks) * MOE_BLOCK, side='right'), N_EXPERTS - 1)
    h_pad = jnp.concatenate([h, jnp.zeros((1, d), h.dtype)], 0)

    def run(args):
        tok, e = args
        gu = h_pad[tok] @ w_gu[e] + b_gu[e]
        g, up = jnp.split(gu, 2, -1)
        g = jnp.minimum(g, SWIGLU_LIMIT)
        up = jnp.clip(up, -SWIGLU_LIMIT, SWIGLU_LIMIT)
        a = (up + 1.0) * g * jax.nn.sigmoid(SWIGLU_ALPHA * g)
        return a @ w_down[e] + b_down[e]

    yb = lax.map(run, (slot_tok.reshape(n_blocks, MOE_BLOCK), block_e)).reshape(n_slots, d)
    y = jax.ops.segment_sum(yb * slot_gate[:, None].astype(yb.dtype), slot_tok, num_segments=t_tok + 1)
    return y[:t_tok]


def setup_inputs(seed: int = 0) -> dict:
    key = jax.random.key(seed)
    ks = iter(jax.random.split(key, 32))
    D = D_MODEL

    def nrm(shape, scale):
        return jax.random.normal(next(ks), shape, jnp.float32) * scale

    def gain(shape):
        return 1.0 + nrm(shape, 0.02)

    return {
        'x': nrm((BATCH, SEQ, D), 1.0),
        'c': nrm((BATCH, D), 1.0),
        'ctx': nrm((BATCH, CTX_LEN, D), 1.0),
        'c_ctx': nrm((D,), 1.0),
        'ada_w': nrm((DEPTH, D, 6 * D), 0.5 * D ** -0.5),
        'ada_b': nrm((DEPTH, 6 * D), 0.02),
        'mla_in_w': nrm((N_EVEN, D, EVEN_IN), D ** -0.5),
        'mla_q_norm': gain((N_EVEN, MLA_Q_RANK)),
        'mla_kv_norm': gain((N_EVEN, MLA_KV_RANK)),
        'mla_w_uq': nrm((N_EVEN, MLA_Q_RANK, MLA_HEADS * MLA_QK_DIM), MLA_Q_RANK ** -0.5),
        'mla_w_ukv': nrm((N_EVEN, MLA_KV_RANK, MLA_HEADS * (MLA_NOPE_DIM + MLA_V_DIM)), MLA_KV_RANK ** -0.5),
        'sc_conv_w': nrm((N_EVEN, 3, SC_WIDTH), 3 ** -0.5),
        'even_out_w': nrm((N_EVEN, D, D), D ** -0.5),
        'odd_in_w': nrm((N_ODD, D, ODD_IN), D ** -0.5),
        'diff_lambda': nrm((N_ODD, 4, DIFF_HEAD_DIM), 0.1),
        'diff_subln': gain((N_ODD, 2 * DIFF_HEAD_DIM)),
        'hy_conv_w': nrm((N_ODD, 3, 3 * HY_WIDTH), 3 ** -0.5),
        'hy_w1': nrm((N_ODD, HY_EMB, HY_FFN), 1.0),
        'hy_b1': nrm((N_ODD, HY_FFN), 0.02),
        'hy_w2': nrm((N_ODD, HY_FFN, HY_FFN), HY_FFN ** -0.5),
        'hy_b2': nrm((N_ODD, HY_FFN), 0.02),
        'hy_w3': nrm((N_ODD, HY_FFN, HY_ORDER * 2 * HY_WIDTH), HY_FFN ** -0.5),
        'hy_skip': nrm((N_ODD, HY_ORDER, HY_WIDTH), 1.0),
        'odd_out_w': nrm((N_ODD, D, D), D ** -0.5),
        'router_w': nrm((DEPTH, D, N_EXPERTS), D ** -0.5),
        'router_b': nrm((DEPTH, N_EXPERTS), 0.01),
        'moe_w_gu': nrm((DEPTH, N_EXPERTS, D, 2 * D_EXPERT), D ** -0.5),
        'moe_b_gu': nrm((DEPTH, N_EXPERTS, 2 * D_EXPERT), 0.02),
        'moe_w_down': nrm((DEPTH, N_EXPERTS, D_EXPERT, D), D_EXPERT ** -0.5),
        'moe_b_down': nrm((DEPTH, N_EXPERTS, D), 0.02),
        'final_norm': gain((D,)),
    }


def reference(x, c, ctx, c_ctx, ada_w, ada_b, mla_in_w, mla_q_norm, mla_kv_norm, mla_w_uq, mla_w_ukv,
              sc_conv_w, even_out_w, odd_in_w, diff_lambda, diff_subln, hy_conv_w, hy_w1, hy_b1, hy_w2,
              hy_b2, hy_w3, hy_skip, odd_out_w, router_w, router_b, moe_w_gu, moe_b_gu, moe_w_down,
              moe_b_down, final_norm):
    b, n, d = x.shape
    m = ctx.shape[1]
    rope_mla = axial_rope(n, MLA_ROPE_DIM)
    rope_diff = axial_rope(n, DIFF_HEAD_DIM)
    h, hc = x, ctx
    for l in range(DEPTH):
        need_ctx = l < DEPTH - 1
        i = l // 2
        sh1, sc1, g1, sh2, sc2, g2 = adaln(c, ada_w[l], ada_b[l])
        csh1, csc1, cg1, csh2, csc2, cg2 = adaln(c_ctx[None], ada_w[l], ada_b[l])
        u = modulate(h, sh1, sc1)
        uc = modulate(hc, csh1, csc1)
        if l % 2 == 0:
            y, yc = even_mixer(u, uc, mla_in_w[i], mla_q_norm[i], mla_kv_norm[i], mla_w_uq[i], mla_w_ukv[i],
                               sc_conv_w[i], even_out_w[i], rope_mla, need_ctx)
        else:
            filt_p = (hy_w1[i], hy_b1[i], hy_w2[i], hy_b2[i], hy_w3[i])
            lam_init = 0.8 - 0.6 * math.exp(-0.3 * l)
            y, yc = odd_mixer(u, uc, odd_in_w[i], diff_lambda[i], diff_subln[i], hy_conv_w[i], filt_p,
                              hy_skip[i], odd_out_w[i], rope_diff, lam_init, need_ctx)
        h = h + g1 * y
        moe_p = (router_w[l], router_b[l], moe_w_gu[l], moe_b_gu[l], moe_w_down[l], moe_b_down[l])
        u2 = modulate(h, sh2, sc2).reshape(b * n, d)
        if need_ctx:
            hc = hc + cg1 * yc
            u2c = modulate(hc, csh2, csc2).reshape(b * m, d)
            f = moe(jnp.concatenate([u2, u2c], 0), *moe_p)
            h = h + g2 * f[:b * n].reshape(b, n, d)
            hc = hc + cg2 * f[b * n:].reshape(b, m, d)
        else:
            h = h + g2 * moe(u2, *moe_p).reshape(b, n, d)
    return rms_norm(h, final_norm)
```

```python
from contextlib import ExitStack
import math
import numpy as np
import concourse.bass as bass
import concourse.mybir as mybir
from concourse.bass_utils import run_bass_kernel_spmd

F32 = mybir.dt.float32
F32R = mybir.dt.float32r
I32 = mybir.dt.int32
ALU = mybir.AluOpType
AF = mybir.ActivationFunctionType
AX = mybir.AxisListType

RING = 8
D = 4096
KC = 32
NLAT = 2048
NCTX = 256
T = NLAT + NCTX
EPS = 1e-6
NE = 32
DE = 640
NB0 = 2 * ((T * 4) // 256 + NE)
NB1 = 2 * ((NLAT * 4) // 256 + NE)


class KB:
    def __init__(self, nc):
        self.nc = nc
        self.E = {'pe': nc.tensor, 'act': nc.scalar, 'dve': nc.vector, 'pool': nc.gpsimd, 'sp': nc.sync}
        self.sems = {}
        self.cnt = {}
        for e in self.E:
            self.sems[e] = nc.alloc_semaphore(name=f"sem_{e}")
            self.cnt[e] = 0
        self.rings = {}
        self.dcnt = {}
        for q in ('sp', 'act', 'pool'):
            self.rings[q] = [nc.alloc_semaphore(name=f"dq_{q}_{i}") for i in range(RING)]
            self.dcnt[q] = 0
        self.semobj = {}
        for e in self.E:
            self.semobj[('c', e)] = self.sems[e]
        for q in self.rings:
            for i, s in enumerate(self.rings[q]):
                self.semobj[('d', q, i)] = s
        self.waited = {e: {} for e in self.E}
        self.state = {}
        self.children = {}
        self.latest = {}
        self.ninst = 0

    def _st(self, key):
        s = self.state.get(key)
        if s is None:
            s = {'w': {}, 'r': {}}
            self.state[key] = s
            if '/' in key:
                self.children.setdefault(key.split('/')[0], set()).add(key)
        return s

    def _conf(self, key):
        if '/' in key:
            return [key, key.split('/')[0]]
        return [key] + list(self.children.get(key, ()))

    def _deps(self, reads, writes):
        deps = {}

        def add(d):
            for sid, v in d.items():
                if deps.get(sid, 0) < v:
                    deps[sid] = v
        for key in reads:
            for ck in self._conf(key):
                if ck in self.state:
                    add(self.state[ck]['w'])
        for key in writes:
            for ck in self._conf(key):
                if ck in self.state:
                    add(self.state[ck]['w'])
                    add(self.state[ck]['r'])
        return deps

    def _commit(self, ev, reads, writes):
        sid, v = ev
        for key in reads:
            s = self._st(key)
            s['r'][sid] = max(s['r'].get(sid, 0), v)
        for key in writes:
            s = self._st(key)
            s['w'] = {sid: v}
            s['r'] = {}
            if '/' not in key:
                for ck in list(self.children.get(key, ())):
                    self.state[ck] = {'w': {}, 'r': {}}

    def _wait(self, eng, deps):
        w = self.waited[eng]
        for sid, v in deps.items():
            if eng == 'pe' and sid == ('c', 'pe'):
                continue
            if w.get(sid, 0) >= v:
                continue
            self.E[eng].wait_ge(self.semobj[sid], v)
            w[sid] = v

    def op(self, eng, fn, reads=(), writes=()):
        deps = self._deps(reads, writes)
        self._wait(eng, deps)
        inst = fn(self.E[eng])
        self.cnt[eng] += 1
        inst.then_inc(self.sems[eng], 1)
        ev = (('c', eng), self.cnt[eng])
        self.latest[ev[0]] = ev[1]
        self._commit(ev, reads, writes)
        self.ninst += 1
        return inst

    def dma(self, q, fn, reads=(), writes=()):
        deps = self._deps(reads, writes)
        i = self.dcnt[q]
        slot = i % RING
        sid = ('d', q, slot)
        if i >= RING:
            deps[sid] = max(deps.get(sid, 0), 16 * (i // RING))
        self._wait(q, deps)
        inst = fn(self.E[q])
        inst.then_inc(self.rings[q][slot], 16)
        self.dcnt[q] += 1
        ev = (sid, 16 * (i // RING + 1))
        self.latest[sid] = ev[1]
        self._commit(ev, reads, writes)
        self.ninst += 1
        return inst

    def barrier(self):
        for e in self.E:
            self._wait(e, dict(self.latest))
        self.state = {}
        self.children = {}


def _dma(out, in_):
    return lambda e: e.dma_start(out=out, in_=in_)


def f32(ap):
    return ap.bitcast(F32)


TCH2 = [(0, 1024, False), (1024, 1024, False), (2048, 256, True)]
TCH = [(0, 512, False), (512, 512, False), (1024, 512, False), (1536, 512, False), (2048, 256, True)]


class Prog:
    def __init__(self, dbg=False):
        self.nc = bass.Bass("TRN2", target_bir_lowering=False)
        self.k = KB(self.nc)
        self.dbg = dbg
        self.I = {}
        self.S = {}
        self.O = {}
        nc = self.nc
        self.ps = [nc.alloc_psum_tensor(f"psb{i}", [128, 512], F32).ap() for i in range(8)]
        self.gl = ExitStack()
        self.ident = self.gsb('ident', [128, 128])
        self.onesR = self.gsb('onesR', [128, 128], F32R)
        self.ones = self.gsb('ones', [128, 128])
        self.triu = self.gsb('triu', [128, 128])
        self.identR = self.gsb('identR', [128, 128], F32R)
        self.mod = [self.gsb(f'mod{l}', [128, 2, 192]) for l in range(2)]
        self.opsc = [self.gsb(f'opsc{l}', [128, 2, 2, 32]) for l in range(2)]
        self.did_const = False

    def sbf(self, es):
        self._stage = getattr(self, '_stage', 0) + 1
        st = self._stage
        return lambda n, s, dt=F32: es.enter_context(self.nc.sbuf_tensor(f's{st}_{n}', s, dt)).ap()

    def gsb(self, name, shape, dt=F32):
        return self.gl.enter_context(self.nc.sbuf_tensor(name, shape, dt)).ap()

    def inp(self, name, shape, dt=F32):
        ap = self.nc.dram_tensor(name, list(shape), dt, kind="ExternalInput").ap()
        self.I[name] = ap
        return ap

    def scratch(self, name, shape, dt=F32, out=False):
        kind = "ExternalOutput" if (self.dbg or out) else "Internal"
        ap = self.nc.dram_tensor(name, list(shape), dt, kind=kind).ap()
        self.S[name] = ap
        return ap

    def consts(self):
        k = self.k
        c = self.inp('consts', [128, 3, 128])
        k.dma('sp', _dma(self.ident, c[:, 0, :]), writes=['ident'])
        k.dma('sp', _dma(self.ones, c[:, 1, :]), writes=['ones'])
        k.dma('sp', _dma(self.triu, c[:, 2, :]), writes=['triu'])
        k.dma('pool', _dma(self.onesR, c[:, 1, :]), writes=['onesR'])
        k.dma('pool', _dma(self.identR, c[:, 0, :]), writes=['identR'])

    def st_adaln(self, l):
        nc, k = self.nc, self.k
        cT = self.I.get('cT') if 'cT' in self.I else self.inp('cT', [128, 32, 2])
        adaW = self.inp(f'adaW{l}', [192, 128, 32, 128])
        adab = self.inp(f'adab{l}', [128, 192])
        mod = self.mod[l]
        ps = self.ps[0]
        with ExitStack() as es:
            sb = self.sbf(es)
            c_sb = sb('a_c', [128, 32, 2])
            sc = sb('a_sc', [128, 32, 2], F32R)
            bT = sb('a_b', [128, 192])
            wb = [sb(f'a_w{i}', [128, 32, 128], F32R) for i in range(3)]
            k.dma('sp', _dma(c_sb, cT), writes=['a_c'])
            k.dma('sp', _dma(bT, adab), writes=['a_b'])
            k.op('act', lambda e: e.activation(out=sc, in_=c_sb, func=AF.Silu), reads=['a_c'], writes=['a_sc'])
            for mt in range(192):
                w = wb[mt % 3]
                wk = f'a_w{mt % 3}'
                k.dma('pool', _dma(w, adaW[mt]), writes=[wk])
                for c in range(32):
                    k.op('pe', lambda e: e.matmul(ps[:, 2 * mt:2 * mt + 2], lhsT=w[:, c, :], rhs=sc[:, c, :],
                                                  start=(c == 0), stop=(c == 31)),
                         reads=[wk, 'a_sc'], writes=['a_ps'])
            pv = ps[:, 0:384].rearrange("p (m j) -> p j m", j=2)
            for j in range(2):
                k.op('dve', lambda e: e.tensor_tensor(out=mod[:, j, :], in0=pv[:, j, :], in1=bT, op=ALU.add),
                     reads=['a_ps', 'a_b'], writes=[f'mod{l}'])
            for j in range(2):
                for wh in range(2):
                    s0 = 32 + 96 * wh
                    k.op('dve', lambda e: e.tensor_scalar(out=self.opsc[l][:, j, wh, :], in0=mod[:, j, s0:s0 + 32],
                                                          scalar1=1.0, scalar2=None, op0=ALU.add),
                         reads=[f'mod{l}'], writes=[f'opsc{l}'])
            k.barrier()

    def rstd_from_ps(self, psS, rstd, tn, n, pk, rk):
        k = self.k
        k.op('dve', lambda e: e.tensor_scalar(out=rstd[:, :tn], in0=psS[:, :tn], scalar1=1.0 / n, scalar2=EPS,
                                              op0=ALU.mult, op1=ALU.add), reads=[pk], writes=[rk])
        k.op('act', lambda e: e.activation(out=rstd[:, :tn], in_=rstd[:, :tn], func=AF.Sqrt), reads=[rk], writes=[rk])
        k.op('dve', lambda e: e.reciprocal(out=rstd[:, :tn], in_=rstd[:, :tn]), reads=[rk], writes=[rk])

    def load_modulate(self, sb_, src, t0, tn, scale_ap, shift_ap, tag, xoff=0):
        k = self.k
        xs_full = sb_['xs']
        xs = xs_full[:, :, xoff:xoff + 512]
        hs = sb_['hs']
        srcv = src[:, t0:t0 + tn].rearrange("(c p) t -> c p t", p=128)
        psS = self.ps[7]
        n = [0]

        def ld(c):
            i = n[0] = n[0] + 1
            h = hs[i % 3]
            hk = f'hs{i % 3}'
            k.dma('sp', _dma(h[:, :tn], srcv[c]), writes=[hk])
            return h, hk
        for c in range(KC):
            h, hk = ld(c)
            sq = sb_['sq'][c % 2]
            sqk = f'sq{c % 2}'
            k.op('act', lambda e: e.activation(out=sq[:, :tn], in_=h[:, :tn], func=AF.Square),
                 reads=[hk], writes=[sqk])
            k.op('pe', lambda e: e.matmul(psS[:, :tn], lhsT=self.onesR, rhs=sq[:, :tn], start=(c == 0), stop=(c == KC - 1)),
                 reads=[sqk, 'onesR'], writes=['ps7'])
        rstd = sb_['rstd']
        self.rstd_from_ps(psS, rstd, tn, D, 'ps7', 'rstd')
        for c in range(KC):
            h, hk = ld(c)
            if shift_ap is not None:
                tmp = sb_['tmp'][c % 2]
                tk = f'tmp{c % 2}'
                k.op('dve', lambda e: e.scalar_tensor_tensor(out=tmp[:, :tn], in0=h[:, :tn], scalar=scale_ap[:, c:c + 1],
                                                             in1=rstd[:, :tn], op0=ALU.mult, op1=ALU.mult),
                     reads=[hk, 'rstd'], writes=[tk])
                k.op('act', lambda e: e.activation(out=xs[:, c, :tn], in_=tmp[:, :tn], func=AF.Identity,
                                                   bias=shift_ap[:, c:c + 1], scale=1.0),
                     reads=[tk], writes=[f'xs/{c}'])
            else:
                k.op('dve', lambda e: e.scalar_tensor_tensor(out=xs[:, c, :tn], in0=h[:, :tn], scalar=scale_ap[:, c:c + 1],
                                                             in1=rstd[:, :tn], op0=ALU.mult, op1=ALU.mult),
                     reads=[hk, 'rstd'], writes=[f'xs/{c}'])
        return xs_full

    def mod_bufs(self, sb, width=512):
        return {'xs': sb('xs', [128, 32, width], F32R), 'hs': [sb(f'hs{i}', [128, 512]) for i in range(3)],
                'sq': [sb(f'sq{i}', [128, 512], F32R) for i in range(2)],
                'rstd': sb('rstd', [128, 512]), 'tmp': [sb(f'tmp{i}', [128, 512]) for i in range(2)]}

    def linear(self, es, xs, xkey, kc, wt, nmt, tn, epilogue, tag, mw_of=None):
        nc, k = self.nc, self.k
        if not hasattr(self, '_lw'):
            self._lw = {}
        key = (tag, kc)
        if key not in self._lw:
            self._lw[key] = [es.enter_context(nc.sbuf_tensor(f's{self._stage}_{tag}_w{i}', [128, kc, 128], F32R)).ap() for i in range(3)]
        wb = self._lw[key]
        for mt in range(nmt):
            mw = 128 if mw_of is None else mw_of(mt)
            i = self._lwc = getattr(self, '_lwc', 0) + 1
            w = wb[i % 3]
            wk = f'{tag}_w{i % 3}'
            k.dma('pool', _dma(w[:, :, :mw], wt[mt][:, :, :mw]), writes=[wk])
            pi = i % 2
            ps = self.ps[pi]
            for c in range(kc):
                k.op('pe', lambda e: e.matmul(ps[:mw, :tn], lhsT=w[:, c, :mw], rhs=xs[:, c, :tn], start=(c == 0), stop=(c == kc - 1)),
                     reads=[wk, xkey], writes=[f'ps{pi}'])
            epilogue(mt, ps, f'ps{pi}', mw)

    def linear2(self, es, xs, xkey, kc, wt, nmt, subs, epilogue, tag):
        nc, k = self.nc, self.k
        key = (tag, kc)
        if key not in self._lw:
            self._lw[key] = [es.enter_context(nc.sbuf_tensor(f's{self._stage}_{tag}_w{i}', [128, kc, 128], F32R)).ap() for i in range(3)]
        wb = self._lw[key]
        for mt in range(nmt):
            i = self._lwc = getattr(self, '_lwc', 0) + 1
            w = wb[i % 3]
            wk = f'{tag}_w{i % 3}'
            k.dma('pool', _dma(w, wt[mt]), writes=[wk])
            for (off, t0, tn) in subs:
                pj = self._lpc = getattr(self, '_lpc', 0) + 1
                pi = pj % 2
                ps = self.ps[pi]
                for c in range(kc):
                    k.op('pe', lambda e: e.matmul(ps[:, :tn], lhsT=w[:, c, :], rhs=xs[:, c, off:off + tn], start=(c == 0), stop=(c == kc - 1)),
                         reads=[wk, xkey], writes=[f'ps{pi}'])
                epilogue(mt, ps, f'ps{pi}', t0, tn)

    def st_inproj(self, l, src, nmt, chunks_mt):
        nc, k = self.nc, self.k
        inW = self.inp(f'inW{l}', [nmt, 128, 32, 128])
        hlT = self.scratch(f'hlT{l}', [nmt * 128, T])
        mod = self.mod[l]
        with ExitStack() as es:
            sb = self.sbf(es)
            sb_ = self.mod_bufs(sb, 1024)
            ost = [sb(f'ost{i}', [128, 512]) for i in range(4)]
            cnt = [0]
            self._lw = {}
            for (T0, TN, isc) in TCH2:
                j = 1 if isc else 0
                subs = [(off, T0 + off, min(512, TN - off)) for off in range(0, TN, 512)]
                for (off, t0, tn) in subs:
                    xs = self.load_modulate(sb_, src, t0, tn, self.opsc[l][:, j, 0, :], mod[:, j, 0:32], 'ip', xoff=off)
                mts = chunks_mt(isc)

                def epi(mi, ps, pk, t0, tn, mts=mts):
                    mt = mts[mi]
                    i = cnt[0] = cnt[0] + 1
                    o = ost[i % 4]
                    ok = f'ost{i % 4}'
                    if i % 2 == 0:
                        k.op('act', lambda e: e.activation(out=o[:, :tn], in_=ps[:, :tn], func=AF.Copy), reads=[pk], writes=[ok])
                    else:
                        k.op('dve', lambda e: e.tensor_copy(out=o[:, :tn], in_=ps[:, :tn]), reads=[pk], writes=[ok])
                    k.dma('sp', _dma(hlT[mt * 128:(mt + 1) * 128, t0:t0 + tn], o[:, :tn]), reads=[ok], writes=[f'hlT/{mt}_{t0}'])
                wts = [inW[mt] for mt in mts]
                self.linear2(es, xs, 'xs', 32, wts, len(mts), subs, epi, 'ip')
            k.barrier()
        return hlT

    def norm_linear(self, es, sb, src_rows, kc, nfeat, gain, t0, tn, wt, nmt, epi, tag):
        nc, k = self.nc, self.k
        bufs = self._nl.get(tag)
        if bufs is None:
            bufs = self._nl[tag] = {'x': sb(f'{tag}_x', [128, kc, 512]), 'xr': sb(f'{tag}_xr', [128, kc, 512], F32R),
                                    'sq': [sb(f'{tag}_sq{i}', [128, 512], F32R) for i in range(2)], 'rstd': sb(f'{tag}_rstd', [128, 512])}
        x, xr, rstd = bufs['x'], bufs['xr'], bufs['rstd']
        k.dma('sp', _dma(x[:, :, :tn], src_rows[:, t0:t0 + tn].rearrange("(c p) t -> p c t", p=128)), writes=[f'{tag}_x'])
        psS = self.ps[7]
        for c in range(kc):
            sq = bufs['sq'][c % 2]
            sqk = f'{tag}_sq{c % 2}'
            k.op('act', lambda e: e.activation(out=sq[:, :tn], in_=x[:, c, :tn], func=AF.Square), reads=[f'{tag}_x'], writes=[sqk])
            k.op('pe', lambda e: e.matmul(psS[:, :tn], lhsT=self.onesR, rhs=sq[:, :tn], start=(c == 0), stop=(c == kc - 1)),
                 reads=[sqk, 'onesR'], writes=['ps7'])
        self.rstd_from_ps(psS, rstd, tn, nfeat, 'ps7', f'{tag}_rstd')
        for c in range(kc):
            k.op('dve', lambda e: e.scalar_tensor_tensor(out=xr[:, c, :tn], in0=x[:, c, :tn], scalar=gain[:, c:c + 1],
                                                         in1=rstd[:, :tn], op0=ALU.mult, op1=ALU.mult),
                 reads=[f'{tag}_x', f'{tag}_rstd'], writes=[f'{tag}_xr/{c}'])
        self.linear(es, xr, f'{tag}_xr', kc, wt, nmt, tn, epi, tag)

    def st_mla_prep(self, hlT):
        nc, k = self.nc, self.k
        uqW = self.inp('uqW', [32, 128, 8, 128])
        ukvW = self.inp('ukvW', [32, 128, 4, 128])
        qg = self.inp('qgain', [128, 8])
        kvg = self.inp('kvgain', [128, 4])
        ropeT = self.inp('rope_mla', [128, NLAT])
        qnT = self.scratch('qnT', [16, 128, T])
        qpT = self.scratch('qpT', [16, 64, T])
        knT = self.scratch('knT', [16, 128, T])
        vT = self.scratch('vT', [16, 128, T])
        kpT = self.scratch('kpT', [64, T])
        qscale = 192.0 ** -0.5
        with ExitStack() as es:
            sb = self.sbf(es)
            self._nl = {}
            self._lw = {}
            qg_sb = sb('qg', [128, 8]); kvg_sb = sb('kvg', [128, 4]); rope = sb('rope', [128, NLAT])
            k.dma('sp', _dma(qg_sb, qg), writes=['qg'])
            k.dma('sp', _dma(kvg_sb, kvg), writes=['kvg'])
            k.dma('sp', _dma(rope, ropeT), writes=['rope'])
            ost = [sb(f'ost{i}', [128, 512]) for i in range(4)]
            tt = [sb(f'tt{i}', [128, 512]) for i in range(2)]
            kpe = sb('kpe', [128, 512])
            cnt = [0]
            for (t0, tn, isc) in TCH:
                def nxt():
                    i = cnt[0] = cnt[0] + 1
                    return ost[i % 4], f'ost{i % 4}', i

                def rope_out(src_ap, srck, scale, t0=t0, tn=tn, isc=isc):
                    o, ok, i = nxt()
                    if isc:
                        k.op('act', lambda e: e.activation(out=o[0:64, :tn], in_=src_ap[0:64, :tn], func=AF.Copy, scale=scale),
                             reads=[srck], writes=[ok])
                    else:
                        t = tt[i % 2]
                        tk = f'tt{i % 2}'
                        k.op('dve', lambda e: e.scalar_tensor_tensor(out=t[:, :tn], in0=src_ap[:, :tn], scalar=scale, in1=rope[:, t0:t0 + tn],
                                                                     op0=ALU.mult, op1=ALU.mult), reads=[srck, 'rope'], writes=[tk])
                        k.op('act', lambda e: e.activation(out=o[64:128, :tn], in_=t[0:64, :tn], func=AF.Copy), reads=[tk], writes=[ok])
                        k.op('dve', lambda e: e.tensor_tensor(out=o[0:64, :tn], in0=o[64:128, :tn], in1=t[64:128, :tn], op=ALU.add),
                             reads=[tk, ok], writes=[ok])
                    return o, ok

                def epi_q(mt, ps, pk, mw, t0=t0, tn=tn):
                    h = mt // 2
                    if mt % 2 == 0:
                        o, ok, i = nxt()
                        k.op('act', lambda e: e.activation(out=o[:, :tn], in_=ps[:, :tn], func=AF.Copy, scale=qscale), reads=[pk], writes=[ok])
                        k.dma('sp', _dma(qnT[h, :, t0:t0 + tn], o[:, :tn]), reads=[ok], writes=[f'qnT/{h}_{t0}'])
                    else:
                        o, ok = rope_out(ps, pk, qscale)
                        k.dma('sp', _dma(qpT[h, :, t0:t0 + tn], o[0:64, :tn]), reads=[ok], writes=[f'qpT/{h}_{t0}'])
                self.norm_linear(es, sb, hlT[0:1024], 8, 1024, qg_sb, t0, tn, [uqW[i] for i in range(32)], 32, epi_q, 'uq')

                def epi_kv(mt, ps, pk, mw, t0=t0, tn=tn):
                    h = mt // 2
                    o, ok, i = nxt()
                    if i % 2 == 0:
                        k.op('act', lambda e: e.activation(out=o[:, :tn], in_=ps[:, :tn], func=AF.Copy), reads=[pk], writes=[ok])
                    else:
                        k.op('dve', lambda e: e.tensor_copy(out=o[:, :tn], in_=ps[:, :tn]), reads=[pk], writes=[ok])
                    dst = knT if mt % 2 == 0 else vT
                    k.dma('sp', _dma(dst[h, :, t0:t0 + tn], o[:, :tn]), reads=[ok], writes=[f'kv{mt % 2}/{h}_{t0}'])
                self.norm_linear(es, sb, hlT[1024:1536], 4, 512, kvg_sb, t0, tn, [ukvW[i] for i in range(32)], 32, epi_kv, 'ukv')
                k.dma('sp', _dma(kpe[:, :tn], hlT[1536:1664, t0:t0 + tn]), writes=['kpe'])
                o, ok = rope_out(kpe, 'kpe', 1.0)
                k.dma('sp', _dma(kpT[:, t0:t0 + tn], o[0:64, :tn]), reads=[ok], writes=[f'kpT/{t0}'])
            k.barrier()
        return qnT, qpT, knT, vT, kpT

    def st_mla_attn(self, qnT, qpT, knT, vT, kpT, catT, with_ctx_q=True):
        nc, k = self.nc, self.k
        with ExitStack() as es:
            sb = self.sbf(es)
            kp = sb('at_kp', [64, T], F32R)
            k.dma('pool', _dma(kp, kpT), writes=['at_kp'])
            kn = [sb(f'at_kn{i}', [128, T], F32R) for i in range(2)]
            qn = [sb(f'at_qn{i}', [128, T], F32R) for i in range(2)]
            qp = [sb(f'at_qp{i}', [64, T], F32R) for i in range(2)]
            vt = [sb(f'at_vt{i}', [128, T]) for i in range(2)]
            vm = [sb(f'at_vm{i}', [128, 18, 128], F32R) for i in range(2)]
            pT = [sb(f'at_p{i}', [128, 512], F32R) for i in range(3)]
            rden = [sb(f'at_rd{i}', [128, 512]) for i in range(2)]
            ob = [sb(f'at_o{i}', [128, 512]) for i in range(2)]
            pc = 0
            qcn = 0
            for h in range(16):
                b = h % 2
                k.dma('pool', _dma(kn[b], knT[h]), writes=[f'at_kn{b}'])
                k.dma('pool', _dma(qn[b], qnT[h]), writes=[f'at_qn{b}'])
                k.dma('pool', _dma(qp[b], qpT[h]), writes=[f'at_qp{b}'])
                k.dma('sp', _dma(vt[b], vT[h]), writes=[f'at_vt{b}'])
                for g in range(5):
                    n = min(4, 18 - 4 * g)
                    pst = self.ps[6]
                    for j in range(n):
                        kt = 4 * g + j
                        k.op('pe', lambda e: e.transpose(out=pst[:, j * 128:(j + 1) * 128], in_=vt[b][:, kt * 128:(kt + 1) * 128], identity=self.ident),
                             reads=[f'at_vt{b}', 'ident'], writes=['ps6'])
                    k.op('act', lambda e: e.activation(out=vm[b][:, 4 * g:4 * g + n, :], in_=pst[:, :n * 128].rearrange("p (j d) -> p j d", d=128), func=AF.Copy),
                         reads=['ps6'], writes=[f'at_vm{b}'])
                qcs = [(t0, tn, list(range(18))) for (t0, tn, isc) in TCH if not isc]
                if with_ctx_q:
                    qcs.append((NLAT, NCTX, [16, 17]))
                for (t0, tn, kts) in qcs:
                    qcn += 1
                    pn = self.ps[2 + qcn % 2]
                    pd = self.ps[4 + qcn % 2]
                    pnk, pdk = f'ps{2 + qcn % 2}', f'ps{4 + qcn % 2}'
                    def emit_s(kt, t0=t0, tn=tn, b=b):
                        nonlocal pc
                        pc += 1
                        pss = self.ps[pc % 2]
                        psk = f'ps{pc % 2}'
                        ks = slice(kt * 128, (kt + 1) * 128)
                        k.op('pe', lambda e: e.matmul(pss[:, :tn], lhsT=kn[b][:, ks], rhs=qn[b][:, t0:t0 + tn], start=True, stop=False),
                             reads=[f'at_kn{b}', f'at_qn{b}'], writes=[psk])
                        k.op('pe', lambda e: e.matmul(pss[:, :tn], lhsT=kp[:, ks], rhs=qp[b][:, t0:t0 + tn], start=False, stop=True),
                             reads=['at_kp', f'at_qp{b}'], writes=[psk])
                        return pss, psk, pc
                    cur = emit_s(kts[0])
                    for ii, kt in enumerate(kts):
                        nxt_ = emit_s(kts[ii + 1]) if ii + 1 < len(kts) else None
                        pss, psk, pci = cur
                        p = pT[pci % 3]
                        ppk = f'at_p{pci % 3}'
                        k.op('act', lambda e: e.activation(out=p[:, :tn], in_=pss[:, :tn], func=AF.Exp), reads=[psk], writes=[ppk])
                        k.op('pe', lambda e: e.matmul(pn[:, :tn], lhsT=vm[b][:, kt, :], rhs=p[:, :tn], start=(ii == 0), stop=(ii == len(kts) - 1)),
                             reads=[f'at_vm{b}', ppk], writes=[pnk])
                        k.op('pe', lambda e: e.matmul(pd[:, :tn], lhsT=self.onesR, rhs=p[:, :tn], start=(ii == 0), stop=(ii == len(kts) - 1)),
                             reads=['onesR', ppk], writes=[pdk])
                        cur = nxt_
                    rd = rden[qcn % 2]
                    o = ob[qcn % 2]
                    k.op('dve', lambda e: e.reciprocal(out=rd[:, :tn], in_=pd[:, :tn]), reads=[pdk], writes=[f'at_rd{qcn % 2}'])
                    k.op('dve', lambda e: e.tensor_tensor(out=o[:, :tn], in0=pn[:, :tn], in1=rd[:, :tn], op=ALU.mult),
                         reads=[pnk, f'at_rd{qcn % 2}'], writes=[f'at_o{qcn % 2}'])
                    k.dma('sp', _dma(catT[h * 128:(h + 1) * 128, t0:t0 + tn], o[:, :tn]), reads=[f'at_o{qcn % 2}'], writes=[f'catT/{h}_{t0}'])
            k.barrier()

    def st_sconv(self, hlT, catT, row0=1664):
        nc, k = self.nc, self.k
        cw = self.inp('sconv_w', [128, 16, 3])
        with ExitStack() as es:
            sb = self.sbf(es)
            cw_sb = sb('cw', [128, 16, 3])
            k.dma('sp', _dma(cw_sb, cw), writes=['cw'])
            bufs = [[sb(f'sc_{nm}{i}', [128, T]) for nm in ('gb', 'gc', 'hh', 'y')] for i in range(2)]
            segs = [(0, NLAT), (NLAT, T)]
            for c in range(16):
                i = c % 2
                gb, gc, hh, y = bufs[i]
                kk = [f'sc_{nm}{i}' for nm in ('gb', 'gc', 'hh', 'y')]
                for j, buf in enumerate((gb, gc, hh)):
                    r0 = row0 + j * 2048 + c * 128
                    k.dma('sp', _dma(buf, hlT[r0:r0 + 128, :]), writes=[kk[j]])
                k.op('dve', lambda e: e.tensor_tensor(out=gc, in0=gc, in1=hh, op=ALU.mult), reads=[kk[1], kk[2]], writes=[kk[1]])
                k.op('act', lambda e: e.activation(out=y, in_=gc, func=AF.Copy, scale=cw_sb[:, c, 1:2]), reads=[kk[1], 'cw'], writes=[kk[3]])
                for (a, bnd) in segs:
                    k.op('dve', lambda e: e.scalar_tensor_tensor(out=y[:, a + 1:bnd], in0=gc[:, a:bnd - 1], scalar=cw_sb[:, c, 0:1], in1=y[:, a + 1:bnd],
                                                                 op0=ALU.mult, op1=ALU.add), reads=[kk[1], kk[3], 'cw'], writes=[kk[3]])
                    k.op('dve', lambda e: e.scalar_tensor_tensor(out=y[:, a:bnd - 1], in0=gc[:, a + 1:bnd], scalar=cw_sb[:, c, 2:3], in1=y[:, a:bnd - 1],
                                                                 op0=ALU.mult, op1=ALU.add), reads=[kk[1], kk[3], 'cw'], writes=[kk[3]])
                k.op('pool', lambda e: e.tensor_tensor(out=y, in0=y, in1=gb, op=ALU.mult), reads=[kk[0], kk[3]], writes=[kk[3]])
                k.dma('sp', _dma(catT[2048 + c * 128:2048 + (c + 1) * 128, :], y), reads=[kk[3]], writes=[f'catT/s{c}'])
            k.barrier()

    def st_outproj(self, l, catT, hsrc, hdst, with_ctx=True):
        nc, k = self.nc, self.k
        outW = self.inp(f'outW{l}', [32, 128, 32, 128])
        mod = self.mod[l]
        with ExitStack() as es:
            sb = self.sbf(es)
            self._lw = {}
            xs = sb('op_xs', [128, 32, 1024], F32R)
            ht = [sb(f'op_h{i}', [128, 512]) for i in range(3)]
            cnt = [0]
            for (T0, TN, isc) in TCH2:
                if isc and not with_ctx:
                    continue
                j = 1 if isc else 0
                subs = [(off, T0 + off, min(512, TN - off)) for off in range(0, TN, 512)]
                for (off, t0, tn) in subs:
                    k.dma('pool', _dma(xs[:, :, off:off + tn], catT[:, t0:t0 + tn].rearrange("(c p) t -> p c t", p=128)), writes=[f'op_xs/{off}'])

                def epi(mt, ps, pk, t0, tn, j=j):
                    i = cnt[0] = cnt[0] + 1
                    h = ht[i % 3]
                    hk = f'op_h{i % 3}'
                    k.dma('sp', _dma(h[:, :tn], hsrc[mt * 128:(mt + 1) * 128, t0:t0 + tn]), writes=[hk])
                    k.op('dve', lambda e: e.scalar_tensor_tensor(out=h[:, :tn], in0=ps[:, :tn], scalar=mod[:, j, 64 + mt:65 + mt], in1=h[:, :tn],
                                                                 op0=ALU.mult, op1=ALU.add), reads=[pk, hk, f'mod{l}'], writes=[hk])
                    k.dma('sp', _dma(hdst[mt * 128:(mt + 1) * 128, t0:t0 + tn], h[:, :tn]), reads=[hk], writes=[f'hdst/{mt}_{t0}'])
                self.linear2(es, xs, 'op_xs', 32, [outW[i] for i in range(32)], 32, subs, epi, 'op')
            k.barrier()

    def st_moe(self, l, hsrc, hdst, with_ctx):
        nc, k = self.nc, self.k
        mod = self.mod[l]
        chunks = [c for c in TCH if (with_ctx or not c[2])]
        ntok = sum(c[1] for c in chunks)
        NT = ntok // 128
        NP = (ntok * 4) // 256 + NE
        NB = 2 * NP
        rw = self.inp(f'routerW{l}', [128, 32, 32])
        rb = self.inp(f'routerb{l}', [1, 32])
        wgu = self.inp(f'moe_wgu{l}', [NE * D, 2 * DE])
        bgu = self.inp(f'moe_bgu{l}', [NE, 2 * DE])
        wdn = self.inp(f'moe_wdn{l}', [NE * DE, D])
        bdn = self.inp(f'moe_bdn{l}', [NE, D])
        pidx = self.I['pidx'] if 'pidx' in self.I else self.inp('pidx', [128, 1])
        blk128 = self.I['blk128'] if 'blk128' in self.I else self.inp('blk128', [128, NB0])
        HD = D // 2
        if 'u2' not in self.S:
            self.scratch('u2', [T, D])
            for i in range(2):
                self.scratch(f'xslots_{i}', [NB0 * 128, HD])
                self.scratch(f'yslots_{i}', [NB0 * 128, HD])
        u2 = self.S['u2']
        xsl = [self.S[f'xslots_{i}'] for i in range(2)]
        ysl = [self.S[f'yslots_{i}'] for i in range(2)]
        with ExitStack() as esg:
            gsb = self.sbf(esg)
            lg_all = gsb('lg_all', [128, NT, 32]); top8 = gsb('top8', [128, NT, 8]); rank_all = gsb('rank_all', [128, NT, 32])
            gate_all = gsb('gate_all', [128, NT, 4]); dest_f = gsb('dest_f', [128, NT * 4]); dest_i = gsb('dest_i', [128, NT * 4], I32)
            base = gsb('base', [128, 32]); pstart = gsb('pstart', [128, 32]); blk_i = gsb('blk_i', [128, NB], I32)
            widx = gsb('widx', [128, NB], I32); didx = gsb('didx', [128, NB], I32)
            with ExitStack() as es:
                sb = self.sbf(es)
                sb_ = self.mod_bufs(sb)
                rw_sb = sb('rw', [128, 32, 32], F32R); rb_sb = sb('rb', [1, 32], F32R)
                k.dma('pool', _dma(rw_sb, rw), writes=['rw'])
                k.dma('pool', _dma(rb_sb, rb), writes=['rb'])
                k.op('dve', lambda e: e.memset(base, 0.0), writes=['base'])
                mask = [sb(f'mask{i}', [128, 32]) for i in range(2)]
                sm = [sb(f'sm{i}', [128, 8]) for i in range(2)]
                u2tm = [sb(f'u2tm{i}', [128, D]) for i in range(2)]
                ti = 0
                trn = 0
                for (t0, tn, isc) in chunks:
                    j = 1 if isc else 0
                    xs = self.load_modulate(sb_, hsrc, t0, tn, self.opsc[l][:, j, 1, :], mod[:, j, 96:128], 'mo')
                    for tt in range(tn // 128):
                        tsl = slice(tt * 128, (tt + 1) * 128)
                        pr = self.ps[3 + ti % 2]
                        prk = f'ps{3 + ti % 2}'
                        for c in range(KC):
                            k.op('pe', lambda e: e.matmul(pr[:, 64:96], lhsT=xs[:, c, tsl], rhs=rw_sb[:, c, :], start=(c == 0), stop=False),
                                 reads=['xs', 'rw'], writes=[prk])
                        k.op('pe', lambda e: e.matmul(pr[:, 64:96], lhsT=self.onesR[0:1, :], rhs=rb_sb, start=False, stop=True),
                             reads=['onesR', 'rb'], writes=[prk])
                        lg = lg_all[:, ti, :]
                        k.op('act', lambda e: e.activation(out=lg, in_=pr[:, 64:96], func=AF.Copy), reads=[prk], writes=['lg_all'])
                        k.op('dve', lambda e: e.max(out=top8[:, ti, :], in_=lg), reads=['lg_all'], writes=['top8'])
                        m = mask[ti % 2]
                        mk = f'mask{ti % 2}'
                        k.op('dve', lambda e: e.tensor_scalar(out=m, in0=lg, scalar1=top8[:, ti, 3:4], scalar2=None, op0=ALU.is_ge),
                             reads=['lg_all', 'top8'], writes=[mk])
                        s_ = sm[ti % 2]
                        sk = f'sm{ti % 2}'
                        k.op('dve', lambda e: e.tensor_scalar(out=s_[:, 0:1], in0=top8[:, ti, 0:1], scalar1=-1.0, scalar2=None, op0=ALU.mult),
                             reads=['top8'], writes=[sk])
                        k.op('act', lambda e: e.activation(out=s_[:, 4:8], in_=top8[:, ti, 0:4], func=AF.Exp, bias=s_[:, 0:1], scale=1.0),
                             reads=['top8', sk], writes=[sk])
                        k.op('dve', lambda e: e.tensor_reduce(out=s_[:, 1:2], in_=s_[:, 4:8], axis=AX.X, op=ALU.add), reads=[sk], writes=[sk])
                        k.op('dve', lambda e: e.reciprocal(out=s_[:, 2:3], in_=s_[:, 1:2]), reads=[sk], writes=[sk])
                        k.op('dve', lambda e: e.tensor_scalar(out=gate_all[:, ti, :], in0=s_[:, 4:8], scalar1=s_[:, 2:3], scalar2=None, op0=ALU.mult),
                             reads=[sk], writes=['gate_all'])
                        k.op('pe', lambda e: e.matmul(pr[:, 0:32], lhsT=self.triu, rhs=m, start=True, stop=True), reads=['triu', mk], writes=[prk])
                        k.op('pe', lambda e: e.matmul(pr[:, 32:64], lhsT=self.ones, rhs=m, start=True, stop=True), reads=['ones', mk], writes=[prk])
                        k.op('dve', lambda e: e.tensor_tensor(out=rank_all[:, ti, :], in0=pr[:, 0:32], in1=base, op=ALU.add),
                             reads=[prk, 'base'], writes=['rank_all'])
                        k.op('dve', lambda e: e.tensor_tensor(out=base, in0=pr[:, 32:64], in1=base, op=ALU.add), reads=[prk, 'base'], writes=['base'])
                        ut = u2tm[ti % 2]
                        uk = f'u2tm{ti % 2}'
                        for g in range(8):
                            trn += 1
                            pt = self.ps[5 + trn % 2]
                            ptk = f'ps{5 + trn % 2}'
                            for jj in range(4):
                                c = 4 * g + jj
                                k.op('pe', lambda e: e.transpose(out=pt[:, jj * 128:(jj + 1) * 128].bitcast(F32R), in_=xs[:, c, tsl], identity=self.identR),
                                     reads=['xs', 'identR'], writes=[ptk])
                            if g % 2 == 0:
                                k.op('act', lambda e: e.activation(out=ut[:, g * 512:(g + 1) * 512], in_=pt, func=AF.Copy), reads=[ptk], writes=[uk])
                            else:
                                k.op('dve', lambda e: e.tensor_copy(out=ut[:, g * 512:(g + 1) * 512], in_=pt), reads=[ptk], writes=[uk])
                        k.dma('sp', _dma(u2[ti * 128:(ti + 1) * 128, :], ut), reads=[uk], writes=[f'u2/{ti}'])
                        ti += 1
                k.barrier()
            with ExitStack() as es:
                sb = self.sbf(es)
                t1 = sb('t1', [128, 32]); t2 = sb('t2', [128, 32]); padded = sb('padded', [128, 32])
                cs = [sb(f'cs{i}', [128, 32]) for i in range(2)]
                b128 = sb('b128', [128, NB]); acc = sb('acc', [128, NB])
                k.dma('sp', _dma(b128, blk128[:, :NB]), writes=['b128'])
                k.op('dve', lambda e: e.tensor_scalar(out=t1, in0=base, scalar1=255.0, scalar2=None, op0=ALU.add), reads=['base'], writes=['t1'])
                ti1 = sb('ti1', [128, 32], I32); ti2 = sb('ti2', [128, 32], I32)
                k.op('dve', lambda e: e.tensor_copy(out=ti1, in_=t1), reads=['t1'], writes=['ti1'])
                k.op('dve', lambda e: e.tensor_scalar(out=ti2, in0=ti1, scalar1=8, scalar2=8, op0=ALU.arith_shift_right, op1=ALU.logical_shift_left),
                     reads=['ti1'], writes=['ti2'])
                k.op('dve', lambda e: e.tensor_copy(out=padded, in_=ti2), reads=['ti2'], writes=['padded'])
                k.op('dve', lambda e: e.tensor_copy(out=cs[0], in_=padded), reads=['padded'], writes=['cs0'])
                cur = 0
                for sh in (1, 2, 4, 8, 16):
                    a, b_ = cs[cur], cs[1 - cur]
                    k.op('dve', lambda e: e.tensor_copy(out=b_[:, 0:sh], in_=a[:, 0:sh]), reads=[f'cs{cur}'], writes=[f'cs{1 - cur}'])
                    k.op('dve', lambda e: e.tensor_tensor(out=b_[:, sh:32], in0=a[:, sh:32], in1=a[:, 0:32 - sh], op=ALU.add),
                         reads=[f'cs{cur}'], writes=[f'cs{1 - cur}'])
                    cur = 1 - cur
                pend = cs[cur]
                pek = f'cs{cur}'
                k.op('dve', lambda e: e.tensor_tensor(out=pstart, in0=pend, in1=padded, op=ALU.subtract), reads=[pek, 'padded'], writes=['pstart'])
                k.op('dve', lambda e: e.memset(acc, 0.0), writes=['acc'])
                for ex in range(NE):
                    k.op('dve', lambda e: e.scalar_tensor_tensor(out=acc, in0=b128, scalar=pend[:, ex:ex + 1], in1=acc, op0=ALU.is_ge, op1=ALU.add),
                         reads=['b128', pek, 'acc'], writes=['acc'])
                k.op('dve', lambda e: e.tensor_scalar(out=acc, in0=acc, scalar1=float(NE - 1), scalar2=None, op0=ALU.min), reads=['acc'], writes=['acc'])
                k.op('dve', lambda e: e.tensor_copy(out=blk_i, in_=acc), reads=['acc'], writes=['blk_i'])
                pix = sb('pix', [128, 1]); acc2 = sb('acc2', [128, NB])
                k.dma('sp', _dma(pix, pidx), writes=['pix'])
                k.op('dve', lambda e: e.tensor_scalar(out=acc2, in0=acc, scalar1=float(D), scalar2=pix[:, 0:1], op0=ALU.mult, op1=ALU.add), reads=['acc', 'pix'], writes=['acc2'])
                k.op('dve', lambda e: e.tensor_copy(out=widx, in_=acc2), reads=['acc2'], writes=['widx'])
                k.op('dve', lambda e: e.tensor_scalar(out=acc2, in0=acc, scalar1=float(DE), scalar2=pix[:, 0:1], op0=ALU.mult, op1=ALU.add), reads=['acc', 'pix'], writes=['acc2'])
                k.op('dve', lambda e: e.tensor_copy(out=didx, in_=acc2), reads=['acc2'], writes=['didx'])
                dall = sb('dall', [128, 32]); prod = [sb(f'prod{i}', [128, 32]) for i in range(2)]
                for ti in range(NT):
                    k.op('dve', lambda e: e.tensor_tensor(out=dall, in0=rank_all[:, ti, :], in1=pstart, op=ALU.add),
                         reads=['rank_all', 'pstart'], writes=['dall'])
                    for k4 in range(4):
                        p_ = prod[k4 % 2]
                        k.op('dve', lambda e: e.scalar_tensor_tensor(out=p_, in0=lg_all[:, ti, :], scalar=top8[:, ti, k4:k4 + 1], in1=dall,
                                                                     op0=ALU.is_equal, op1=ALU.mult), reads=['lg_all', 'top8', 'dall'], writes=[f'prod{k4 % 2}'])
                        k.op('dve', lambda e: e.tensor_reduce(out=dest_f[:, ti * 4 + k4:ti * 4 + k4 + 1], in_=p_, axis=AX.X, op=ALU.add),
                             reads=[f'prod{k4 % 2}'], writes=['dest_f'])
                k.op('dve', lambda e: e.tensor_scalar(out=dest_f, in0=dest_f, scalar1=0.0, scalar2=float(NB * 128 - 1), op0=ALU.max, op1=ALU.min), reads=['dest_f'], writes=['dest_f'])
                k.op('dve', lambda e: e.tensor_copy(out=dest_i, in_=dest_f), reads=['dest_f'], writes=['dest_i'])
                if self.dbg:
                    dd = self.scratch(f'dbg_dest{l}', [128, NT * 4], I32)
                    k.dma('sp', _dma(dd, dest_i), reads=['dest_i'], writes=['dbgd'])
                    db = self.scratch(f'dbg_blk{l}', [128, NB], I32)
                    k.dma('sp', _dma(db, blk_i), reads=['blk_i'], writes=['dbgb'])
                    dg = self.scratch(f'dbg_gate{l}', [128, NT, 4])
                    k.dma('sp', _dma(dg, gate_all), reads=['gate_all'], writes=['dbgg'])
                xrow = [sb(f'xrow{i}', [128, D]) for i in range(2)]
                for ti in range(NT):
                    xr = xrow[ti % 2]
                    xk = f'xrow{ti % 2}'
                    k.dma('sp', _dma(xr, u2[ti * 128:(ti + 1) * 128, :]), writes=[xk])
                    for k4 in range(4):
                        col = ti * 4 + k4
                        for hf in range(2):
                            k.dma('pool', lambda e: e.indirect_dma_start(out=xsl[hf], out_offset=bass.IndirectOffsetOnAxis(ap=dest_i[:, col:col + 1], axis=0),
                                                                         in_=xr[:, hf * HD:(hf + 1) * HD], in_offset=None), reads=[xk, 'dest_i'], writes=[f'xsl{hf}/{col}'])
                k.barrier()
            with ExitStack() as es:
                sb = self.sbf(es)
                xbs = [sb(f'xb{i}', [128, D]) for i in range(1)]
                XT = [sb(f'XT{i}', [128, 32, 128], F32R) for i in range(2)]
                wg = [sb(f'wg{i}', [128, 2 * DE], F32R) for i in range(3)]
                bg = sb('bg', [128, 2 * DE])
                gu = sb('gu', [128, 2 * DE]); gm = sb('gm', [128, DE]); sg = sb('sg', [128, DE]); upc = sb('upc', [128, DE]); aa = upc
                aT = sb('aT', [128, 5, 128], F32R)
                wd = sb('wd', [128, 5, D], F32R)
                bd = sb('bd', [128, D])
                yb = [sb(f'yb{i}', [128, 512]) for i in range(2)]
                wcnt = 0
                ycnt = 0
                trn = 0

                def gather(out, src, idx_ap, eoff, keys_w):
                    k.dma('pool', lambda e: e.indirect_dma_start(out=out, out_offset=None, in_=src,
                                                                 in_offset=bass.IndirectOffsetOnAxis(ap=idx_ap, axis=0), element_offset=eoff),
                          reads=['widx', 'didx', 'blk_i'], writes=keys_w)
                nts = [(0, 512), (512, 512), (1024, 256)]
                for pr in range(NP):
                    b0 = 2 * pr
                    for s_ in range(2):
                        blk = b0 + s_
                        xb = xbs[0]
                        xbk = 'xb0'
                        for hf in range(2):
                            k.dma('sp', _dma(xb[:, hf * HD:(hf + 1) * HD], xsl[hf][blk * 128:(blk + 1) * 128, :]), writes=[f'{xbk}/{hf}'])
                        for g in range(8):
                            trn += 1
                            pt = self.ps[6 + trn % 2]
                            ptk = f'ps{6 + trn % 2}'
                            for jj in range(4):
                                c = 4 * g + jj
                                k.op('pe', lambda e: e.transpose(out=pt[:, jj * 128:(jj + 1) * 128], in_=xb[:, c * 128:(c + 1) * 128], identity=self.ident),
                                     reads=[xbk, 'ident'], writes=[ptk])
                            pv = pt.rearrange("p (j s) -> p j s", s=128)
                            if g % 2 == 0:
                                k.op('act', lambda e: e.activation(out=XT[s_][:, 4 * g:4 * g + 4, :], in_=pv, func=AF.Copy), reads=[ptk], writes=[f'XT{s_}/{g}'])
                            else:
                                k.op('dve', lambda e: e.tensor_copy(out=XT[s_][:, 4 * g:4 * g + 4, :], in_=pv), reads=[ptk], writes=[f'XT{s_}/{g}'])
                    gather(bg, bgu, blk_i[:, b0:b0 + 1], 0, ['bg'])
                    gather(bd, bdn, blk_i[:, b0:b0 + 1], 0, ['bd'])
                    for c in range(5):
                        gather(wd[:, c, :], wdn, didx[:, b0:b0 + 1], c * 128 * D, [f'wd/{c}'])
                    for c in range(KC):
                        wcnt += 1
                        w_ = wg[wcnt % 3]
                        wk = f'wg{wcnt % 3}'
                        gather(w_, wgu, widx[:, b0:b0 + 1], c * 128 * 2 * DE, [wk])
                        for s_ in range(2):
                            for nt, (n0, nw) in enumerate(nts):
                                pi = 3 * s_ + nt
                                k.op('pe', lambda e: e.matmul(self.ps[pi][:, :nw], lhsT=XT[s_][:, c, :], rhs=w_[:, n0:n0 + nw], start=(c == 0), stop=(c == KC - 1)),
                                     reads=[f'XT{s_}', wk], writes=[f'ps{pi}'])
                    for s_ in range(2):
                        blk = b0 + s_
                        for nt, (n0, nw) in enumerate(nts):
                            pi = 3 * s_ + nt
                            k.op('dve', lambda e: e.tensor_tensor(out=gu[:, n0:n0 + nw], in0=self.ps[pi][:, :nw], in1=bg[:, n0:n0 + nw], op=ALU.add),
                                 reads=[f'ps{pi}', 'bg'], writes=['gu'])
                        k.op('dve', lambda e: e.tensor_scalar(out=gm, in0=gu[:, 0:DE], scalar1=7.0, scalar2=None, op0=ALU.min), reads=['gu'], writes=['gm'])
                        k.op('act', lambda e: e.activation(out=sg, in_=gm, func=AF.Sigmoid, scale=1.702), reads=['gm'], writes=['sg'])
                        k.op('dve', lambda e: e.tensor_scalar(out=upc, in0=gu[:, DE:2 * DE], scalar1=-7.0, scalar2=7.0, op0=ALU.max, op1=ALU.min), reads=['gu'], writes=['upc'])
                        k.op('dve', lambda e: e.scalar_tensor_tensor(out=upc, in0=upc, scalar=1.0, in1=gm, op0=ALU.add, op1=ALU.mult), reads=['upc', 'gm'], writes=['upc'])
                        k.op('pool', lambda e: e.tensor_tensor(out=aa, in0=upc, in1=sg, op=ALU.mult), reads=['upc', 'sg'], writes=['upc'])
                        pa, pb = self.ps[6], self.ps[7]
                        for c in range(5):
                            dst = pa[:, c * 128:(c + 1) * 128] if c < 4 else pb[:, 0:128]
                            k.op('pe', lambda e: e.transpose(out=dst, in_=aa[:, c * 128:(c + 1) * 128], identity=self.ident),
                                 reads=['upc', 'ident'], writes=['ps6' if c < 4 else 'ps7'])
                        k.op('act', lambda e: e.activation(out=aT[:, 0:4, :], in_=pa.rearrange("p (j s) -> p j s", s=128), func=AF.Copy), reads=['ps6'], writes=['aT/0'])
                        k.op('dve', lambda e: e.tensor_copy(out=aT[:, 4, :], in_=pb[:, 0:128]), reads=['ps7'], writes=['aT/1'])
                        for nt in range(8):
                            pyi = 6 + nt % 2
                            py = self.ps[pyi]
                            pyk = f'ps{pyi}'
                            for c in range(5):
                                k.op('pe', lambda e: e.matmul(py, lhsT=aT[:, c, :], rhs=wd[:, c, nt * 512:(nt + 1) * 512], start=(c == 0), stop=(c == 4)),
                                     reads=['aT', 'wd'], writes=[pyk])
                            ycnt += 1
                            y_ = yb[ycnt % 2]
                            yk = f'yb{ycnt % 2}'
                            k.op('dve', lambda e: e.tensor_tensor(out=y_, in0=py, in1=bd[:, nt * 512:(nt + 1) * 512], op=ALU.add), reads=[pyk, 'bd'], writes=[yk])
                            k.dma('sp', _dma(ysl[nt // 4][blk * 128:(blk + 1) * 128, (nt % 4) * 512:(nt % 4 + 1) * 512], y_), reads=[yk], writes=[f'ysl/{blk}_{nt}'])
                k.barrier()
            with ExitStack() as es:
                sb = self.sbf(es)
                yr = [sb(f'yr{i}', [128, D]) for i in range(2)]
                accb = [sb(f'accb{i}', [128, D]) for i in range(2)]
                hb = [sb(f'hb{i}', [128, 32, 128]) for i in range(2)]
                yc = 0
                trn = 0
                t_of = []
                for (t0, tn, isc) in chunks:
                    for tt in range(tn // 128):
                        t_of.append((t0 + tt * 128, 1 if isc else 0))
                for ti in range(NT):
                    tok0, j = t_of[ti]
                    a_ = accb[ti % 2]
                    ak = f'accb{ti % 2}'
                    for k4 in range(4):
                        yc += 1
                        y_ = yr[yc % 2]
                        yk = f'yr{yc % 2}'
                        col = ti * 4 + k4
                        for hf in range(2):
                            k.dma('pool', lambda e: e.indirect_dma_start(out=y_[:, hf * HD:(hf + 1) * HD], out_offset=None, in_=ysl[hf],
                                                                         in_offset=bass.IndirectOffsetOnAxis(ap=dest_i[:, col:col + 1], axis=0)),
                                  reads=['dest_i'], writes=[f'{yk}/{hf}'])
                        if k4 == 0:
                            k.op('dve', lambda e: e.tensor_scalar(out=a_, in0=y_, scalar1=gate_all[:, ti, 0:1], scalar2=None, op0=ALU.mult),
                                 reads=[yk, 'gate_all'], writes=[ak])
                        else:
                            eng = 'dve'
                            k.op(eng, lambda e: e.scalar_tensor_tensor(out=a_, in0=y_, scalar=gate_all[:, ti, k4:k4 + 1], in1=a_, op0=ALU.mult, op1=ALU.add),
                                 reads=[yk, 'gate_all', ak], writes=[ak])
                    h_ = hb[ti % 2]
                    hk = f'hb{ti % 2}'
                    k.dma('sp', _dma(h_, hsrc[:, tok0:tok0 + 128].rearrange("(c p) t -> p c t", p=128)), writes=[hk])
                    for g in range(8):
                        trn += 1
                        pt = self.ps[trn % 2]
                        ptk = f'ps{trn % 2}'
                        for jj in range(4):
                            c = 4 * g + jj
                            k.op('pe', lambda e: e.transpose(out=pt[:, jj * 128:(jj + 1) * 128], in_=a_[:, c * 128:(c + 1) * 128], identity=self.ident),
                                 reads=[ak, 'ident'], writes=[ptk])
                        for jj in range(4):
                            c = 4 * g + jj
                            k.op('dve', lambda e: e.scalar_tensor_tensor(out=h_[:, c, :], in0=pt[:, jj * 128:(jj + 1) * 128], scalar=mod[:, j, 160 + c:161 + c],
                                                                         in1=h_[:, c, :], op0=ALU.mult, op1=ALU.add), reads=[ptk, hk, f'mod{l}'], writes=[hk])
                    k.dma('sp', _dma(hdst[:, tok0:tok0 + 128].rearrange("(c p) t -> p c t", p=128), h_), reads=[hk], writes=[f'hdst/{ti}'])
                k.barrier()

    def st_diff_attn(self, hlT, catT, lam_init):
        nc, k = self.nc, self.k
        cos4 = self.inp('cos4', [128, NLAT]); sin4 = self.inp('sin4', [128, NLAT])
        dlam = self.inp('dlam', [1, 256]); subln = self.inp('subln', [128, 1])
        sc = 64.0 ** -0.5
        with ExitStack() as es:
            sb = self.sbf(es)
            c4 = sb('c4', [128, NLAT]); s4 = sb('s4', [128, NLAT])
            k.dma('sp', _dma(c4, cos4), writes=['c4']); k.dma('sp', _dma(s4, sin4), writes=['s4'])
            dl = sb('dl', [1, 256]); sl = sb('sl', [128, 1]); g2 = sb('g2', [128, 1]); nlam = sb('nlam', [128, 1])
            lt = sb('lt', [1, 128]); ls = sb('ls', [1, 8])
            k.dma('sp', _dma(dl, dlam), writes=['dl']); k.dma('sp', _dma(sl, subln), writes=['sl'])
            k.op('dve', lambda e: e.tensor_scalar(out=g2, in0=sl, scalar1=1.0 - lam_init, scalar2=None, op0=ALU.mult), reads=['sl'], writes=['g2'])
            k.op('dve', lambda e: e.tensor_tensor(out=lt[:, 0:64], in0=dl[:, 0:64], in1=dl[:, 64:128], op=ALU.mult), reads=['dl'], writes=['lt'])
            k.op('dve', lambda e: e.tensor_tensor(out=lt[:, 64:128], in0=dl[:, 128:192], in1=dl[:, 192:256], op=ALU.mult), reads=['dl'], writes=['lt'])
            k.op('dve', lambda e: e.tensor_reduce(out=ls[:, 0:1], in_=lt[:, 0:64], axis=AX.X, op=ALU.add), reads=['lt'], writes=['ls'])
            k.op('dve', lambda e: e.tensor_reduce(out=ls[:, 1:2], in_=lt[:, 64:128], axis=AX.X, op=ALU.add), reads=['lt'], writes=['ls'])
            k.op('act', lambda e: e.activation(out=ls[:, 2:4], in_=ls[:, 0:2], func=AF.Exp), reads=['ls'], writes=['ls'])
            k.op('dve', lambda e: e.tensor_tensor(out=ls[:, 4:5], in0=ls[:, 3:4], in1=ls[:, 2:3], op=ALU.subtract), reads=['ls'], writes=['ls'])
            k.op('dve', lambda e: e.tensor_scalar(out=ls[:, 5:6], in0=ls[:, 4:5], scalar1=-lam_init, scalar2=None, op0=ALU.add), reads=['ls'], writes=['ls'])
            k.op('pe', lambda e: e.matmul(self.ps[7][:, 0:1], lhsT=self.ones[0:1, :], rhs=ls[:, 5:6], start=True, stop=True), reads=['ones', 'ls'], writes=['ps7'])
            k.op('dve', lambda e: e.tensor_copy(out=nlam, in_=self.ps[7][:, 0:1]), reads=['ps7'], writes=['nlam'])
            raw = {n: sb(f'raw_{n}', [128, T]) for n in ('q', 'qs', 'k', 'ks', 'v')}
            t1 = sb('t1', [128, NLAT]); t2 = sb('t2', [128, NLAT])
            qr = [sb(f'qr{i}', [128, NLAT], F32R) for i in range(2)]
            kr = [sb(f'kr{i}', [128, T], F32R) for i in range(2)]
            vm = [sb(f'vm{i}', [128, 18, 128], F32R) for i in range(2)]
            pT = [sb(f'p{i}', [128, 512], F32R) for i in range(3)]
            rd = sb('rd', [128, 512]); o0 = sb('o0', [128, 512]); o1 = sb('o1', [128, 512]); sq = sb('sq', [128, 512], F32R)
            rstd = sb('rstd', [128, 512]); oo = [sb(f'oo{i}', [128, 512]) for i in range(2)]
            pc = 0
            qcn = 0
            for h in range(16):
                b = h % 2
                for n, mt, w in (('q', h, NLAT), ('qs', 16 + h, NLAT), ('k', 32 + h, T), ('ks', 48 + h, NLAT), ('v', 64 + h, T)):
                    k.dma('sp', _dma(raw[n][:, :w], hlT[mt * 128:(mt + 1) * 128, 0:w]), writes=[f'raw_{n}'])
                for (x, xsw, dst, dk) in ((raw['q'], raw['qs'], qr[b], f'qr{b}'), (raw['k'], raw['ks'], kr[b], f'kr{b}')):
                    xk = 'raw_q' if x is raw['q'] else 'raw_k'
                    xsk = 'raw_qs' if x is raw['q'] else 'raw_ks'
                    k.op('dve', lambda e: e.tensor_tensor(out=t1, in0=x[:, :NLAT], in1=c4, op=ALU.mult), reads=[xk, 'c4'], writes=['t1'])
                    k.op('pool', lambda e: e.tensor_tensor(out=t2, in0=xsw[:, :NLAT], in1=s4, op=ALU.mult), reads=[xsk, 's4'], writes=['t2'])
                    k.op('dve', lambda e: e.tensor_tensor(out=dst[:, :NLAT], in0=t1, in1=t2, op=ALU.add), reads=['t1', 't2'], writes=[dk])
                k.op('act', lambda e: e.activation(out=kr[b][:, NLAT:T], in_=raw['k'][:, NLAT:T], func=AF.Copy), reads=['raw_k'], writes=[f'kr{b}'])
                for g in range(5):
                    n = min(4, 18 - 4 * g)
                    pst = self.ps[6]
                    for j in range(n):
                        kt = 4 * g + j
                        k.op('pe', lambda e: e.transpose(out=pst[:, j * 128:(j + 1) * 128], in_=raw['v'][:, kt * 128:(kt + 1) * 128], identity=self.ident),
                             reads=['raw_v', 'ident'], writes=['ps6'])
                    k.op('act', lambda e: e.activation(out=vm[b][:, 4 * g:4 * g + n, :], in_=pst[:, :n * 128].rearrange("p (j d) -> p j d", d=128), func=AF.Copy),
                         reads=['ps6'], writes=[f'vm{b}'])
                for (t0, tn, isc) in TCH:
                    if isc:
                        continue
                    qcn += 1
                    for c in range(2):
                        pn = self.ps[2 + c]; pd = self.ps[4 + c]
                        rs = slice(64 * c, 64 * c + 64)
                        def emit_s(kt, t0=t0, tn=tn, b=b, rs=rs):
                            nonlocal pc
                            pc += 1
                            pss = self.ps[pc % 2]; psk = f'ps{pc % 2}'
                            ks = slice(kt * 128, (kt + 1) * 128)
                            k.op('pe', lambda e: e.matmul(pss[:, :tn], lhsT=kr[b][rs, ks], rhs=qr[b][rs, t0:t0 + tn], start=True, stop=True),
                                 reads=[f'kr{b}', f'qr{b}'], writes=[psk])
                            return pss, psk, pc
                        cur = emit_s(0)
                        for kt in range(18):
                            nxt_ = emit_s(kt + 1) if kt + 1 < 18 else None
                            pss, psk, pci = cur
                            p = pT[pci % 3]; ppk = f'p{pci % 3}'
                            k.op('act', lambda e: e.activation(out=p[:, :tn], in_=pss[:, :tn], func=AF.Exp, scale=sc), reads=[psk], writes=[ppk])
                            k.op('pe', lambda e: e.matmul(pn[:, :tn], lhsT=vm[b][:, kt, :], rhs=p[:, :tn], start=(kt == 0), stop=(kt == 17)),
                                 reads=[f'vm{b}', ppk], writes=[f'ps{2 + c}'])
                            k.op('pe', lambda e: e.matmul(pd[:, :tn], lhsT=self.onesR, rhs=p[:, :tn], start=(kt == 0), stop=(kt == 17)),
                                 reads=['onesR', ppk], writes=[f'ps{4 + c}'])
                            cur = nxt_
                        oc = o0 if c == 0 else o1
                        k.op('dve', lambda e: e.reciprocal(out=rd[:, :tn], in_=pd[:, :tn]), reads=[f'ps{4 + c}'], writes=['rd'])
                        k.op('dve', lambda e: e.tensor_tensor(out=oc[:, :tn], in0=pn[:, :tn], in1=rd[:, :tn], op=ALU.mult), reads=[f'ps{2 + c}', 'rd'], writes=[f'o{c}'])
                    k.op('dve', lambda e: e.scalar_tensor_tensor(out=o0[:, :tn], in0=o1[:, :tn], scalar=nlam[:, 0:1], in1=o0[:, :tn], op0=ALU.mult, op1=ALU.add),
                         reads=['o0', 'o1', 'nlam'], writes=['o0'])
                    k.op('act', lambda e: e.activation(out=sq[:, :tn], in_=o0[:, :tn], func=AF.Square), reads=['o0'], writes=['sq'])
                    k.op('pe', lambda e: e.matmul(self.ps[7][:, :tn], lhsT=self.onesR, rhs=sq[:, :tn], start=True, stop=True), reads=['onesR', 'sq'], writes=['ps7'])
                    self.rstd_from_ps(self.ps[7], rstd, tn, 128, 'ps7', 'rstd')
                    ob = oo[qcn % 2]
                    k.op('dve', lambda e: e.scalar_tensor_tensor(out=ob[:, :tn], in0=o0[:, :tn], scalar=g2[:, 0:1], in1=rstd[:, :tn], op0=ALU.mult, op1=ALU.mult),
                         reads=['o0', 'g2', 'rstd'], writes=[f'oo{qcn % 2}'])
                    k.dma('sp', _dma(catT[h * 128:(h + 1) * 128, t0:t0 + tn], ob[:, :tn]), reads=[f'oo{qcn % 2}'], writes=[f'catT/{h}_{t0}'])
            k.barrier()

    def st_hyena(self, hlT, catT, row0):
        nc, k = self.nc, self.k
        N2 = 2 * NLAT
        hcw = self.inp('hy_convw', [128, 48, 3])
        zf = self.inp('hy_zfeatT', [33, NLAT]); w1 = self.inp('hy_w1', [33, 64]); b1 = self.inp('hy_b1', [64, 1])
        w2 = self.inp('hy_w2', [64, 64]); b2 = self.inp('hy_b2', [64, 1]); w3 = self.inp('hy_w3T', [64, 64, 128])
        tnb = self.inp('hy_tnorm', [128, NLAT]); ndec = self.inp('hy_ndecay', [128, 16]); skip = self.inp('hy_skip', [128, 2, 16])
        Ct = self.inp('dft_C', [16, 128, 16, 128]); St = self.inp('dft_S', [16, 128, 16, 128])
        CTt = self.inp('dft_CT', [8, 128, 16, 256]); nSTt = self.inp('dft_nST', [8, 128, 16, 256])
        hyc = self.scratch('hyc', [6144, NLAT])
        edn = self.scratch('hy_edn', [2, 2, 2048, NLAT])
        hz = self.scratch('hy_z1', [2048, NLAT])
        with ExitStack() as es:
            sb = self.sbf(es)
            cw = sb('cw', [128, 48, 3])
            k.dma('sp', _dma(cw, hcw), writes=['cw'])
            pb = [sb(f'p{i}', [128, NLAT]) for i in range(2)]; yb = [sb(f'y{i}', [128, NLAT]) for i in range(2)]
            for c in range(48):
                i = c % 2
                p, y = pb[i], yb[i]
                pk, yk = f'p{i}', f'y{i}'
                k.dma('sp', _dma(p, hlT[row0 + c * 128:row0 + (c + 1) * 128, 0:NLAT]), writes=[pk])
                k.op('act', lambda e: e.activation(out=y, in_=p, func=AF.Copy, scale=cw[:, c, 1:2]), reads=[pk, 'cw'], writes=[yk])
                k.op('dve', lambda e: e.scalar_tensor_tensor(out=y[:, 1:NLAT], in0=p[:, 0:NLAT - 1], scalar=cw[:, c, 0:1], in1=y[:, 1:NLAT], op0=ALU.mult, op1=ALU.add),
                     reads=[pk, yk, 'cw'], writes=[yk])
                k.op('dve', lambda e: e.scalar_tensor_tensor(out=y[:, 0:NLAT - 1], in0=p[:, 1:NLAT], scalar=cw[:, c, 2:3], in1=y[:, 0:NLAT - 1], op0=ALU.mult, op1=ALU.add),
                     reads=[pk, yk, 'cw'], writes=[yk])
                k.dma('sp', _dma(hyc[c * 128:(c + 1) * 128, :], y), reads=[yk], writes=[f'hyc/{c}'])
            k.barrier()
        with ExitStack() as es:
            sb = self.sbf(es)
            zfr = sb('zfr', [33, NLAT], F32R); w1r = sb('w1r', [33, 64], F32R); w2r = sb('w2r', [64, 64], F32R)
            b1s = sb('b1s', [64, 1]); b2s = sb('b2s', [64, 1])
            w3r = [sb(f'w3r{i}', [64, 128], F32R) for i in range(4)]
            tn_sb = sb('tn_sb', [128, NLAT]); nd = sb('nd', [128, 16])
            for dst, src, q, kk in ((zfr, zf, 'pool', 'zfr'), (w1r, w1, 'pool', 'w1r'), (w2r, w2, 'pool', 'w2r'), (b1s, b1, 'sp', 'b1s'), (b2s, b2, 'sp', 'b2s'),
                                    (tn_sb, tnb, 'sp', 'tn_sb'), (nd, ndec, 'sp', 'nd')):
                k.dma(q, _dma(dst, src), writes=[kk])
            hid = [sb(f'hid{i}', [64, NLAT], F32R) for i in range(2)]
            ya = sb('ya', [64, 512]); yq = sb('yq', [64, 512]); yi = sb('yi', [64, 512], I32); ym = sb('ym', [64, 512])
            TWO_PI = 2.0 * math.pi

            def sin_layer(wr, wk, src, srck, bs, bk, dst, dstk):
                for q4 in range(4):
                    ts = slice(q4 * 512, (q4 + 1) * 512)
                    ps = self.ps[q4 % 2]; pk = f'ps{q4 % 2}'
                    k.op('pe', lambda e: e.matmul(ps[0:64, :], lhsT=wr, rhs=src[:, ts], start=True, stop=True), reads=[wk, srck], writes=[pk])
                    k.op('dve', lambda e: e.tensor_scalar(out=ya, in0=ps[0:64, :], scalar1=bs[:, 0:1], scalar2=None, op0=ALU.add), reads=[pk, bk], writes=['ya'])
                    k.op('dve', lambda e: e.tensor_scalar(out=yq, in0=ya, scalar1=1.0 / TWO_PI, scalar2=None, op0=ALU.mult), reads=['ya'], writes=['yq'])
                    k.op('dve', lambda e: e.tensor_copy(out=yi, in_=yq), reads=['yq'], writes=['yi'])
                    k.op('dve', lambda e: e.tensor_copy(out=yq, in_=yi), reads=['yi'], writes=['yq'])
                    k.op('dve', lambda e: e.scalar_tensor_tensor(out=ya, in0=yq, scalar=-TWO_PI, in1=ya, op0=ALU.mult, op1=ALU.add), reads=['yq', 'ya'], writes=['ya'])
                    k.op('dve', lambda e: e.tensor_scalar(out=ym, in0=ya, scalar1=math.pi, scalar2=None, op0=ALU.is_gt), reads=['ya'], writes=['ym'])
                    k.op('dve', lambda e: e.scalar_tensor_tensor(out=ya, in0=ym, scalar=-TWO_PI, in1=ya, op0=ALU.mult, op1=ALU.add), reads=['ym', 'ya'], writes=['ya'])
                    k.op('dve', lambda e: e.tensor_scalar(out=ym, in0=ya, scalar1=-math.pi, scalar2=None, op0=ALU.is_lt), reads=['ya'], writes=['ym'])
                    k.op('dve', lambda e: e.scalar_tensor_tensor(out=ya, in0=ym, scalar=TWO_PI, in1=ya, op0=ALU.mult, op1=ALU.add), reads=['ym', 'ya'], writes=['ya'])
                    k.op('dve', lambda e: e.tensor_scalar(out=ya, in0=ya, scalar1=-3.1415925, scalar2=3.1415925, op0=ALU.max, op1=ALU.min), reads=['ya'], writes=['ya'])
                    k.op('act', lambda e: e.activation(out=dst[:, ts], in_=ya, func=AF.Sin), reads=['ya'], writes=[dstk])
            sin_layer(w1r, 'w1r', zfr, 'zfr', b1s, 'b1s', hid[0], 'hid0')
            sin_layer(w2r, 'w2r', hid[0], 'hid0', b2s, 'b2s', hid[1], 'hid1')
            h2 = hid[1]
            win = sb('win', [128, NLAT]); fw = sb('fw', [128, NLAT]); bw = sb('bw', [128, NLAT]); ab = sb('ab', [128, NLAT])
            ee = [sb(f'ee{i}', [128, NLAT]) for i in range(2)]; dd = [sb(f'dd{i}', [128, NLAT]) for i in range(2)]
            l1 = sb('l1', [128, 4])
            wc = 0
            it = 0
            for cc in range(16):
                k.op('act', lambda e: e.activation(out=win, in_=tn_sb, func=AF.Exp, scale=nd[:, cc:cc + 1]), reads=['tn_sb', 'nd'], writes=['win'])
                k.op('dve', lambda e: e.tensor_scalar(out=win, in0=win, scalar1=0.05, scalar2=None, op0=ALU.add), reads=['win'], writes=['win'])
                for o in range(2):
                    it += 1
                    for di, dst, dk in ((0, fw, 'fw'), (1, bw, 'bw')):
                        mt = o * 32 + di * 16 + cc
                        wc += 1
                        wr = w3r[wc % 4]; wk = f'w3r{wc % 4}'
                        k.dma('pool', _dma(wr, w3[mt]), writes=[wk])
                        for q4 in range(4):
                            ts = slice(q4 * 512, (q4 + 1) * 512)
                            ps = self.ps[2 + q4 % 2]; pk = f'ps{2 + q4 % 2}'
                            k.op('pe', lambda e: e.matmul(ps, lhsT=wr, rhs=h2[:, ts], start=True, stop=True), reads=[wk, 'hid1'], writes=[pk])
                            k.op('dve', lambda e: e.tensor_tensor(out=dst[:, ts], in0=ps, in1=win[:, ts], op=ALU.mult), reads=[pk, 'win'], writes=[dk])
                    e_, d_ = ee[it % 2], dd[it % 2]
                    ek, dk = f'ee{it % 2}', f'dd{it % 2}'
                    k.op('dve', lambda e: e.tensor_tensor(out=e_, in0=fw, in1=bw, op=ALU.add), reads=['fw', 'bw'], writes=[ek])
                    k.op('pool', lambda e: e.tensor_tensor(out=d_, in0=bw, in1=fw, op=ALU.subtract), reads=['fw', 'bw'], writes=[dk])
                    k.op('act', lambda e: e.activation(out=ab[:, 1:NLAT], in_=fw[:, 1:NLAT], func=AF.Abs, accum_out=l1[:, 0:1]), reads=['fw'], writes=['ab', 'l1'])
                    k.op('act', lambda e: e.activation(out=ab[:, 1:NLAT], in_=bw[:, 1:NLAT], func=AF.Abs, accum_out=l1[:, 1:2]), reads=['bw', 'ab'], writes=['ab', 'l1'])
                    k.op('act', lambda e: e.activation(out=l1[:, 2:3], in_=e_[:, 0:1], func=AF.Abs), reads=[ek, 'l1'], writes=['l1'])
                    k.op('dve', lambda e: e.tensor_tensor(out=l1[:, 0:1], in0=l1[:, 0:1], in1=l1[:, 1:2], op=ALU.add), reads=['l1'], writes=['l1'])
                    k.op('dve', lambda e: e.tensor_tensor(out=l1[:, 0:1], in0=l1[:, 0:1], in1=l1[:, 2:3], op=ALU.add), reads=['l1'], writes=['l1'])
                    k.op('dve', lambda e: e.reciprocal(out=l1[:, 3:4], in_=l1[:, 0:1]), reads=['l1'], writes=['l1'])
                    k.op('dve', lambda e: e.tensor_scalar(out=e_, in0=e_, scalar1=l1[:, 3:4], scalar2=None, op0=ALU.mult), reads=[ek, 'l1'], writes=[ek])
                    k.op('dve', lambda e: e.tensor_scalar(out=d_, in0=d_, scalar1=l1[:, 3:4], scalar2=None, op0=ALU.mult), reads=[dk, 'l1'], writes=[dk])
                    k.dma('sp', _dma(edn[o, 0, cc * 128:(cc + 1) * 128, :], e_), reads=[ek], writes=[f'edn/{o}_0_{cc}'])
                    k.dma('sp', _dma(edn[o, 1, cc * 128:(cc + 1) * 128, :], d_), reads=[dk], writes=[f'edn/{o}_1_{cc}'])
            k.barrier()
        for o in range(2):
            zsrc = hyc if o == 0 else hz
            gate0 = 2048 * (o + 1)
            with ExitStack() as es:
                sb = self.sbf(es)
                sk = sb('sk', [128, 2, 16])
                k.dma('sp', _dma(sk, skip), writes=['sk'])
                zf_ = [sb(f'zf{i}', [128, NLAT]) for i in range(2)]
                ld = [sb(f'ld{i}', [128, NLAT]) for i in range(2)]
                ztm = sb('ztm', [128, 16, 256], F32R); etm = sb('etm', [128, 16, 256], F32R); dtm = sb('dtm', [128, 16, 256], F32R)
                Yre = sb('Yre', [128, 16, 256], F32R); Yim = sb('Yim', [128, 16, 256], F32R)
                Cb = [sb(f'Cb{i}', [128, 16, 128], F32R) for i in range(2)]; Sb = [sb(f'Sb{i}', [128, 16, 128], F32R) for i in range(2)]
                CTb = [sb(f'CTb{i}', [128, 16, 256], F32R) for i in range(1)]; STb = [sb(f'STb{i}', [128, 16, 256], F32R) for i in range(1)]
                hre = sb('hre', [128, 256]); him = sb('him', [128, 256]); ta = sb('ta', [128, 256]); tb = sb('tb', [128, 256])
                u1 = [sb(f'u1{i}', [128, 256]) for i in range(2)]; gt = [sb(f'gt{i}', [128, 256]) for i in range(2)]
                trn = 0
                fcn = 0
                tqn = 0
                for cq in range(8):
                    for half in range(2):
                        ch0 = cq * 256 + half * 128
                        k.dma('sp', _dma(zf_[half], zsrc[ch0:ch0 + 128, :]), writes=[f'zf{half}'])
                        for (src_ap, srck, dst_tm, dstk) in ((zf_[half], f'zf{half}', ztm, 'ztm'), (None, 'e', etm, 'etm'), (None, 'd', dtm, 'dtm')):
                            if src_ap is None:
                                li = 0 if srck == 'e' else 1
                                src_ap = ld[li]
                                k.dma('sp', _dma(src_ap, edn[o, li, ch0:ch0 + 128, :]), writes=[f'ld{li}'])
                                srck = f'ld{li}'
                            for g in range(4):
                                trn += 1
                                pt = self.ps[6 + trn % 2]; ptk = f'ps{6 + trn % 2}'
                                for jj in range(4):
                                    tc = 4 * g + jj
                                    k.op('pe', lambda e: e.transpose(out=pt[:, jj * 128:(jj + 1) * 128], in_=src_ap[:, tc * 128:(tc + 1) * 128], identity=self.ident),
                                         reads=[srck, 'ident'], writes=[ptk])
                                k.op('act' if trn % 2 else 'dve',
                                     (lambda e: e.activation(out=dst_tm[:, 4 * g:4 * g + 4, half * 128:(half + 1) * 128], in_=pt.rearrange("p (j s) -> p j s", s=128), func=AF.Copy)) if trn % 2 else
                                     (lambda e: e.tensor_copy(out=dst_tm[:, 4 * g:4 * g + 4, half * 128:(half + 1) * 128], in_=pt.rearrange("p (j s) -> p j s", s=128))),
                                     reads=[ptk], writes=[f'{dstk}/{half}_{g}'])
                    for ft in range(16):
                        fcn += 1
                        C_, S_ = Cb[fcn % 2], Sb[fcn % 2]
                        ck, skk = f'Cb{fcn % 2}', f'Sb{fcn % 2}'
                        k.dma('pool', _dma(C_, Ct[ft]), writes=[ck])
                        k.dma('pool', _dma(S_, St[ft]), writes=[skk])
                        z0, z1 = (0, 1) if ft % 2 == 0 else (4, 5)
                        for (pi, W_, wk_, X_, xk_) in ((z0, C_, ck, ztm, 'ztm'), (z1, S_, skk, ztm, 'ztm'), (2, C_, ck, etm, 'etm'), (3, S_, skk, dtm, 'dtm')):
                            for tc in range(16):
                                k.op('pe', lambda e: e.matmul(self.ps[pi][:, :256], lhsT=W_[:, tc, :], rhs=X_[:, tc, :], start=(tc == 0), stop=(tc == 15)),
                                     reads=[wk_, xk_], writes=[f'ps{pi}'])
                        zre, zs = self.ps[z0][:, :256], self.ps[z1][:, :256]
                        zrk, zsk = f'ps{z0}', f'ps{z1}'
                        k.op('act', lambda e: e.activation(out=hre, in_=self.ps[2][:, :256], func=AF.Copy), reads=['ps2'], writes=['hre'])
                        k.op('act', lambda e: e.activation(out=him, in_=self.ps[3][:, :256], func=AF.Copy), reads=['ps3'], writes=['him'])
                        k.op('dve', lambda e: e.tensor_tensor(out=ta, in0=zre, in1=hre, op=ALU.mult), reads=[zrk, 'hre'], writes=['ta'])
                        k.op('dve', lambda e: e.tensor_tensor(out=tb, in0=zs, in1=him, op=ALU.mult), reads=[zsk, 'him'], writes=['tb'])
                        k.op('pool', lambda e: e.tensor_tensor(out=Yre[:, ft, :], in0=ta, in1=tb, op=ALU.add), reads=['ta', 'tb'], writes=[f'Yre/{ft}'])
                        k.op('dve', lambda e: e.tensor_tensor(out=ta, in0=zre, in1=him, op=ALU.mult), reads=[zrk, 'him', f'Yre/{ft}'], writes=['ta'])
                        k.op('dve', lambda e: e.tensor_tensor(out=tb, in0=zs, in1=hre, op=ALU.mult), reads=[zsk, 'hre', f'Yre/{ft}'], writes=['tb'])
                        k.op('pool', lambda e: e.tensor_tensor(out=Yim[:, ft, :], in0=ta, in1=tb, op=ALU.subtract), reads=['ta', 'tb'], writes=[f'Yim/{ft}'])
                    for tq in range(8):
                        tqn += 1
                        CT_, ST_ = (CTb[0], STb[0]) if tqn % 2 else (etm, dtm)
                        ctk, stk = ('CTb0', 'STb0') if tqn % 2 else ('etm', 'dtm')
                        k.dma('pool', _dma(CT_, CTt[tq]), writes=[ctk])
                        k.dma('pool', _dma(ST_, nSTt[tq]), writes=[stk])
                        tsl = slice(tq * 256, (tq + 1) * 256)
                        for half in range(2):
                            ch0 = cq * 256 + half * 128
                            cchunk = ch0 // 128
                            py = self.ps[4 + half]; pyk = f'ps{4 + half}'
                            cs_ = slice(half * 128, (half + 1) * 128)
                            for fc in range(16):
                                k.op('pe', lambda e: e.matmul(py[:, :256], lhsT=Yre[:, fc, cs_], rhs=CT_[:, fc, :], start=(fc == 0), stop=False),
                                     reads=['Yre', ctk], writes=[pyk])
                            for fc in range(16):
                                k.op('pe', lambda e: e.matmul(py[:, :256], lhsT=Yim[:, fc, cs_], rhs=ST_[:, fc, :], start=False, stop=(fc == 15)),
                                     reads=['Yim', stk], writes=[pyk])
                            u_ = u1[half]; g_ = gt[half]
                            k.dma('sp', _dma(g_, hyc[gate0 + ch0:gate0 + ch0 + 128, tsl]), writes=[f'gt{half}'])
                            k.op('act', lambda e: e.activation(out=u_, in_=zf_[half][:, tsl], func=AF.Copy, scale=sk[:, o, cchunk:cchunk + 1]),
                                 reads=[f'zf{half}', 'sk'], writes=[f'u1{half}'])
                            k.op('dve', lambda e: e.scalar_tensor_tensor(out=u_, in0=py[:, :256], scalar=2.0 / N2, in1=u_, op0=ALU.mult, op1=ALU.add),
                                 reads=[pyk, f'u1{half}'], writes=[f'u1{half}'])
                            k.op('pool', lambda e: e.tensor_tensor(out=u_, in0=u_, in1=g_, op=ALU.mult), reads=[f'u1{half}', f'gt{half}'], writes=[f'u1{half}'])
                            if o == 0:
                                k.dma('sp', _dma(hz[ch0:ch0 + 128, tsl], u_), reads=[f'u1{half}'], writes=[f'hz/{ch0}_{tq}'])
                            else:
                                k.dma('sp', _dma(catT[2048 + ch0:2048 + ch0 + 128, tsl], u_), reads=[f'u1{half}'], writes=[f'catT/y{ch0}_{tq}'])
                k.barrier()

    def st_final(self, hsrc, outT):
        nc, k = self.nc, self.k
        fn = self.inp('final_gain', [128, 32])
        with ExitStack() as es:
            sb = self.sbf(es)
            sb_ = self.mod_bufs(sb)
            fg = sb('fg', [128, 32])
            k.dma('sp', _dma(fg, fn), writes=['fg'])
            xo = sb_['xs'].bitcast(F32)
            for (t0, tn, isc) in TCH:
                if isc:
                    continue
                self.load_modulate(sb_, hsrc, t0, tn, fg, None, 'fin')
                k.dma('sp', _dma(outT[:, t0:t0 + tn].rearrange("(c p) t -> p c t", p=128), xo[:, :, :tn]), reads=['xs'], writes=[f'outT/{t0}'])
            k.barrier()


def lhsT_tiles(W, kc=None):
    K, M = W.shape
    assert K % 128 == 0 and M % 128 == 0
    return np.ascontiguousarray(W.reshape(K // 128, 128, M // 128, 128).transpose(2, 1, 0, 3))


def fm_vec(v):
    return np.ascontiguousarray(v.reshape(-1, 128).T)


def host_consts():
    c = np.zeros((128, 3, 128), np.float32)
    c[:, 0, :] = np.eye(128, dtype=np.float32)
    c[:, 1, :] = 1.0
    c[:, 2, :] = np.triu(np.ones((128, 128), np.float32), 1)
    return c


SW64 = np.concatenate([np.arange(32, 64), np.arange(0, 32)])
IN0_COLS = np.concatenate([np.arange(0, 1600), 1536 + SW64, np.arange(1600, 7744)])


def rope_table(rot_dim, n=NLAT, grid_w=64, theta=10000.0):
    rows = n // grid_w
    row = np.repeat(np.arange(rows), grid_w).astype(np.float32)
    col = np.tile(np.arange(grid_w), rows).astype(np.float32)
    quarter = rot_dim // 4
    inv = (1.0 / (theta ** (np.arange(quarter, dtype=np.float32) / quarter))).astype(np.float32)
    ang = np.concatenate([row[:, None] * inv, col[:, None] * inv], -1).astype(np.float32)
    cs, sn = np.cos(ang).T, np.sin(ang).T
    return np.ascontiguousarray(np.concatenate([cs, cs, -sn, sn], 0).astype(np.float32))


def uq_cols():
    cols = []
    for h in range(16):
        b = h * 192
        cols += [np.arange(b, b + 128), b + 128 + np.arange(64), b + 128 + SW64]
    return np.concatenate(cols)


def moe_inputs(z, l):
    return {
        f'routerW{l}': np.ascontiguousarray(z['router_w'][l].reshape(32, 128, 32).transpose(1, 0, 2)),
        f'routerb{l}': np.ascontiguousarray(z['router_b'][l].reshape(1, 32)),
        f'moe_wgu{l}': z['moe_w_gu'][l].reshape(NE * D, 2 * DE),
        f'moe_bgu{l}': z['moe_b_gu'][l],
        f'moe_wdn{l}': z['moe_w_down'][l].reshape(NE * DE, D),
        f'moe_bdn{l}': z['moe_b_down'][l],
        'blk128': np.ascontiguousarray(np.broadcast_to((np.arange(NB0, dtype=np.float32) * 128.0)[None, :], (128, NB0))),
        'pidx': np.arange(128, dtype=np.float32).reshape(128, 1),
    }


def hyena_consts():
    n = NLAT
    f32 = np.float32
    t = np.linspace(0.0, 1.0, n, dtype=f32)[:, None]
    ang = (f32(2.0 * math.pi / n) * np.arange(n, dtype=f32)[:, None]) * np.linspace(1e-4, 15, 16, dtype=f32)[None, :]
    zfeat = np.concatenate([t, np.cos(ang), -np.sin(ang)], -1).astype(f32)
    decay = np.abs(np.linspace(math.log(1e-2) / 1.5, math.log(1e-2) / 0.3, 2048, dtype=f32))
    tt = np.arange(n, dtype=np.float64)[:, None]
    ff = (np.arange(n, dtype=np.float64) + 0.5)[None, :]
    a = 2.0 * math.pi * tt * ff / (2 * n)
    C = np.cos(a).astype(f32); S = np.sin(a).astype(f32)
    def inv_tiles(M):
        return np.ascontiguousarray(M.T.reshape(16, 128, 8, 256).transpose(2, 1, 0, 3))
    return {
        'hy_zfeatT': np.ascontiguousarray(zfeat.T),
        'hy_tnorm': np.ascontiguousarray(np.broadcast_to(t[:, 0][None, :], (128, n))).astype(f32),
        'hy_ndecay': fm_vec(-decay),
        'dft_C': lhsT_tiles(C), 'dft_S': lhsT_tiles(S), 'dft_CT': inv_tiles(C), 'dft_nST': inv_tiles(-S),
    }


def odd_in_cols():
    q = np.arange(0, 2048); kk = np.arange(2048, 4096); v = np.arange(4096, 6144); hy = np.arange(6144, 12288)
    sw = np.concatenate([np.arange(32, 64), np.arange(0, 32)])
    def swp(base):
        return np.concatenate([base[g * 64:(g + 1) * 64][sw] for g in range(32)])
    return np.concatenate([q, swp(q), kk, swp(kk), v, hy])


def rope4(n=NLAT):
    r = rope_table(64, n)
    return np.ascontiguousarray(np.concatenate([r[0:64], r[0:64]], 0)), np.ascontiguousarray(np.concatenate([r[64:128], r[64:128]], 0))


LAM_INIT1 = 0.8 - 0.6 * math.exp(-0.3 * 1)


def odd_inputs(z):
    c4, s4 = rope4()
    d = {
        'inW1': lhsT_tiles(np.ascontiguousarray(z['odd_in_w'][0][:, odd_in_cols()])),
        'cos4': c4, 'sin4': s4,
        'dlam': np.ascontiguousarray(z['diff_lambda'][0].reshape(1, 256)),
        'subln': np.ascontiguousarray(z['diff_subln'][0].reshape(128, 1)),
        'hy_convw': np.ascontiguousarray(z['hy_conv_w'][0].reshape(3, 48, 128).transpose(2, 1, 0)),
        'hy_w1': z['hy_w1'][0], 'hy_b1': np.ascontiguousarray(z['hy_b1'][0].reshape(64, 1)),
        'hy_w2': z['hy_w2'][0], 'hy_b2': np.ascontiguousarray(z['hy_b2'][0].reshape(64, 1)),
        'hy_w3T': np.ascontiguousarray(z['hy_w3'][0].reshape(64, 64, 128).transpose(1, 0, 2)),
        'hy_skip': np.ascontiguousarray(z['hy_skip'][0].reshape(2, 16, 128).transpose(2, 0, 1)),
        'outW1': lhsT_tiles(z['odd_out_w'][0]),
    }
    d.update(hyena_consts())
    return d


def odd_mts(isc):
    return (list(range(32, 48)) + list(range(64, 80))) if isc else list(range(128))


def build_program(dbg=False):
    P = Prog(dbg=dbg)
    P.consts()
    hT0 = P.inp('hT0', [D, T])
    P.st_adaln(0)
    hlT0 = P.st_inproj(0, hT0, 61, lambda isc: list(range(61)))
    qnT, qpT, knT, vT, kpT = P.st_mla_prep(hlT0)
    catT = P.scratch('catT', [D, T])
    P.st_mla_attn(qnT, qpT, knT, vT, kpT, catT)
    P.st_sconv(hlT0, catT)
    hA = P.scratch('hA', [D, T])
    P.st_outproj(0, catT, hT0, hA)
    hB = P.scratch('hB', [D, T])
    P.st_moe(0, hA, hB, True)
    P.st_adaln(1)
    hlT1 = P.st_inproj(1, hB, 128, odd_mts)
    P.st_diff_attn(hlT1, catT, LAM_INIT1)
    P.st_hyena(hlT1, catT, 80 * 128)
    hC = P.scratch('hC', [D, T])
    P.st_outproj(1, catT, hB, hC, with_ctx=False)
    hD = P.scratch('hD', [D, T])
    P.st_moe(1, hC, hD, False)
    outT = P.scratch('outT', [D, NLAT], out=True)
    P.st_final(hD, outT)
    P.k.barrier()
    return P


def host_inputs(z, nb):
    shared = {'consts': host_consts()}
    for l in range(2):
        shared[f'adaW{l}'] = lhsT_tiles(z['ada_w'][l])
        shared[f'adab{l}'] = fm_vec(z['ada_b'][l])
        shared.update(moe_inputs(z, l))
    shared['inW0'] = lhsT_tiles(np.ascontiguousarray(z['mla_in_w'][0][:, IN0_COLS]))
    shared['uqW'] = lhsT_tiles(np.ascontiguousarray(z['mla_w_uq'][0][:, uq_cols()]))
    shared['ukvW'] = lhsT_tiles(z['mla_w_ukv'][0])
    shared['qgain'] = fm_vec(z['mla_q_norm'][0])
    shared['kvgain'] = fm_vec(z['mla_kv_norm'][0])
    shared['rope_mla'] = rope_table(64)
    shared['sconv_w'] = np.ascontiguousarray(z['sc_conv_w'][0].reshape(3, 16, 128).transpose(2, 1, 0))
    shared['outW0'] = lhsT_tiles(z['even_out_w'][0])
    shared.update(odd_inputs(z))
    shared['final_gain'] = fm_vec(z['final_norm'])
    maps = []
    for b in range(nb):
        m = dict(shared)
        m['hT0'] = np.ascontiguousarray(np.concatenate([z['x'][b].T, z['ctx'][b].T], axis=1))
        m['cT'] = np.ascontiguousarray(np.stack([fm_vec(z['c'][b]), fm_vec(z['c_ctx'])], axis=-1))
        maps.append(m)
    return maps


def kernel(**inputs):
    z = {k: np.asarray(v, dtype=np.float32) for k, v in inputs.items()}
    nb = z['x'].shape[0]
    P = build_program()
    maps = host_inputs(z, nb)
    maps = [{n: m[n] for n in P.I} for m in maps]
    res = run_bass_kernel_spmd(P.nc, maps, core_ids=list(range(nb)))
    out = np.stack([np.ascontiguousarray(r['outT'].T) for r in res.results], axis=0)
    return out.astype(np.float32)
```

```python
from contextlib import ExitStack
import math
import numpy as np
import concourse.bass as bass
import concourse.mybir as mybir
from concourse.bass_utils import run_bass_kernel_spmd

F32 = mybir.dt.float32
F32R = mybir.dt.float32r
I32 = mybir.dt.int32
ALU = mybir.AluOpType
AF = mybir.ActivationFunctionType
AX = mybir.AxisListType

RING = 8
D = 4096
KC = 32
NLAT = 2048
NCTX = 256
T = NLAT + NCTX
EPS = 1e-6
NE = 32
DE = 640
NB0 = 2 * ((T * 4) // 256 + NE)
NB1 = 2 * ((NLAT * 4) // 256 + NE)


class KB:
    def __init__(self, nc):
        self.nc = nc
        self.E = {'pe': nc.tensor, 'act': nc.scalar, 'dve': nc.vector, 'pool': nc.gpsimd, 'sp': nc.sync}
        self.sems = {}
        self.cnt = {}
        for e in self.E:
            self.sems[e] = nc.alloc_semaphore(name=f"sem_{e}")
            self.cnt[e] = 0
        self.rings = {}
        self.dcnt = {}
        for q in ('sp', 'act', 'pool'):
            self.rings[q] = [nc.alloc_semaphore(name=f"dq_{q}_{i}") for i in range(RING)]
            self.dcnt[q] = 0
        self.semobj = {}
        for e in self.E:
            self.semobj[('c', e)] = self.sems[e]
        for q in self.rings:
            for i, s in enumerate(self.rings[q]):
                self.semobj[('d', q, i)] = s
        self.waited = {e: {} for e in self.E}
        self.state = {}
        self.children = {}
        self.latest = {}
        self.ninst = 0

    def _st(self, key):
        s = self.state.get(key)
        if s is None:
            s = {'w': {}, 'r': {}}
            self.state[key] = s
            if '/' in key:
                self.children.setdefault(key.split('/')[0], set()).add(key)
        return s

    def _conf(self, key):
        if '/' in key:
            return [key, key.split('/')[0]]
        return [key] + list(self.children.get(key, ()))

    def _deps(self, reads, writes):
        deps = {}

        def add(d):
            for sid, v in d.items():
                if deps.get(sid, 0) < v:
                    deps[sid] = v
        for key in reads:
            for ck in self._conf(key):
                if ck in self.state:
                    add(self.state[ck]['w'])
        for key in writes:
            for ck in self._conf(key):
                if ck in self.state:
                    add(self.state[ck]['w'])
                    add(self.state[ck]['r'])
        return deps

    def _commit(self, ev, reads, writes):
        sid, v = ev
        for key in reads:
            s = self._st(key)
            s['r'][sid] = max(s['r'].get(sid, 0), v)
        for key in writes:
            s = self._st(key)
            s['w'] = {sid: v}
            s['r'] = {}
            if '/' not in key:
                for ck in list(self.children.get(key, ())):
                    self.state[ck] = {'w': {}, 'r': {}}

    def _wait(self, eng, deps):
        w = self.waited[eng]
        for sid, v in deps.items():
            if eng == 'pe' and sid == ('c', 'pe'):
                continue
            if w.get(sid, 0) >= v:
                continue
            self.E[eng].wait_ge(self.semobj[sid], v)
            w[sid] = v

    def op(self, eng, fn, reads=(), writes=()):
        deps = self._deps(reads, writes)
        self._wait(eng, deps)
        inst = fn(self.E[eng])
        self.cnt[eng] += 1
        inst.then_inc(self.sems[eng], 1)
        ev = (('c', eng), self.cnt[eng])
        self.latest[ev[0]] = ev[1]
        self._commit(ev, reads, writes)
        self.ninst += 1
        return inst

    def dma(self, q, fn, reads=(), writes=()):
        deps = self._deps(reads, writes)
        i = self.dcnt[q]
        slot = i % RING
        sid = ('d', q, slot)
        if i >= RING:
            deps[sid] = max(deps.get(sid, 0), 16 * (i // RING))
        self._wait(q, deps)
        inst = fn(self.E[q])
        inst.then_inc(self.rings[q][slot], 16)
        self.dcnt[q] += 1
        ev = (sid, 16 * (i // RING + 1))
        self.latest[sid] = ev[1]
        self._commit(ev, reads, writes)
        self.ninst += 1
        return inst

    def barrier(self):
        for e in self.E:
            self._wait(e, dict(self.latest))
        self.state = {}
        self.children = {}


def _dma(out, in_):
    return lambda e: e.dma_start(out=out, in_=in_)


def f32(ap):
    return ap.bitcast(F32)


TCH = [(0, 512, False), (512, 512, False), (1024, 512, False), (1536, 512, False), (2048, 256, True)]


class Prog:
    def __init__(self, dbg=False):
        self.nc = bass.Bass("TRN2", target_bir_lowering=False)
        self.k = KB(self.nc)
        self.dbg = dbg
        self.I = {}
        self.S = {}
        self.O = {}
        nc = self.nc
        self.ps = [nc.alloc_psum_tensor(f"psb{i}", [128, 512], F32).ap() for i in range(8)]
        self.gl = ExitStack()
        self.ident = self.gsb('ident', [128, 128])
        self.onesR = self.gsb('onesR', [128, 128], F32R)
        self.ones = self.gsb('ones', [128, 128])
        self.triu = self.gsb('triu', [128, 128])
        self.identR = self.gsb('identR', [128, 128], F32R)
        self.mod = [self.gsb(f'mod{l}', [128, 2, 192]) for l in range(2)]
        self.opsc = [self.gsb(f'opsc{l}', [128, 2, 2, 32]) for l in range(2)]
        self.did_const = False
        self.bregs = {}
        for nm, val in (('e', NE - 1), ('d', NE * DE - 1), ('w', NE * D - 1)):
            r = nc.gpsimd.alloc_register(f'bnd_{nm}')
            nc.gpsimd.reg_mov(r, val)
            self.bregs[nm] = r

    def sbf(self, es):
        self._stage = getattr(self, '_stage', 0) + 1
        st = self._stage
        return lambda n, s, dt=F32: es.enter_context(self.nc.sbuf_tensor(f's{st}_{n}', s, dt)).ap()

    def gsb(self, name, shape, dt=F32):
        return self.gl.enter_context(self.nc.sbuf_tensor(name, shape, dt)).ap()

    def inp(self, name, shape, dt=F32):
        ap = self.nc.dram_tensor(name, list(shape), dt, kind="ExternalInput").ap()
        self.I[name] = ap
        return ap

    def scratch(self, name, shape, dt=F32, out=False):
        kind = "ExternalOutput" if (self.dbg or out) else "Internal"
        ap = self.nc.dram_tensor(name, list(shape), dt, kind=kind).ap()
        self.S[name] = ap
        return ap

    def consts(self):
        k = self.k
        c = self.inp('consts', [128, 3, 128])
        k.dma('sp', _dma(self.ident, c[:, 0, :]), writes=['ident'])
        k.dma('sp', _dma(self.ones, c[:, 1, :]), writes=['ones'])
        k.dma('sp', _dma(self.triu, c[:, 2, :]), writes=['triu'])
        k.dma('pool', _dma(self.onesR, c[:, 1, :]), writes=['onesR'])
        k.dma('pool', _dma(self.identR, c[:, 0, :]), writes=['identR'])

    def st_adaln(self, l):
        nc, k = self.nc, self.k
        cT = self.I.get('cT') if 'cT' in self.I else self.inp('cT', [128, 32, 2])
        adaW = self.inp(f'adaW{l}', [192, 128, 32, 128])
        adab = self.inp(f'adab{l}', [128, 192])
        mod = self.mod[l]
        ps = self.ps[0]
        with ExitStack() as es:
            sb = self.sbf(es)
            c_sb = sb('a_c', [128, 32, 2])
            sc = sb('a_sc', [128, 32, 2], F32R)
            bT = sb('a_b', [128, 192])
            wb = [sb(f'a_w{i}', [128, 32, 128], F32R) for i in range(3)]
            k.dma('sp', _dma(c_sb, cT), writes=['a_c'])
            k.dma('sp', _dma(bT, adab), writes=['a_b'])
            k.op('act', lambda e: e.activation(out=sc, in_=c_sb, func=AF.Silu), reads=['a_c'], writes=['a_sc'])
            for mt in range(192):
                w = wb[mt % 3]
                wk = f'a_w{mt % 3}'
                k.dma('pool', _dma(w, adaW[mt]), writes=[wk])
                for c in range(32):
                    k.op('pe', lambda e: e.matmul(ps[:, 2 * mt:2 * mt + 2], lhsT=w[:, c, :], rhs=sc[:, c, :],
                                                  start=(c == 0), stop=(c == 31)),
                         reads=[wk, 'a_sc'], writes=['a_ps'])
            pv = ps[:, 0:384].rearrange("p (m j) -> p j m", j=2)
            for j in range(2):
                k.op('dve', lambda e: e.tensor_tensor(out=mod[:, j, :], in0=pv[:, j, :], in1=bT, op=ALU.add),
                     reads=['a_ps', 'a_b'], writes=[f'mod{l}'])
            for j in range(2):
                for wh in range(2):
                    s0 = 32 + 96 * wh
                    k.op('dve', lambda e: e.tensor_scalar(out=self.opsc[l][:, j, wh, :], in0=mod[:, j, s0:s0 + 32],
                                                          scalar1=1.0, scalar2=None, op0=ALU.add),
                         reads=[f'mod{l}'], writes=[f'opsc{l}'])
            k.barrier()

    def rstd_from_ps(self, psS, rstd, tn, n, pk, rk):
        k = self.k
        k.op('dve', lambda e: e.tensor_scalar(out=rstd[:, :tn], in0=psS[:, :tn], scalar1=1.0 / n, scalar2=EPS,
                                              op0=ALU.mult, op1=ALU.add), reads=[pk], writes=[rk])
        k.op('act', lambda e: e.activation(out=rstd[:, :tn], in_=rstd[:, :tn], func=AF.Sqrt), reads=[rk], writes=[rk])
        k.op('dve', lambda e: e.reciprocal(out=rstd[:, :tn], in_=rstd[:, :tn]), reads=[rk], writes=[rk])

    def load_modulate(self, sb_, src, t0, tn, scale_ap, shift_ap, tag):
        k = self.k
        xs = sb_['xs']
        hs = sb_['hs']
        srcv = src[:, t0:t0 + tn].rearrange("(c p) t -> c p t", p=128)
        psS = self.ps[7]
        n = [0]

        def ld(c):
            i = n[0] = n[0] + 1
            h = hs[i % 4]
            hk = f'hs{i % 4}'
            k.dma('sp', _dma(h[:, :tn], srcv[c]), writes=[hk])
            return h, hk
        for c in range(KC):
            h, hk = ld(c)
            sq = sb_['sq'][c % 2]
            sqk = f'sq{c % 2}'
            k.op('act', lambda e: e.activation(out=sq[:, :tn], in_=h[:, :tn], func=AF.Square),
                 reads=[hk], writes=[sqk])
            k.op('pe', lambda e: e.matmul(psS[:, :tn], lhsT=self.onesR, rhs=sq[:, :tn], start=(c == 0), stop=(c == KC - 1)),
                 reads=[sqk, 'onesR'], writes=['ps7'])
        rstd = sb_['rstd']
        self.rstd_from_ps(psS, rstd, tn, D, 'ps7', 'rstd')
        for c in range(KC):
            h, hk = ld(c)
            if shift_ap is not None:
                tmp = sb_['tmp'][c % 2]
                tk = f'tmp{c % 2}'
                k.op('dve', lambda e: e.scalar_tensor_tensor(out=tmp[:, :tn], in0=h[:, :tn], scalar=scale_ap[:, c:c + 1],
                                                             in1=rstd[:, :tn], op0=ALU.mult, op1=ALU.mult),
                     reads=[hk, 'rstd'], writes=[tk])
                k.op('act', lambda e: e.activation(out=xs[:, c, :tn], in_=tmp[:, :tn], func=AF.Identity,
                                                   bias=shift_ap[:, c:c + 1], scale=1.0),
                     reads=[tk], writes=[f'xs/{c}'])
            else:
                k.op('dve', lambda e: e.scalar_tensor_tensor(out=xs[:, c, :tn], in0=h[:, :tn], scalar=scale_ap[:, c:c + 1],
                                                             in1=rstd[:, :tn], op0=ALU.mult, op1=ALU.mult),
                     reads=[hk, 'rstd'], writes=[f'xs/{c}'])
        return xs

    def mod_bufs(self, sb):
        return {'xs': sb('xs', [128, 32, 512], F32R), 'hs': [sb(f'hs{i}', [128, 512]) for i in range(4)],
                'sq': [sb(f'sq{i}', [128, 512], F32R) for i in range(2)],
                'rstd': sb('rstd', [128, 512]), 'tmp': [sb(f'tmp{i}', [128, 512]) for i in range(2)]}

    def linear(self, es, xs, xkey, kc, wt, nmt, tn, epilogue, tag, mw_of=None):
        nc, k = self.nc, self.k
        if not hasattr(self, '_lw'):
            self._lw = {}
        key = (tag, kc)
        if key not in self._lw:
            self._lw[key] = [es.enter_context(nc.sbuf_tensor(f's{self._stage}_{tag}_w{i}', [128, kc, 128], F32R)).ap() for i in range(3)]
        wb = self._lw[key]
        for mt in range(nmt):
            mw = 128 if mw_of is None else mw_of(mt)
            i = self._lwc = getattr(self, '_lwc', 0) + 1
            w = wb[i % 3]
            wk = f'{tag}_w{i % 3}'
            k.dma('pool', _dma(w[:, :, :mw], wt[mt][:, :, :mw]), writes=[wk])
            pi = i % 2
            ps = self.ps[pi]
            for c in range(kc):
                k.op('pe', lambda e: e.matmul(ps[:mw, :tn], lhsT=w[:, c, :mw], rhs=xs[:, c, :tn], start=(c == 0), stop=(c == kc - 1)),
                     reads=[wk, xkey], writes=[f'ps{pi}'])
            epilogue(mt, ps, f'ps{pi}', mw)

    def st_inproj(self, l, src, nmt, chunks_mt):
        nc, k = self.nc, self.k
        inW = self.inp(f'inW{l}', [nmt, 128, 32, 128])
        hlT = self.scratch(f'hlT{l}', [nmt * 128, T])
        mod = self.mod[l]
        with ExitStack() as es:
            sb = self.sbf(es)
            sb_ = self.mod_bufs(sb)
            ost = [sb(f'ost{i}', [128, 512]) for i in range(4)]
            cnt = [0]
            self._lw = {}
            for (t0, tn, isc) in TCH:
                j = 1 if isc else 0
                xs = self.load_modulate(sb_, src, t0, tn, self.opsc[l][:, j, 0, :], mod[:, j, 0:32], 'ip')
                mts = chunks_mt(isc)

                def epi(mi, ps, pk, mw, t0=t0, tn=tn, mts=mts):
                    mt = mts[mi]
                    i = cnt[0] = cnt[0] + 1
                    o = ost[i % 4]
                    ok = f'ost{i % 4}'
                    if i % 2 == 0:
                        k.op('act', lambda e: e.activation(out=o[:, :tn], in_=ps[:, :tn], func=AF.Copy), reads=[pk], writes=[ok])
                    else:
                        k.op('dve', lambda e: e.tensor_copy(out=o[:, :tn], in_=ps[:, :tn]), reads=[pk], writes=[ok])
                    k.dma('sp', _dma(hlT[mt * 128:(mt + 1) * 128, t0:t0 + tn], o[:, :tn]), reads=[ok], writes=[f'hlT/{mt}_{t0}'])
                wts = [inW[mt] for mt in mts]
                self.linear(es, xs, 'xs', 32, wts, len(mts), tn, epi, 'ip')
            k.barrier()
        return hlT

    def norm_linear(self, es, sb, src_rows, kc, nfeat, gain, t0, tn, wt, nmt, epi, tag):
        nc, k = self.nc, self.k
        bufs = self._nl.get(tag)
        if bufs is None:
            bufs = self._nl[tag] = {'x': sb(f'{tag}_x', [128, kc, 512]), 'xr': sb(f'{tag}_xr', [128, kc, 512], F32R),
                                    'sq': [sb(f'{tag}_sq{i}', [128, 512], F32R) for i in range(2)], 'rstd': sb(f'{tag}_rstd', [128, 512])}
        x, xr, rstd = bufs['x'], bufs['xr'], bufs['rstd']
        k.dma('sp', _dma(x[:, :, :tn], src_rows[:, t0:t0 + tn].rearrange("(c p) t -> p c t", p=128)), writes=[f'{tag}_x'])
        psS = self.ps[7]
        for c in range(kc):
            sq = bufs['sq'][c % 2]
            sqk = f'{tag}_sq{c % 2}'
            k.op('act', lambda e: e.activation(out=sq[:, :tn], in_=x[:, c, :tn], func=AF.Square), reads=[f'{tag}_x'], writes=[sqk])
            k.op('pe', lambda e: e.matmul(psS[:, :tn], lhsT=self.onesR, rhs=sq[:, :tn], start=(c == 0), stop=(c == kc - 1)),
                 reads=[sqk, 'onesR'], writes=['ps7'])
        self.rstd_from_ps(psS, rstd, tn, nfeat, 'ps7', f'{tag}_rstd')
        for c in range(kc):
            k.op('dve', lambda e: e.scalar_tensor_tensor(out=xr[:, c, :tn], in0=x[:, c, :tn], scalar=gain[:, c:c + 1],
                                                         in1=rstd[:, :tn], op0=ALU.mult, op1=ALU.mult),
                 reads=[f'{tag}_x', f'{tag}_rstd'], writes=[f'{tag}_xr/{c}'])
        self.linear(es, xr, f'{tag}_xr', kc, wt, nmt, tn, epi, tag)

    def st_mla_prep(self, hlT):
        nc, k = self.nc, self.k
        uqW = self.inp('uqW', [32, 128, 8, 128])
        ukvW = self.inp('ukvW', [32, 128, 4, 128])
        qg = self.inp('qgain', [128, 8])
        kvg = self.inp('kvgain', [128, 4])
        ropeT = self.inp('rope_mla', [128, NLAT])
        qnT = self.scratch('qnT', [16, 128, T])
        qpT = self.scratch('qpT', [16, 64, T])
        knT = self.scratch('knT', [16, 128, T])
        vT = self.scratch('vT', [16, 128, T])
        kpT = self.scratch('kpT', [64, T])
        qscale = 192.0 ** -0.5
        with ExitStack() as es:
            sb = self.sbf(es)
            self._nl = {}
            self._lw = {}
            qg_sb = sb('qg', [128, 8]); kvg_sb = sb('kvg', [128, 4]); rope = sb('rope', [128, NLAT])
            k.dma('sp', _dma(qg_sb, qg), writes=['qg'])
            k.dma('sp', _dma(kvg_sb, kvg), writes=['kvg'])
            k.dma('sp', _dma(rope, ropeT), writes=['rope'])
            ost = [sb(f'ost{i}', [128, 512]) for i in range(4)]
            tt = [sb(f'tt{i}', [128, 512]) for i in range(2)]
            kpe = sb('kpe', [128, 512])
            cnt = [0]
            for (t0, tn, isc) in TCH:
                def nxt():
                    i = cnt[0] = cnt[0] + 1
                    return ost[i % 4], f'ost{i % 4}', i

                def rope_out(src_ap, srck, scale, t0=t0, tn=tn, isc=isc):
                    o, ok, i = nxt()
                    if isc:
                        k.op('act', lambda e: e.activation(out=o[0:64, :tn], in_=src_ap[0:64, :tn], func=AF.Copy, scale=scale),
                             reads=[srck], writes=[ok])
                    else:
                        t = tt[i % 2]
                        tk = f'tt{i % 2}'
                        k.op('dve', lambda e: e.scalar_tensor_tensor(out=t[:, :tn], in0=src_ap[:, :tn], scalar=scale, in1=rope[:, t0:t0 + tn],
                                                                     op0=ALU.mult, op1=ALU.mult), reads=[srck, 'rope'], writes=[tk])
                        k.op('act', lambda e: e.activation(out=o[64:128, :tn], in_=t[0:64, :tn], func=AF.Copy), reads=[tk], writes=[ok])
                        k.op('dve', lambda e: e.tensor_tensor(out=o[0:64, :tn], in0=o[64:128, :tn], in1=t[64:128, :tn], op=ALU.add),
                             reads=[tk, ok], writes=[ok])
                    return o, ok

                def epi_q(mt, ps, pk, mw, t0=t0, tn=tn):
                    h = mt // 2
                    if mt % 2 == 0:
                        o, ok, i = nxt()
                        k.op('act', lambda e: e.activation(out=o[:, :tn], in_=ps[:, :tn], func=AF.Copy, scale=qscale), reads=[pk], writes=[ok])
                        k.dma('sp', _dma(qnT[h, :, t0:t0 + tn], o[:, :tn]), reads=[ok], writes=[f'qnT/{h}_{t0}'])
                    else:
                        o, ok = rope_out(ps, pk, qscale)
                        k.dma('sp', _dma(qpT[h, :, t0:t0 + tn], o[0:64, :tn]), reads=[ok], writes=[f'qpT/{h}_{t0}'])
                self.norm_linear(es, sb, hlT[0:1024], 8, 1024, qg_sb, t0, tn, [uqW[i] for i in range(32)], 32, epi_q, 'uq')

                def epi_kv(mt, ps, pk, mw, t0=t0, tn=tn):
                    h = mt // 2
                    o, ok, i = nxt()
                    if i % 2 == 0:
                        k.op('act', lambda e: e.activation(out=o[:, :tn], in_=ps[:, :tn], func=AF.Copy), reads=[pk], writes=[ok])
                    else:
                        k.op('dve', lambda e: e.tensor_copy(out=o[:, :tn], in_=ps[:, :tn]), reads=[pk], writes=[ok])
                    dst = knT if mt % 2 == 0 else vT
                    k.dma('sp', _dma(dst[h, :, t0:t0 + tn], o[:, :tn]), reads=[ok], writes=[f'kv{mt % 2}/{h}_{t0}'])
                self.norm_linear(es, sb, hlT[1024:1536], 4, 512, kvg_sb, t0, tn, [ukvW[i] for i in range(32)], 32, epi_kv, 'ukv')
                k.dma('sp', _dma(kpe[:, :tn], hlT[1536:1664, t0:t0 + tn]), writes=['kpe'])
                o, ok = rope_out(kpe, 'kpe', 1.0)
                k.dma('sp', _dma(kpT[:, t0:t0 + tn], o[0:64, :tn]), reads=[ok], writes=[f'kpT/{t0}'])
            k.barrier()
        return qnT, qpT, knT, vT, kpT

    def st_mla_attn(self, qnT, qpT, knT, vT, kpT, catT, with_ctx_q=True):
        nc, k = self.nc, self.k
        with ExitStack() as es:
            sb = self.sbf(es)
            kp = sb('at_kp', [64, T], F32R)
            k.dma('pool', _dma(kp, kpT), writes=['at_kp'])
            kn = [sb(f'at_kn{i}', [128, T], F32R) for i in range(2)]
            qn = [sb(f'at_qn{i}', [128, T], F32R) for i in range(2)]
            qp = [sb(f'at_qp{i}', [64, T], F32R) for i in range(2)]
            vt = [sb(f'at_vt{i}', [128, T]) for i in range(2)]
            vm = [sb(f'at_vm{i}', [128, 18, 128], F32R) for i in range(2)]
            pT = [sb(f'at_p{i}', [128, 512], F32R) for i in range(3)]
            rden = [sb(f'at_rd{i}', [128, 512]) for i in range(2)]
            ob = [sb(f'at_o{i}', [128, 512]) for i in range(2)]
            pc = 0
            qcn = 0
            for h in range(16):
                b = h % 2
                k.dma('pool', _dma(kn[b], knT[h]), writes=[f'at_kn{b}'])
                k.dma('pool', _dma(qn[b], qnT[h]), writes=[f'at_qn{b}'])
                k.dma('pool', _dma(qp[b], qpT[h]), writes=[f'at_qp{b}'])
                k.dma('sp', _dma(vt[b], vT[h]), writes=[f'at_vt{b}'])
                for g in range(5):
                    n = min(4, 18 - 4 * g)
                    pst = self.ps[6]
                    for j in range(n):
                        kt = 4 * g + j
                        k.op('pe', lambda e: e.transpose(out=pst[:, j * 128:(j + 1) * 128], in_=vt[b][:, kt * 128:(kt + 1) * 128], identity=self.ident),
                             reads=[f'at_vt{b}', 'ident'], writes=['ps6'])
                    k.op('act', lambda e: e.activation(out=vm[b][:, 4 * g:4 * g + n, :], in_=pst[:, :n * 128].rearrange("p (j d) -> p j d", d=128), func=AF.Copy),
                         reads=['ps6'], writes=[f'at_vm{b}'])
                qcs = [(t0, tn, list(range(18))) for (t0, tn, isc) in TCH if not isc]
                if with_ctx_q:
                    qcs.append((NLAT, NCTX, [16, 17]))
                for (t0, tn, kts) in qcs:
                    qcn += 1
                    pn = self.ps[2 + qcn % 2]
                    pd = self.ps[4 + qcn % 2]
                    pnk, pdk = f'ps{2 + qcn % 2}', f'ps{4 + qcn % 2}'
                    def emit_s(kt, t0=t0, tn=tn, b=b):
                        nonlocal pc
                        pc += 1
                        pss = self.ps[pc % 2]
                        psk = f'ps{pc % 2}'
                        ks = slice(kt * 128, (kt + 1) * 128)
                        k.op('pe', lambda e: e.matmul(pss[:, :tn], lhsT=kn[b][:, ks], rhs=qn[b][:, t0:t0 + tn], start=True, stop=False),
                             reads=[f'at_kn{b}', f'at_qn{b}'], writes=[psk])
                        k.op('pe', lambda e: e.matmul(pss[:, :tn], lhsT=kp[:, ks], rhs=qp[b][:, t0:t0 + tn], start=False, stop=True),
                             reads=['at_kp', f'at_qp{b}'], writes=[psk])
                        return pss, psk, pc
                    cur = emit_s(kts[0])
                    for ii, kt in enumerate(kts):
                        nxt_ = emit_s(kts[ii + 1]) if ii + 1 < len(kts) else None
                        pss, psk, pci = cur
                        p = pT[pci % 3]
                        ppk = f'at_p{pci % 3}'
                        k.op('act', lambda e: e.activation(out=p[:, :tn], in_=pss[:, :tn], func=AF.Exp), reads=[psk], writes=[ppk])
                        k.op('pe', lambda e: e.matmul(pn[:, :tn], lhsT=vm[b][:, kt, :], rhs=p[:, :tn], start=(ii == 0), stop=(ii == len(kts) - 1)),
                             reads=[f'at_vm{b}', ppk], writes=[pnk])
                        k.op('pe', lambda e: e.matmul(pd[:, :tn], lhsT=self.onesR, rhs=p[:, :tn], start=(ii == 0), stop=(ii == len(kts) - 1)),
                             reads=['onesR', ppk], writes=[pdk])
                        cur = nxt_
                    rd = rden[qcn % 2]
                    o = ob[qcn % 2]
                    k.op('dve', lambda e: e.reciprocal(out=rd[:, :tn], in_=pd[:, :tn]), reads=[pdk], writes=[f'at_rd{qcn % 2}'])
                    k.op('dve', lambda e: e.tensor_tensor(out=o[:, :tn], in0=pn[:, :tn], in1=rd[:, :tn], op=ALU.mult),
                         reads=[pnk, f'at_rd{qcn % 2}'], writes=[f'at_o{qcn % 2}'])
                    k.dma('sp', _dma(catT[h * 128:(h + 1) * 128, t0:t0 + tn], o[:, :tn]), reads=[f'at_o{qcn % 2}'], writes=[f'catT/{h}_{t0}'])
            k.barrier()

    def st_sconv(self, hlT, catT, row0=1664):
        nc, k = self.nc, self.k
        cw = self.inp('sconv_w', [128, 16, 3])
        with ExitStack() as es:
            sb = self.sbf(es)
            cw_sb = sb('cw', [128, 16, 3])
            k.dma('sp', _dma(cw_sb, cw), writes=['cw'])
            bufs = [[sb(f'sc_{nm}{i}', [128, T]) for nm in ('gb', 'gc', 'hh', 'y')] for i in range(2)]
            segs = [(0, NLAT), (NLAT, T)]
            for c in range(16):
                i = c % 2
                gb, gc, hh, y = bufs[i]
                kk = [f'sc_{nm}{i}' for nm in ('gb', 'gc', 'hh', 'y')]
                for j, buf in enumerate((gb, gc, hh)):
                    r0 = row0 + j * 2048 + c * 128
                    k.dma('sp', _dma(buf, hlT[r0:r0 + 128, :]), writes=[kk[j]])
                k.op('dve', lambda e: e.tensor_tensor(out=gc, in0=gc, in1=hh, op=ALU.mult), reads=[kk[1], kk[2]], writes=[kk[1]])
                k.op('act', lambda e: e.activation(out=y, in_=gc, func=AF.Copy, scale=cw_sb[:, c, 1:2]), reads=[kk[1], 'cw'], writes=[kk[3]])
                for (a, bnd) in segs:
                    k.op('dve', lambda e: e.scalar_tensor_tensor(out=y[:, a + 1:bnd], in0=gc[:, a:bnd - 1], scalar=cw_sb[:, c, 0:1], in1=y[:, a + 1:bnd],
                                                                 op0=ALU.mult, op1=ALU.add), reads=[kk[1], kk[3], 'cw'], writes=[kk[3]])
                    k.op('dve', lambda e: e.scalar_tensor_tensor(out=y[:, a:bnd - 1], in0=gc[:, a + 1:bnd], scalar=cw_sb[:, c, 2:3], in1=y[:, a:bnd - 1],
                                                                 op0=ALU.mult, op1=ALU.add), reads=[kk[1], kk[3], 'cw'], writes=[kk[3]])
                k.op('pool', lambda e: e.tensor_tensor(out=y, in0=y, in1=gb, op=ALU.mult), reads=[kk[0], kk[3]], writes=[kk[3]])
                k.dma('sp', _dma(catT[2048 + c * 128:2048 + (c + 1) * 128, :], y), reads=[kk[3]], writes=[f'catT/s{c}'])
            k.barrier()

    def st_outproj(self, l, catT, hsrc, hdst, with_ctx=True):
        nc, k = self.nc, self.k
        outW = self.inp(f'outW{l}', [32, 128, 32, 128])
        mod = self.mod[l]
        with ExitStack() as es:
            sb = self.sbf(es)
            self._lw = {}
            xs = sb('op_xs', [128, 32, 512], F32R)
            ht = [sb(f'op_h{i}', [128, 512]) for i in range(3)]
            cnt = [0]
            for (t0, tn, isc) in TCH:
                if isc and not with_ctx:
                    continue
                j = 1 if isc else 0
                k.dma('pool', _dma(xs[:, :, :tn], catT[:, t0:t0 + tn].rearrange("(c p) t -> p c t", p=128)), writes=['op_xs'])

                def epi(mt, ps, pk, mw, t0=t0, tn=tn, j=j):
                    i = cnt[0] = cnt[0] + 1
                    h = ht[i % 3]
                    hk = f'op_h{i % 3}'
                    k.dma('sp', _dma(h[:, :tn], hsrc[mt * 128:(mt + 1) * 128, t0:t0 + tn]), writes=[hk])
                    k.op('dve', lambda e: e.scalar_tensor_tensor(out=h[:, :tn], in0=ps[:, :tn], scalar=mod[:, j, 64 + mt:65 + mt], in1=h[:, :tn],
                                                                 op0=ALU.mult, op1=ALU.add), reads=[pk, hk, f'mod{l}'], writes=[hk])
                    k.dma('sp', _dma(hdst[mt * 128:(mt + 1) * 128, t0:t0 + tn], h[:, :tn]), reads=[hk], writes=[f'hdst/{mt}_{t0}'])
                self.linear(es, xs, 'op_xs', 32, [outW[i] for i in range(32)], 32, tn, epi, 'op')
            k.barrier()

    def st_moe(self, l, hsrc, hdst, with_ctx):
        nc, k = self.nc, self.k
        mod = self.mod[l]
        chunks = [c for c in TCH if (with_ctx or not c[2])]
        ntok = sum(c[1] for c in chunks)
        NT = ntok // 128
        NP = (ntok * 4) // 256 + NE
        NB = 2 * NP
        rw = self.inp(f'routerW{l}', [128, 32, 32])
        rb = self.inp(f'routerb{l}', [1, 32])
        wgu = self.inp(f'moe_wgu{l}', [NE * D, 2 * DE])
        bgu = self.inp(f'moe_bgu{l}', [NE, 2 * DE])
        wdn = self.inp(f'moe_wdn{l}', [NE * DE, D])
        bdn = self.inp(f'moe_bdn{l}', [NE, D])
        pidx = self.I['pidx'] if 'pidx' in self.I else self.inp('pidx', [128, 1])
        blk128 = self.I['blk128'] if 'blk128' in self.I else self.inp('blk128', [128, NB0])
        HD = D // 2
        if 'u2' not in self.S:
            self.scratch('u2', [T, D])
            for i in range(2):
                self.scratch(f'xslots_{i}', [NB0 * 128, HD])
                self.scratch(f'yslots_{i}', [NB0 * 128, HD])
        u2 = self.S['u2']
        xsl = [self.S[f'xslots_{i}'] for i in range(2)]
        ysl = [self.S[f'yslots_{i}'] for i in range(2)]
        with ExitStack() as esg:
            gsb = self.sbf(esg)
            lg_all = gsb('lg_all', [128, NT, 32]); top8 = gsb('top8', [128, NT, 8]); rank_all = gsb('rank_all', [128, NT, 32])
            gate_all = gsb('gate_all', [128, NT, 4]); dest_f = gsb('dest_f', [128, NT * 4]); dest_i = gsb('dest_i', [128, NT * 4], I32)
            base = gsb('base', [128, 32]); pstart = gsb('pstart', [128, 32]); blk_i = gsb('blk_i', [128, NB], I32)
            widx = gsb('widx', [128, NB], I32); didx = gsb('didx', [128, NB], I32)
            with ExitStack() as es:
                sb = self.sbf(es)
                sb_ = self.mod_bufs(sb)
                rw_sb = sb('rw', [128, 32, 32], F32R); rb_sb = sb('rb', [1, 32], F32R)
                k.dma('pool', _dma(rw_sb, rw), writes=['rw'])
                k.dma('pool', _dma(rb_sb, rb), writes=['rb'])
                k.op('dve', lambda e: e.memset(base, 0.0), writes=['base'])
                mask = [sb(f'mask{i}', [128, 32]) for i in range(2)]
                sm = [sb(f'sm{i}', [128, 8]) for i in range(2)]
                u2tm = [sb(f'u2tm{i}', [128, D]) for i in range(2)]
                ti = 0
                trn = 0
                for (t0, tn, isc) in chunks:
                    j = 1 if isc else 0
                    xs = self.load_modulate(sb_, hsrc, t0, tn, self.opsc[l][:, j, 1, :], mod[:, j, 96:128], 'mo')
                    for tt in range(tn // 128):
                        tsl = slice(tt * 128, (tt + 1) * 128)
                        pr = self.ps[3 + ti % 2]
                        prk = f'ps{3 + ti % 2}'
                        for c in range(KC):
                            k.op('pe', lambda e: e.matmul(pr[:, 64:96], lhsT=xs[:, c, tsl], rhs=rw_sb[:, c, :], start=(c == 0), stop=False),
                                 reads=['xs', 'rw'], writes=[prk])
                        k.op('pe', lambda e: e.matmul(pr[:, 64:96], lhsT=self.onesR[0:1, :], rhs=rb_sb, start=False, stop=True),
                             reads=['onesR', 'rb'], writes=[prk])
                        lg = lg_all[:, ti, :]
                        k.op('act', lambda e: e.activation(out=lg, in_=pr[:, 64:96], func=AF.Copy), reads=[prk], writes=['lg_all'])
                        k.op('dve', lambda e: e.max(out=top8[:, ti, :], in_=lg), reads=['lg_all'], writes=['top8'])
                        m = mask[ti % 2]
                        mk = f'mask{ti % 2}'
                        k.op('dve', lambda e: e.tensor_scalar(out=m, in0=lg, scalar1=top8[:, ti, 3:4], scalar2=None, op0=ALU.is_ge),
                             reads=['lg_all', 'top8'], writes=[mk])
                        s_ = sm[ti % 2]
                        sk = f'sm{ti % 2}'
                        k.op('dve', lambda e: e.tensor_scalar(out=s_[:, 0:1], in0=top8[:, ti, 0:1], scalar1=-1.0, scalar2=None, op0=ALU.mult),
                             reads=['top8'], writes=[sk])
                        k.op('act', lambda e: e.activation(out=s_[:, 4:8], in_=top8[:, ti, 0:4], func=AF.Exp, bias=s_[:, 0:1], scale=1.0),
                             reads=['top8', sk], writes=[sk])
                        k.op('dve', lambda e: e.tensor_reduce(out=s_[:, 1:2], in_=s_[:, 4:8], axis=AX.X, op=ALU.add), reads=[sk], writes=[sk])
                        k.op('dve', lambda e: e.reciprocal(out=s_[:, 2:3], in_=s_[:, 1:2]), reads=[sk], writes=[sk])
                        k.op('dve', lambda e: e.tensor_scalar(out=gate_all[:, ti, :], in0=s_[:, 4:8], scalar1=s_[:, 2:3], scalar2=None, op0=ALU.mult),
                             reads=[sk], writes=['gate_all'])
                        k.op('pe', lambda e: e.matmul(pr[:, 0:32], lhsT=self.triu, rhs=m, start=True, stop=True), reads=['triu', mk], writes=[prk])
                        k.op('pe', lambda e: e.matmul(pr[:, 32:64], lhsT=self.ones, rhs=m, start=True, stop=True), reads=['ones', mk], writes=[prk])
                        k.op('dve', lambda e: e.tensor_tensor(out=rank_all[:, ti, :], in0=pr[:, 0:32], in1=base, op=ALU.add),
                             reads=[prk, 'base'], writes=['rank_all'])
                        k.op('dve', lambda e: e.tensor_tensor(out=base, in0=pr[:, 32:64], in1=base, op=ALU.add), reads=[prk, 'base'], writes=['base'])
                        ut = u2tm[ti % 2]
                        uk = f'u2tm{ti % 2}'
                        for g in range(8):
                            trn += 1
                            pt = self.ps[5 + trn % 2]
                            ptk = f'ps{5 + trn % 2}'
                            for jj in range(4):
                                c = 4 * g + jj
                                k.op('pe', lambda e: e.transpose(out=pt[:, jj * 128:(jj + 1) * 128].bitcast(F32R), in_=xs[:, c, tsl], identity=self.identR),
                                     reads=['xs', 'identR'], writes=[ptk])
                            if g % 2 == 0:
                                k.op('act', lambda e: e.activation(out=ut[:, g * 512:(g + 1) * 512], in_=pt, func=AF.Copy), reads=[ptk], writes=[uk])
                            else:
                                k.op('dve', lambda e: e.tensor_copy(out=ut[:, g * 512:(g + 1) * 512], in_=pt), reads=[ptk], writes=[uk])
                        k.dma('sp', _dma(u2[ti * 128:(ti + 1) * 128, :], ut), reads=[uk], writes=[f'u2/{ti}'])
                        ti += 1
                k.barrier()
            with ExitStack() as es:
                sb = self.sbf(es)
                t1 = sb('t1', [128, 32]); t2 = sb('t2', [128, 32]); padded = sb('padded', [128, 32])
                cs = [sb(f'cs{i}', [128, 32]) for i in range(2)]
                b128 = sb('b128', [128, NB]); acc = sb('acc', [128, NB])
                k.dma('sp', _dma(b128, blk128[:, :NB]), writes=['b128'])
                k.op('dve', lambda e: e.tensor_scalar(out=t1, in0=base, scalar1=255.0, scalar2=None, op0=ALU.add), reads=['base'], writes=['t1'])
                ti1 = sb('ti1', [128, 32], I32); ti2 = sb('ti2', [128, 32], I32)
                k.op('dve', lambda e: e.tensor_copy(out=ti1, in_=t1), reads=['t1'], writes=['ti1'])
                k.op('dve', lambda e: e.tensor_scalar(out=ti2, in0=ti1, scalar1=8, scalar2=8, op0=ALU.arith_shift_right, op1=ALU.logical_shift_left),
                     reads=['ti1'], writes=['ti2'])
                k.op('dve', lambda e: e.tensor_copy(out=padded, in_=ti2), reads=['ti2'], writes=['padded'])
                k.op('dve', lambda e: e.tensor_copy(out=cs[0], in_=padded), reads=['padded'], writes=['cs0'])
                cur = 0
                for sh in (1, 2, 4, 8, 16):
                    a, b_ = cs[cur], cs[1 - cur]
                    k.op('dve', lambda e: e.tensor_copy(out=b_[:, 0:sh], in_=a[:, 0:sh]), reads=[f'cs{cur}'], writes=[f'cs{1 - cur}'])
                    k.op('dve', lambda e: e.tensor_tensor(out=b_[:, sh:32], in0=a[:, sh:32], in1=a[:, 0:32 - sh], op=ALU.add),
                         reads=[f'cs{cur}'], writes=[f'cs{1 - cur}'])
                    cur = 1 - cur
                pend = cs[cur]
                pek = f'cs{cur}'
                k.op('dve', lambda e: e.tensor_tensor(out=pstart, in0=pend, in1=padded, op=ALU.subtract), reads=[pek, 'padded'], writes=['pstart'])
                k.op('dve', lambda e: e.memset(acc, 0.0), writes=['acc'])
                for ex in range(NE):
                    k.op('dve', lambda e: e.scalar_tensor_tensor(out=acc, in0=b128, scalar=pend[:, ex:ex + 1], in1=acc, op0=ALU.is_ge, op1=ALU.add),
                         reads=['b128', pek, 'acc'], writes=['acc'])
                k.op('dve', lambda e: e.tensor_scalar(out=acc, in0=acc, scalar1=float(NE - 1), scalar2=None, op0=ALU.min), reads=['acc'], writes=['acc'])
                unus = sb('unus', [128, NB])
                BIG = 4194304.0
                k.op('dve', lambda e: e.tensor_scalar(out=unus, in0=b128, scalar1=pend[:, NE - 1:NE], scalar2=BIG, op0=ALU.is_ge, op1=ALU.mult),
                     reads=['b128', pek], writes=['unus'])
                eix = sb('eix', [128, NB])
                k.op('dve', lambda e: e.tensor_tensor(out=eix, in0=acc, in1=unus, op=ALU.add), reads=['acc', 'unus'], writes=['eix'])
                k.op('dve', lambda e: e.tensor_copy(out=blk_i, in_=eix), reads=['eix'], writes=['blk_i'])
                pix = sb('pix', [128, 1]); acc2 = sb('acc2', [128, NB])
                k.dma('sp', _dma(pix, pidx), writes=['pix'])
                k.op('dve', lambda e: e.tensor_scalar(out=acc2, in0=acc, scalar1=float(D), scalar2=pix[:, 0:1], op0=ALU.mult, op1=ALU.add), reads=['acc', 'pix'], writes=['acc2'])
                k.op('dve', lambda e: e.tensor_tensor(out=acc2, in0=acc2, in1=unus, op=ALU.add), reads=['acc2', 'unus'], writes=['acc2'])
                k.op('dve', lambda e: e.tensor_copy(out=widx, in_=acc2), reads=['acc2'], writes=['widx'])
                k.op('dve', lambda e: e.tensor_scalar(out=acc2, in0=acc, scalar1=float(DE), scalar2=pix[:, 0:1], op0=ALU.mult, op1=ALU.add), reads=['acc', 'pix'], writes=['acc2'])
                k.op('dve', lambda e: e.tensor_tensor(out=acc2, in0=acc2, in1=unus, op=ALU.add), reads=['acc2', 'unus'], writes=['acc2'])
                k.op('dve', lambda e: e.tensor_copy(out=didx, in_=acc2), reads=['acc2'], writes=['didx'])
                dall = sb('dall', [128, 32]); prod = [sb(f'prod{i}', [128, 32]) for i in range(2)]
                for ti in range(NT):
                    k.op('dve', lambda e: e.tensor_tensor(out=dall, in0=rank_all[:, ti, :], in1=pstart, op=ALU.add),
                         reads=['rank_all', 'pstart'], writes=['dall'])
                    for k4 in range(4):
                        p_ = prod[k4 % 2]
                        k.op('dve', lambda e: e.scalar_tensor_tensor(out=p_, in0=lg_all[:, ti, :], scalar=top8[:, ti, k4:k4 + 1], in1=dall,
                                                                     op0=ALU.is_equal, op1=ALU.mult), reads=['lg_all', 'top8', 'dall'], writes=[f'prod{k4 % 2}'])
                        k.op('dve', lambda e: e.tensor_reduce(out=dest_f[:, ti * 4 + k4:ti * 4 + k4 + 1], in_=p_, axis=AX.X, op=ALU.add),
                             reads=[f'prod{k4 % 2}'], writes=['dest_f'])
                k.op('dve', lambda e: e.tensor_scalar(out=dest_f, in0=dest_f, scalar1=0.0, scalar2=float(NB * 128 - 1), op0=ALU.max, op1=ALU.min), reads=['dest_f'], writes=['dest_f'])
                k.op('dve', lambda e: e.tensor_copy(out=dest_i, in_=dest_f), reads=['dest_f'], writes=['dest_i'])
                if self.dbg:
                    dd = self.scratch(f'dbg_dest{l}', [128, NT * 4], I32)
                    k.dma('sp', _dma(dd, dest_i), reads=['dest_i'], writes=['dbgd'])
                    db = self.scratch(f'dbg_blk{l}', [128, NB], I32)
                    k.dma('sp', _dma(db, blk_i), reads=['blk_i'], writes=['dbgb'])
                    dg = self.scratch(f'dbg_gate{l}', [128, NT, 4])
                    k.dma('sp', _dma(dg, gate_all), reads=['gate_all'], writes=['dbgg'])
                xrow = [sb(f'xrow{i}', [128, D]) for i in range(2)]
                for ti in range(NT):
                    xr = xrow[ti % 2]
                    xk = f'xrow{ti % 2}'
                    k.dma('sp', _dma(xr, u2[ti * 128:(ti + 1) * 128, :]), writes=[xk])
                    for k4 in range(4):
                        col = ti * 4 + k4
                        for hf in range(2):
                            k.dma('pool', lambda e: e.indirect_dma_start(out=xsl[hf], out_offset=bass.IndirectOffsetOnAxis(ap=dest_i[:, col:col + 1], axis=0),
                                                                         in_=xr[:, hf * HD:(hf + 1) * HD], in_offset=None), reads=[xk, 'dest_i'], writes=[f'xsl{hf}/{col}'])
                k.barrier()
            with ExitStack() as es:
                sb = self.sbf(es)
                xb = sb('xb', [128, D])
                XT = [sb(f'XT{i}', [128, 32, 128], F32R) for i in range(2)]
                wg = [sb(f'wg{i}', [128, 2 * DE], F32R) for i in range(4)]
                bg = sb('bg', [128, 2 * DE])
                gu = sb('gu', [128, 2 * DE]); gm = sb('gm', [128, DE]); sg = sb('sg', [128, DE]); upc = sb('upc', [128, DE]); aa = sb('aa', [128, DE])
                aT = sb('aT', [128, 5, 128], F32R)
                wd = sb('wd', [128, 5, D], F32R)
                bd = sb('bd', [128, D])
                yb = [sb(f'yb{i}', [128, 512]) for i in range(3)]
                wcnt = 0
                ycnt = 0
                trn = 0

                def gather(out, src, idx_ap, eoff, keys_w, bound):
                    k.dma('pool', lambda e: e.indirect_dma_start(out=out, out_offset=None, in_=src,
                                                                 in_offset=bass.IndirectOffsetOnAxis(ap=idx_ap, axis=0), element_offset=eoff,
                                                                 bounds_check=bound, oob_is_err=False),
                          reads=['widx', 'didx', 'blk_i'], writes=keys_w)
                nts = [(0, 512), (512, 512), (1024, 256)]
                for pr in range(NP):
                    b0 = 2 * pr
                    for s_ in range(2):
                        blk = b0 + s_
                        for hf in range(2):
                            k.dma('sp', _dma(xb[:, hf * HD:(hf + 1) * HD], xsl[hf][blk * 128:(blk + 1) * 128, :]), writes=[f'xb/{hf}'])
                        for g in range(8):
                            trn += 1
                            pt = self.ps[6 + trn % 2]
                            ptk = f'ps{6 + trn % 2}'
                            for jj in range(4):
                                c = 4 * g + jj
                                k.op('pe', lambda e: e.transpose(out=pt[:, jj * 128:(jj + 1) * 128], in_=xb[:, c * 128:(c + 1) * 128], identity=self.ident),
                                     reads=['xb', 'ident'], writes=[ptk])
                            pv = pt.rearrange("p (j s) -> p j s", s=128)
                            if g % 2 == 0:
                                k.op('act', lambda e: e.activation(out=XT[s_][:, 4 * g:4 * g + 4, :], in_=pv, func=AF.Copy), reads=[ptk], writes=[f'XT{s_}/{g}'])
                            else:
                                k.op('dve', lambda e: e.tensor_copy(out=XT[s_][:, 4 * g:4 * g + 4, :], in_=pv), reads=[ptk], writes=[f'XT{s_}/{g}'])
                    gather(bg, bgu, blk_i[:, b0:b0 + 1], 0, ['bg'], self.bregs['e'])
                    gather(bd, bdn, blk_i[:, b0:b0 + 1], 0, ['bd'], self.bregs['e'])
                    for c in range(5):
                        gather(wd[:, c, :], wdn, didx[:, b0:b0 + 1], c * 128 * D, [f'wd/{c}'], self.bregs['d'])
                    for c in range(KC):
                        wcnt += 1
                        w_ = wg[wcnt % 4]
                        wk = f'wg{wcnt % 4}'
                        gather(w_, wgu, widx[:, b0:b0 + 1], c * 128 * 2 * DE, [wk], self.bregs['w'])
                        for s_ in range(2):
                            for nt, (n0, nw) in enumerate(nts):
                                pi = 3 * s_ + nt
                                k.op('pe', lambda e: e.matmul(self.ps[pi][:, :nw], lhsT=XT[s_][:, c, :], rhs=w_[:, n0:n0 + nw], start=(c == 0), stop=(c == KC - 1)),
                                     reads=[f'XT{s_}', wk], writes=[f'ps{pi}'])
                    for s_ in range(2):
                        blk = b0 + s_
                        for nt, (n0, nw) in enumerate(nts):
                            pi = 3 * s_ + nt
                            k.op('dve', lambda e: e.tensor_tensor(out=gu[:, n0:n0 + nw], in0=self.ps[pi][:, :nw], in1=bg[:, n0:n0 + nw], op=ALU.add),
                                 reads=[f'ps{pi}', 'bg'], writes=['gu'])
                        k.op('dve', lambda e: e.tensor_scalar(out=gm, in0=gu[:, 0:DE], scalar1=7.0, scalar2=None, op0=ALU.min), reads=['gu'], writes=['gm'])
                        k.op('act', lambda e: e.activation(out=sg, in_=gm, func=AF.Sigmoid, scale=1.702), reads=['gm'], writes=['sg'])
                        k.op('dve', lambda e: e.tensor_scalar(out=upc, in0=gu[:, DE:2 * DE], scalar1=-7.0, scalar2=7.0, op0=ALU.max, op1=ALU.min), reads=['gu'], writes=['upc'])
                        k.op('dve', lambda e: e.scalar_tensor_tensor(out=upc, in0=upc, scalar=1.0, in1=gm, op0=ALU.add, op1=ALU.mult), reads=['upc', 'gm'], writes=['upc'])
                        k.op('pool', lambda e: e.tensor_tensor(out=aa, in0=upc, in1=sg, op=ALU.mult), reads=['upc', 'sg'], writes=['aa'])
                        pa, pb = self.ps[6], self.ps[7]
                        for c in range(5):
                            dst = pa[:, c * 128:(c + 1) * 128] if c < 4 else pb[:, 0:128]
                            k.op('pe', lambda e: e.transpose(out=dst, in_=aa[:, c * 128:(c + 1) * 128], identity=self.ident),
                                 reads=['aa', 'ident'], writes=['ps6' if c < 4 else 'ps7'])
                        k.op('act', lambda e: e.activation(out=aT[:, 0:4, :], in_=pa.rearrange("p (j s) -> p j s", s=128), func=AF.Copy), reads=['ps6'], writes=['aT/0'])
                        k.op('dve', lambda e: e.tensor_copy(out=aT[:, 4, :], in_=pb[:, 0:128]), reads=['ps7'], writes=['aT/1'])
                        for nt in range(8):
                            pyi = 6 + nt % 2
                            py = self.ps[pyi]
                            pyk = f'ps{pyi}'
                            for c in range(5):
                                k.op('pe', lambda e: e.matmul(py, lhsT=aT[:, c, :], rhs=wd[:, c, nt * 512:(nt + 1) * 512], start=(c == 0), stop=(c == 4)),
                                     reads=['aT', 'wd'], writes=[pyk])
                            ycnt += 1
                            y_ = yb[ycnt % 3]
                            yk = f'yb{ycnt % 3}'
                            k.op('dve', lambda e: e.tensor_tensor(out=y_, in0=py, in1=bd[:, nt * 512:(nt + 1) * 512], op=ALU.add), reads=[pyk, 'bd'], writes=[yk])
                            k.dma('sp', _dma(ysl[nt // 4][blk * 128:(blk + 1) * 128, (nt % 4) * 512:(nt % 4 + 1) * 512], y_), reads=[yk], writes=[f'ysl/{blk}_{nt}'])
                k.barrier()
            with ExitStack() as es:
                sb = self.sbf(es)
                yr = [sb(f'yr{i}', [128, D]) for i in range(2)]
                accb = [sb(f'accb{i}', [128, D]) for i in range(2)]
                hb = [sb(f'hb{i}', [128, 32, 128]) for i in range(2)]
                yc = 0
                trn = 0
                t_of = []
                for (t0, tn, isc) in chunks:
                    for tt in range(tn // 128):
                        t_of.append((t0 + tt * 128, 1 if isc else 0))
                for ti in range(NT):
                    tok0, j = t_of[ti]
                    a_ = accb[ti % 2]
                    ak = f'accb{ti % 2}'
                    for k4 in range(4):
                        yc += 1
                        y_ = yr[yc % 2]
                        yk = f'yr{yc % 2}'
                        col = ti * 4 + k4
                        for hf in range(2):
                            k.dma('pool', lambda e: e.indirect_dma_start(out=y_[:, hf * HD:(hf + 1) * HD], out_offset=None, in_=ysl[hf],
                                                                         in_offset=bass.IndirectOffsetOnAxis(ap=dest_i[:, col:col + 1], axis=0)),
                                  reads=['dest_i'], writes=[f'{yk}/{hf}'])
                        if k4 == 0:
                            k.op('dve', lambda e: e.tensor_scalar(out=a_, in0=y_, scalar1=gate_all[:, ti, 0:1], scalar2=None, op0=ALU.mult),
                                 reads=[yk, 'gate_all'], writes=[ak])
                        else:
                            eng = 'dve'
                            k.op(eng, lambda e: e.scalar_tensor_tensor(out=a_, in0=y_, scalar=gate_all[:, ti, k4:k4 + 1], in1=a_, op0=ALU.mult, op1=ALU.add),
                                 reads=[yk, 'gate_all', ak], writes=[ak])
                    h_ = hb[ti % 2]
                    hk = f'hb{ti % 2}'
                    k.dma('sp', _dma(h_, hsrc[:, tok0:tok0 + 128].rearrange("(c p) t -> p c t", p=128)), writes=[hk])
                    for g in range(8):
                        trn += 1
                        pt = self.ps[trn % 2]
                        ptk = f'ps{trn % 2}'
                        for jj in range(4):
                            c = 4 * g + jj
                            k.op('pe', lambda e: e.transpose(out=pt[:, jj * 128:(jj + 1) * 128], in_=a_[:, c * 128:(c + 1) * 128], identity=self.ident),
                                 reads=[ak, 'ident'], writes=[ptk])
                        for jj in range(4):
                            c = 4 * g + jj
                            k.op('dve', lambda e: e.scalar_tensor_tensor(out=h_[:, c, :], in0=pt[:, jj * 128:(jj + 1) * 128], scalar=mod[:, j, 160 + c:161 + c],
                                                                         in1=h_[:, c, :], op0=ALU.mult, op1=ALU.add), reads=[ptk, hk, f'mod{l}'], writes=[hk])
                    k.dma('sp', _dma(hdst[:, tok0:tok0 + 128].rearrange("(c p) t -> p c t", p=128), h_), reads=[hk], writes=[f'hdst/{ti}'])
                k.barrier()

    def st_diff_attn(self, hlT, catT, lam_init):
        nc, k = self.nc, self.k
        cos4 = self.inp('cos4', [128, NLAT]); sin4 = self.inp('sin4', [128, NLAT])
        dlam = self.inp('dlam', [1, 256]); subln = self.inp('subln', [128, 1])
        sc = 64.0 ** -0.5
        with ExitStack() as es:
            sb = self.sbf(es)
            c4 = sb('c4', [128, NLAT]); s4 = sb('s4', [128, NLAT])
            k.dma('sp', _dma(c4, cos4), writes=['c4']); k.dma('sp', _dma(s4, sin4), writes=['s4'])
            dl = sb('dl', [1, 256]); sl = sb('sl', [128, 1]); g2 = sb('g2', [128, 1]); nlam = sb('nlam', [128, 1])
            lt = sb('lt', [1, 128]); ls = sb('ls', [1, 8])
            k.dma('sp', _dma(dl, dlam), writes=['dl']); k.dma('sp', _dma(sl, subln), writes=['sl'])
            k.op('dve', lambda e: e.tensor_scalar(out=g2, in0=sl, scalar1=1.0 - lam_init, scalar2=None, op0=ALU.mult), reads=['sl'], writes=['g2'])
            k.op('dve', lambda e: e.tensor_tensor(out=lt[:, 0:64], in0=dl[:, 0:64], in1=dl[:, 64:128], op=ALU.mult), reads=['dl'], writes=['lt'])
            k.op('dve', lambda e: e.tensor_tensor(out=lt[:, 64:128], in0=dl[:, 128:192], in1=dl[:, 192:256], op=ALU.mult), reads=['dl'], writes=['lt'])
            k.op('dve', lambda e: e.tensor_reduce(out=ls[:, 0:1], in_=lt[:, 0:64], axis=AX.X, op=ALU.add), reads=['lt'], writes=['ls'])
            k.op('dve', lambda e: e.tensor_reduce(out=ls[:, 1:2], in_=lt[:, 64:128], axis=AX.X, op=ALU.add), reads=['lt'], writes=['ls'])
            k.op('act', lambda e: e.activation(out=ls[:, 2:4], in_=ls[:, 0:2], func=AF.Exp), reads=['ls'], writes=['ls'])
            k.op('dve', lambda e: e.tensor_tensor(out=ls[:, 4:5], in0=ls[:, 3:4], in1=ls[:, 2:3], op=ALU.subtract), reads=['ls'], writes=['ls'])
            k.op('dve', lambda e: e.tensor_scalar(out=ls[:, 5:6], in0=ls[:, 4:5], scalar1=-lam_init, scalar2=None, op0=ALU.add), reads=['ls'], writes=['ls'])
            k.op('pe', lambda e: e.matmul(self.ps[7][:, 0:1], lhsT=self.ones[0:1, :], rhs=ls[:, 5:6], start=True, stop=True), reads=['ones', 'ls'], writes=['ps7'])
            k.op('dve', lambda e: e.tensor_copy(out=nlam, in_=self.ps[7][:, 0:1]), reads=['ps7'], writes=['nlam'])
            raw = {n: sb(f'raw_{n}', [128, T]) for n in ('q', 'qs', 'k', 'ks', 'v')}
            t1 = sb('t1', [128, NLAT]); t2 = sb('t2', [128, NLAT])
            qr = [sb(f'qr{i}', [128, NLAT], F32R) for i in range(2)]
            kr = [sb(f'kr{i}', [128, T], F32R) for i in range(2)]
            vm = [sb(f'vm{i}', [128, 18, 128], F32R) for i in range(2)]
            pT = [sb(f'p{i}', [128, 512], F32R) for i in range(3)]
            rd = sb('rd', [128, 512]); o0 = sb('o0', [128, 512]); o1 = sb('o1', [128, 512]); sq = sb('sq', [128, 512], F32R)
            rstd = sb('rstd', [128, 512]); oo = [sb(f'oo{i}', [128, 512]) for i in range(2)]
            pc = 0
            qcn = 0
            for h in range(16):
                b = h % 2
                for n, mt, w in (('q', h, NLAT), ('qs', 16 + h, NLAT), ('k', 32 + h, T), ('ks', 48 + h, NLAT), ('v', 64 + h, T)):
                    k.dma('sp', _dma(raw[n][:, :w], hlT[mt * 128:(mt + 1) * 128, 0:w]), writes=[f'raw_{n}'])
                for (x, xsw, dst, dk) in ((raw['q'], raw['qs'], qr[b], f'qr{b}'), (raw['k'], raw['ks'], kr[b], f'kr{b}')):
                    xk = 'raw_q' if x is raw['q'] else 'raw_k'
                    xsk = 'raw_qs' if x is raw['q'] else 'raw_ks'
                    k.op('dve', lambda e: e.tensor_tensor(out=t1, in0=x[:, :NLAT], in1=c4, op=ALU.mult), reads=[xk, 'c4'], writes=['t1'])
                    k.op('pool', lambda e: e.tensor_tensor(out=t2, in0=xsw[:, :NLAT], in1=s4, op=ALU.mult), reads=[xsk, 's4'], writes=['t2'])
                    k.op('dve', lambda e: e.tensor_tensor(out=dst[:, :NLAT], in0=t1, in1=t2, op=ALU.add), reads=['t1', 't2'], writes=[dk])
                k.op('act', lambda e: e.activation(out=kr[b][:, NLAT:T], in_=raw['k'][:, NLAT:T], func=AF.Copy), reads=['raw_k'], writes=[f'kr{b}'])
                for g in range(5):
                    n = min(4, 18 - 4 * g)
                    pst = self.ps[6]
                    for j in range(n):
                        kt = 4 * g + j
                        k.op('pe', lambda e: e.transpose(out=pst[:, j * 128:(j + 1) * 128], in_=raw['v'][:, kt * 128:(kt + 1) * 128], identity=self.ident),
                             reads=['raw_v', 'ident'], writes=['ps6'])
                    k.op('act', lambda e: e.activation(out=vm[b][:, 4 * g:4 * g + n, :], in_=pst[:, :n * 128].rearrange("p (j d) -> p j d", d=128), func=AF.Copy),
                         reads=['ps6'], writes=[f'vm{b}'])
                for (t0, tn, isc) in TCH:
                    if isc:
                        continue
                    qcn += 1
                    for c in range(2):
                        pn = self.ps[2 + c]; pd = self.ps[4 + c]
                        rs = slice(64 * c, 64 * c + 64)
                        def emit_s(kt, t0=t0, tn=tn, b=b, rs=rs):
                            nonlocal pc
                            pc += 1
                            pss = self.ps[pc % 2]; psk = f'ps{pc % 2}'
                            ks = slice(kt * 128, (kt + 1) * 128)
                            k.op('pe', lambda e: e.matmul(pss[:, :tn], lhsT=kr[b][rs, ks], rhs=qr[b][rs, t0:t0 + tn], start=True, stop=True),
                                 reads=[f'kr{b}', f'qr{b}'], writes=[psk])
                            return pss, psk, pc
                        cur = emit_s(0)
                        for kt in range(18):
                            nxt_ = emit_s(kt + 1) if kt + 1 < 18 else None
                            pss, psk, pci = cur
                            p = pT[pci % 3]; ppk = f'p{pci % 3}'
                            k.op('act', lambda e: e.activation(out=p[:, :tn], in_=pss[:, :tn], func=AF.Exp, scale=sc), reads=[psk], writes=[ppk])
                            k.op('pe', lambda e: e.matmul(pn[:, :tn], lhsT=vm[b][:, kt, :], rhs=p[:, :tn], start=(kt == 0), stop=(kt == 17)),
                                 reads=[f'vm{b}', ppk], writes=[f'ps{2 + c}'])
                            k.op('pe', lambda e: e.matmul(pd[:, :tn], lhsT=self.onesR, rhs=p[:, :tn], start=(kt == 0), stop=(kt == 17)),
                                 reads=['onesR', ppk], writes=[f'ps{4 + c}'])
                            cur = nxt_
                        oc = o0 if c == 0 else o1
                        k.op('dve', lambda e: e.reciprocal(out=rd[:, :tn], in_=pd[:, :tn]), reads=[f'ps{4 + c}'], writes=['rd'])
                        k.op('dve', lambda e: e.tensor_tensor(out=oc[:, :tn], in0=pn[:, :tn], in1=rd[:, :tn], op=ALU.mult), reads=[f'ps{2 + c}', 'rd'], writes=[f'o{c}'])
                    k.op('dve', lambda e: e.scalar_tensor_tensor(out=o0[:, :tn], in0=o1[:, :tn], scalar=nlam[:, 0:1], in1=o0[:, :tn], op0=ALU.mult, op1=ALU.add),
                         reads=['o0', 'o1', 'nlam'], writes=['o0'])
                    k.op('act', lambda e: e.activation(out=sq[:, :tn], in_=o0[:, :tn], func=AF.Square), reads=['o0'], writes=['sq'])
                    k.op('pe', lambda e: e.matmul(self.ps[7][:, :tn], lhsT=self.onesR, rhs=sq[:, :tn], start=True, stop=True), reads=['onesR', 'sq'], writes=['ps7'])
                    self.rstd_from_ps(self.ps[7], rstd, tn, 128, 'ps7', 'rstd')
                    ob = oo[qcn % 2]
                    k.op('dve', lambda e: e.scalar_tensor_tensor(out=ob[:, :tn], in0=o0[:, :tn], scalar=g2[:, 0:1], in1=rstd[:, :tn], op0=ALU.mult, op1=ALU.mult),
                         reads=['o0', 'g2', 'rstd'], writes=[f'oo{qcn % 2}'])
                    k.dma('sp', _dma(catT[h * 128:(h + 1) * 128, t0:t0 + tn], ob[:, :tn]), reads=[f'oo{qcn % 2}'], writes=[f'catT/{h}_{t0}'])
            k.barrier()

    def st_hyena(self, hlT, catT, row0):
        nc, k = self.nc, self.k
        N2 = 2 * NLAT
        hcw = self.inp('hy_convw', [128, 48, 3])
        zf = self.inp('hy_zfeatT', [33, NLAT]); w1 = self.inp('hy_w1', [33, 64]); b1 = self.inp('hy_b1', [64, 1])
        w2 = self.inp('hy_w2', [64, 64]); b2 = self.inp('hy_b2', [64, 1]); w3 = self.inp('hy_w3T', [64, 64, 128])
        tnb = self.inp('hy_tnorm', [128, NLAT]); ndec = self.inp('hy_ndecay', [128, 16]); skip = self.inp('hy_skip', [128, 2, 16])
        Ct = self.inp('dft_C', [16, 128, 16, 128]); St = self.inp('dft_S', [16, 128, 16, 128])
        CTt = self.inp('dft_CT', [8, 128, 16, 256]); nSTt = self.inp('dft_nST', [8, 128, 16, 256])
        hyc = self.scratch('hyc', [6144, NLAT])
        edn = self.scratch('hy_edn', [2, 2, 2048, NLAT])
        hz = self.scratch('hy_z1', [2048, NLAT])
        with ExitStack() as es:
            sb = self.sbf(es)
            cw = sb('cw', [128, 48, 3])
            k.dma('sp', _dma(cw, hcw), writes=['cw'])
            pb = [sb(f'p{i}', [128, NLAT]) for i in range(2)]; yb = [sb(f'y{i}', [128, NLAT]) for i in range(2)]
            for c in range(48):
                i = c % 2
                p, y = pb[i], yb[i]
                pk, yk = f'p{i}', f'y{i}'
                k.dma('sp', _dma(p, hlT[row0 + c * 128:row0 + (c + 1) * 128, 0:NLAT]), writes=[pk])
                k.op('act', lambda e: e.activation(out=y, in_=p, func=AF.Copy, scale=cw[:, c, 1:2]), reads=[pk, 'cw'], writes=[yk])
                k.op('dve', lambda e: e.scalar_tensor_tensor(out=y[:, 1:NLAT], in0=p[:, 0:NLAT - 1], scalar=cw[:, c, 0:1], in1=y[:, 1:NLAT], op0=ALU.mult, op1=ALU.add),
                     reads=[pk, yk, 'cw'], writes=[yk])
                k.op('dve', lambda e: e.scalar_tensor_tensor(out=y[:, 0:NLAT - 1], in0=p[:, 1:NLAT], scalar=cw[:, c, 2:3], in1=y[:, 0:NLAT - 1], op0=ALU.mult, op1=ALU.add),
                     reads=[pk, yk, 'cw'], writes=[yk])
                k.dma('sp', _dma(hyc[c * 128:(c + 1) * 128, :], y), reads=[yk], writes=[f'hyc/{c}'])
            k.barrier()
        with ExitStack() as es:
            sb = self.sbf(es)
            zfr = sb('zfr', [33, NLAT], F32R); w1r = sb('w1r', [33, 64], F32R); w2r = sb('w2r', [64, 64], F32R)
            b1s = sb('b1s', [64, 1]); b2s = sb('b2s', [64, 1])
            w3r = [sb(f'w3r{i}', [64, 128], F32R) for i in range(4)]
            tn_sb = sb('tn_sb', [128, NLAT]); nd = sb('nd', [128, 16])
            for dst, src, q, kk in ((zfr, zf, 'pool', 'zfr'), (w1r, w1, 'pool', 'w1r'), (w2r, w2, 'pool', 'w2r'), (b1s, b1, 'sp', 'b1s'), (b2s, b2, 'sp', 'b2s'),
                                    (tn_sb, tnb, 'sp', 'tn_sb'), (nd, ndec, 'sp', 'nd')):
                k.dma(q, _dma(dst, src), writes=[kk])
            hid = [sb(f'hid{i}', [64, NLAT], F32R) for i in range(2)]
            ya = sb('ya', [64, 512]); yq = sb('yq', [64, 512]); yi = sb('yi', [64, 512], I32); ym = sb('ym', [64, 512])
            TWO_PI = 2.0 * math.pi

            def sin_layer(wr, wk, src, srck, bs, bk, dst, dstk):
                for q4 in range(4):
                    ts = slice(q4 * 512, (q4 + 1) * 512)
                    ps = self.ps[q4 % 2]; pk = f'ps{q4 % 2}'
                    k.op('pe', lambda e: e.matmul(ps[0:64, :], lhsT=wr, rhs=src[:, ts], start=True, stop=True), reads=[wk, srck], writes=[pk])
                    k.op('dve', lambda e: e.tensor_scalar(out=ya, in0=ps[0:64, :], scalar1=bs[:, 0:1], scalar2=None, op0=ALU.add), reads=[pk, bk], writes=['ya'])
                    k.op('dve', lambda e: e.tensor_scalar(out=yq, in0=ya, scalar1=1.0 / TWO_PI, scalar2=None, op0=ALU.mult), reads=['ya'], writes=['yq'])
                    k.op('dve', lambda e: e.tensor_copy(out=yi, in_=yq), reads=['yq'], writes=['yi'])
                    k.op('dve', lambda e: e.tensor_copy(out=yq, in_=yi), reads=['yi'], writes=['yq'])
                    k.op('dve', lambda e: e.scalar_tensor_tensor(out=ya, in0=yq, scalar=-TWO_PI, in1=ya, op0=ALU.mult, op1=ALU.add), reads=['yq', 'ya'], writes=['ya'])
                    k.op('dve', lambda e: e.tensor_scalar(out=ym, in0=ya, scalar1=math.pi, scalar2=None, op0=ALU.is_gt), reads=['ya'], writes=['ym'])
                    k.op('dve', lambda e: e.scalar_tensor_tensor(out=ya, in0=ym, scalar=-TWO_PI, in1=ya, op0=ALU.mult, op1=ALU.add), reads=['ym', 'ya'], writes=['ya'])
                    k.op('dve', lambda e: e.tensor_scalar(out=ym, in0=ya, scalar1=-math.pi, scalar2=None, op0=ALU.is_lt), reads=['ya'], writes=['ym'])
                    k.op('dve', lambda e: e.scalar_tensor_tensor(out=ya, in0=ym, scalar=TWO_PI, in1=ya, op0=ALU.mult, op1=ALU.add), reads=['ym', 'ya'], writes=['ya'])
                    k.op('dve', lambda e: e.tensor_scalar(out=ya, in0=ya, scalar1=-3.1415925, scalar2=3.1415925, op0=ALU.max, op1=ALU.min), reads=['ya'], writes=['ya'])
                    k.op('act', lambda e: e.activation(out=dst[:, ts], in_=ya, func=AF.Sin), reads=['ya'], writes=[dstk])
            sin_layer(w1r, 'w1r', zfr, 'zfr', b1s, 'b1s', hid[0], 'hid0')
            sin_layer(w2r, 'w2r', hid[0], 'hid0', b2s, 'b2s', hid[1], 'hid1')
            h2 = hid[1]
            win = sb('win', [128, NLAT]); fw = sb('fw', [128, NLAT]); bw = sb('bw', [128, NLAT]); ab = sb('ab', [128, NLAT])
            ee = [sb(f'ee{i}', [128, NLAT]) for i in range(2)]; dd = [sb(f'dd{i}', [128, NLAT]) for i in range(2)]
            l1 = sb('l1', [128, 4])
            wc = 0
            it = 0
            for cc in range(16):
                k.op('act', lambda e: e.activation(out=win, in_=tn_sb, func=AF.Exp, scale=nd[:, cc:cc + 1]), reads=['tn_sb', 'nd'], writes=['win'])
                k.op('dve', lambda e: e.tensor_scalar(out=win, in0=win, scalar1=0.05, scalar2=None, op0=ALU.add), reads=['win'], writes=['win'])
                for o in range(2):
                    it += 1
                    for di, dst, dk in ((0, fw, 'fw'), (1, bw, 'bw')):
                        mt = o * 32 + di * 16 + cc
                        wc += 1
                        wr = w3r[wc % 4]; wk = f'w3r{wc % 4}'
                        k.dma('pool', _dma(wr, w3[mt]), writes=[wk])
                        for q4 in range(4):
                            ts = slice(q4 * 512, (q4 + 1) * 512)
                            ps = self.ps[2 + q4 % 2]; pk = f'ps{2 + q4 % 2}'
                            k.op('pe', lambda e: e.matmul(ps, lhsT=wr, rhs=h2[:, ts], start=True, stop=True), reads=[wk, 'hid1'], writes=[pk])
                            k.op('dve', lambda e: e.tensor_tensor(out=dst[:, ts], in0=ps, in1=win[:, ts], op=ALU.mult), reads=[pk, 'win'], writes=[dk])
                    e_, d_ = ee[it % 2], dd[it % 2]
                    ek, dk = f'ee{it % 2}', f'dd{it % 2}'
                    k.op('dve', lambda e: e.tensor_tensor(out=e_, in0=fw, in1=bw, op=ALU.add), reads=['fw', 'bw'], writes=[ek])
                    k.op('pool', lambda e: e.tensor_tensor(out=d_, in0=bw, in1=fw, op=ALU.subtract), reads=['fw', 'bw'], writes=[dk])
                    k.op('act', lambda e: e.activation(out=ab[:, 1:NLAT], in_=fw[:, 1:NLAT], func=AF.Abs, accum_out=l1[:, 0:1]), reads=['fw'], writes=['ab', 'l1'])
                    k.op('act', lambda e: e.activation(out=ab[:, 1:NLAT], in_=bw[:, 1:NLAT], func=AF.Abs, accum_out=l1[:, 1:2]), reads=['bw', 'ab'], writes=['ab', 'l1'])
                    k.op('act', lambda e: e.activation(out=l1[:, 2:3], in_=e_[:, 0:1], func=AF.Abs), reads=[ek, 'l1'], writes=['l1'])
                    k.op('dve', lambda e: e.tensor_tensor(out=l1[:, 0:1], in0=l1[:, 0:1], in1=l1[:, 1:2], op=ALU.add), reads=['l1'], writes=['l1'])
                    k.op('dve', lambda e: e.tensor_tensor(out=l1[:, 0:1], in0=l1[:, 0:1], in1=l1[:, 2:3], op=ALU.add), reads=['l1'], writes=['l1'])
                    k.op('dve', lambda e: e.reciprocal(out=l1[:, 3:4], in_=l1[:, 0:1]), reads=['l1'], writes=['l1'])
                    k.op('dve', lambda e: e.tensor_scalar(out=e_, in0=e_, scalar1=l1[:, 3:4], scalar2=None, op0=ALU.mult), reads=[ek, 'l1'], writes=[ek])
                    k.op('dve', lambda e: e.tensor_scalar(out=d_, in0=d_, scalar1=l1[:, 3:4], scalar2=None, op0=ALU.mult), reads=[dk, 'l1'], writes=[dk])
                    k.dma('sp', _dma(edn[o, 0, cc * 128:(cc + 1) * 128, :], e_), reads=[ek], writes=[f'edn/{o}_0_{cc}'])
                    k.dma('sp', _dma(edn[o, 1, cc * 128:(cc + 1) * 128, :], d_), reads=[dk], writes=[f'edn/{o}_1_{cc}'])
            k.barrier()
        for o in range(2):
            zsrc = hyc if o == 0 else hz
            gate0 = 2048 * (o + 1)
            with ExitStack() as es:
                sb = self.sbf(es)
                sk = sb('sk', [128, 2, 16])
                k.dma('sp', _dma(sk, skip), writes=['sk'])
                zf_ = [sb(f'zf{i}', [128, NLAT]) for i in range(2)]
                ld = [sb(f'ld{i}', [128, NLAT]) for i in range(2)]
                ztm = sb('ztm', [128, 16, 256], F32R); etm = sb('etm', [128, 16, 256], F32R); dtm = sb('dtm', [128, 16, 256], F32R)
                Yre = sb('Yre', [128, 16, 256], F32R); Yim = sb('Yim', [128, 16, 256], F32R)
                Cb = [sb(f'Cb{i}', [128, 16, 128], F32R) for i in range(2)]; Sb = [sb(f'Sb{i}', [128, 16, 128], F32R) for i in range(2)]
                CTb = [sb(f'CTb{i}', [128, 16, 256], F32R) for i in range(1)]; STb = [sb(f'STb{i}', [128, 16, 256], F32R) for i in range(1)]
                hre = sb('hre', [128, 256]); him = sb('him', [128, 256]); ta = sb('ta', [128, 256]); tb = sb('tb', [128, 256])
                u1 = [sb(f'u1{i}', [128, 256]) for i in range(2)]; gt = [sb(f'gt{i}', [128, 256]) for i in range(2)]
                trn = 0
                fcn = 0
                tqn = 0
                for cq in range(8):
                    for half in range(2):
                        ch0 = cq * 256 + half * 128
                        k.dma('sp', _dma(zf_[half], zsrc[ch0:ch0 + 128, :]), writes=[f'zf{half}'])
                        for (src_ap, srck, dst_tm, dstk) in ((zf_[half], f'zf{half}', ztm, 'ztm'), (None, 'e', etm, 'etm'), (None, 'd', dtm, 'dtm')):
                            if src_ap is None:
                                li = 0 if srck == 'e' else 1
                                src_ap = ld[li]
                                k.dma('sp', _dma(src_ap, edn[o, li, ch0:ch0 + 128, :]), writes=[f'ld{li}'])
                                srck = f'ld{li}'
                            for g in range(4):
                                trn += 1
                                pt = self.ps[6 + trn % 2]; ptk = f'ps{6 + trn % 2}'
                                for jj in range(4):
                                    tc = 4 * g + jj
                                    k.op('pe', lambda e: e.transpose(out=pt[:, jj * 128:(jj + 1) * 128], in_=src_ap[:, tc * 128:(tc + 1) * 128], identity=self.ident),
                                         reads=[srck, 'ident'], writes=[ptk])
                                k.op('act' if trn % 2 else 'dve',
                                     (lambda e: e.activation(out=dst_tm[:, 4 * g:4 * g + 4, half * 128:(half + 1) * 128], in_=pt.rearrange("p (j s) -> p j s", s=128), func=AF.Copy)) if trn % 2 else
                                     (lambda e: e.tensor_copy(out=dst_tm[:, 4 * g:4 * g + 4, half * 128:(half + 1) * 128], in_=pt.rearrange("p (j s) -> p j s", s=128))),
                                     reads=[ptk], writes=[f'{dstk}/{half}_{g}'])
                    for ft in range(16):
                        fcn += 1
                        C_, S_ = Cb[fcn % 2], Sb[fcn % 2]
                        ck, skk = f'Cb{fcn % 2}', f'Sb{fcn % 2}'
                        k.dma('pool', _dma(C_, Ct[ft]), writes=[ck])
                        k.dma('pool', _dma(S_, St[ft]), writes=[skk])
                        z0, z1 = (0, 1) if ft % 2 == 0 else (4, 5)
                        for (pi, W_, wk_, X_, xk_) in ((z0, C_, ck, ztm, 'ztm'), (z1, S_, skk, ztm, 'ztm'), (2, C_, ck, etm, 'etm'), (3, S_, skk, dtm, 'dtm')):
                            for tc in range(16):
                                k.op('pe', lambda e: e.matmul(self.ps[pi][:, :256], lhsT=W_[:, tc, :], rhs=X_[:, tc, :], start=(tc == 0), stop=(tc == 15)),
                                     reads=[wk_, xk_], writes=[f'ps{pi}'])
                        zre, zs = self.ps[z0][:, :256], self.ps[z1][:, :256]
                        zrk, zsk = f'ps{z0}', f'ps{z1}'
                        k.op('act', lambda e: e.activation(out=hre, in_=self.ps[2][:, :256], func=AF.Copy), reads=['ps2'], writes=['hre'])
                        k.op('act', lambda e: e.activation(out=him, in_=self.ps[3][:, :256], func=AF.Copy), reads=['ps3'], writes=['him'])
                        k.op('dve', lambda e: e.tensor_tensor(out=ta, in0=zre, in1=hre, op=ALU.mult), reads=[zrk, 'hre'], writes=['ta'])
                        k.op('dve', lambda e: e.tensor_tensor(out=tb, in0=zs, in1=him, op=ALU.mult), reads=[zsk, 'him'], writes=['tb'])
                        k.op('pool', lambda e: e.tensor_tensor(out=Yre[:, ft, :], in0=ta, in1=tb, op=ALU.add), reads=['ta', 'tb'], writes=[f'Yre/{ft}'])
                        k.op('dve', lambda e: e.tensor_tensor(out=ta, in0=zre, in1=him, op=ALU.mult), reads=[zrk, 'him', f'Yre/{ft}'], writes=['ta'])
                        k.op('dve', lambda e: e.tensor_tensor(out=tb, in0=zs, in1=hre, op=ALU.mult), reads=[zsk, 'hre', f'Yre/{ft}'], writes=['tb'])
                        k.op('pool', lambda e: e.tensor_tensor(out=Yim[:, ft, :], in0=ta, in1=tb, op=ALU.subtract), reads=['ta', 'tb'], writes=[f'Yim/{ft}'])
                    for tq in range(8):
                        tqn += 1
                        CT_, ST_ = (CTb[0], STb[0]) if tqn % 2 else (etm, dtm)
                        ctk, stk = ('CTb0', 'STb0') if tqn % 2 else ('etm', 'dtm')
                        k.dma('pool', _dma(CT_, CTt[tq]), writes=[ctk])
                        k.dma('pool', _dma(ST_, nSTt[tq]), writes=[stk])
                        tsl = slice(tq * 256, (tq + 1) * 256)
                        for half in range(2):
                            ch0 = cq * 256 + half * 128
                            cchunk = ch0 // 128
                            py = self.ps[4 + half]; pyk = f'ps{4 + half}'
                            cs_ = slice(half * 128, (half + 1) * 128)
                            for fc in range(16):
                                k.op('pe', lambda e: e.matmul(py[:, :256], lhsT=Yre[:, fc, cs_], rhs=CT_[:, fc, :], start=(fc == 0), stop=False),
                                     reads=['Yre', ctk], writes=[pyk])
                            for fc in range(16):
                                k.op('pe', lambda e: e.matmul(py[:, :256], lhsT=Yim[:, fc, cs_], rhs=ST_[:, fc, :], start=False, stop=(fc == 15)),
                                     reads=['Yim', stk], writes=[pyk])
                            u_ = u1[half]; g_ = gt[half]
                            k.dma('sp', _dma(g_, hyc[gate0 + ch0:gate0 + ch0 + 128, tsl]), writes=[f'gt{half}'])
                            k.op('act', lambda e: e.activation(out=u_, in_=zf_[half][:, tsl], func=AF.Copy, scale=sk[:, o, cchunk:cchunk + 1]),
                                 reads=[f'zf{half}', 'sk'], writes=[f'u1{half}'])
                            k.op('dve', lambda e: e.scalar_tensor_tensor(out=u_, in0=py[:, :256], scalar=2.0 / N2, in1=u_, op0=ALU.mult, op1=ALU.add),
                                 reads=[pyk, f'u1{half}'], writes=[f'u1{half}'])
                            k.op('pool', lambda e: e.tensor_tensor(out=u_, in0=u_, in1=g_, op=ALU.mult), reads=[f'u1{half}', f'gt{half}'], writes=[f'u1{half}'])
                            if o == 0:
                                k.dma('sp', _dma(hz[ch0:ch0 + 128, tsl], u_), reads=[f'u1{half}'], writes=[f'hz/{ch0}_{tq}'])
                            else:
                                k.dma('sp', _dma(catT[2048 + ch0:2048 + ch0 + 128, tsl], u_), reads=[f'u1{half}'], writes=[f'catT/y{ch0}_{tq}'])
                k.barrier()

    def st_final(self, hsrc, outT):
        nc, k = self.nc, self.k
        fn = self.inp('final_gain', [128, 32])
        with ExitStack() as es:
            sb = self.sbf(es)
            sb_ = self.mod_bufs(sb)
            fg = sb('fg', [128, 32])
            k.dma('sp', _dma(fg, fn), writes=['fg'])
            xo = sb_['xs'].bitcast(F32)
            for (t0, tn, isc) in TCH:
                if isc:
                    continue
                self.load_modulate(sb_, hsrc, t0, tn, fg, None, 'fin')
                k.dma('sp', _dma(outT[:, t0:t0 + tn].rearrange("(c p) t -> p c t", p=128), xo[:, :, :tn]), reads=['xs'], writes=[f'outT/{t0}'])
            k.barrier()


def lhsT_tiles(W, kc=None):
    K, M = W.shape
    assert K % 128 == 0 and M % 128 == 0
    return np.ascontiguousarray(W.reshape(K // 128, 128, M // 128, 128).transpose(2, 1, 0, 3))


def fm_vec(v):
    return np.ascontiguousarray(v.reshape(-1, 128).T)


def host_consts():
    c = np.zeros((128, 3, 128), np.float32)
    c[:, 0, :] = np.eye(128, dtype=np.float32)
    c[:, 1, :] = 1.0
    c[:, 2, :] = np.triu(np.ones((128, 128), np.float32), 1)
    return c


SW64 = np.concatenate([np.arange(32, 64), np.arange(0, 32)])
IN0_COLS = np.concatenate([np.arange(0, 1600), 1536 + SW64, np.arange(1600, 7744)])


def rope_table(rot_dim, n=NLAT, grid_w=64, theta=10000.0):
    rows = n // grid_w
    row = np.repeat(np.arange(rows), grid_w).astype(np.float32)
    col = np.tile(np.arange(grid_w), rows).astype(np.float32)
    quarter = rot_dim // 4
    inv = (1.0 / (theta ** (np.arange(quarter, dtype=np.float32) / quarter))).astype(np.float32)
    ang = np.concatenate([row[:, None] * inv, col[:, None] * inv], -1).astype(np.float32)
    cs, sn = np.cos(ang).T, np.sin(ang).T
    return np.ascontiguousarray(np.concatenate([cs, cs, -sn, sn], 0).astype(np.float32))


def uq_cols():
    cols = []
    for h in range(16):
        b = h * 192
        cols += [np.arange(b, b + 128), b + 128 + np.arange(64), b + 128 + SW64]
    return np.concatenate(cols)


def moe_inputs(z, l):
    return {
        f'routerW{l}': np.ascontiguousarray(z['router_w'][l].reshape(32, 128, 32).transpose(1, 0, 2)),
        f'routerb{l}': np.ascontiguousarray(z['router_b'][l].reshape(1, 32)),
        f'moe_wgu{l}': z['moe_w_gu'][l].reshape(NE * D, 2 * DE),
        f'moe_bgu{l}': z['moe_b_gu'][l],
        f'moe_wdn{l}': z['moe_w_down'][l].reshape(NE * DE, D),
        f'moe_bdn{l}': z['moe_b_down'][l],
        'blk128': np.ascontiguousarray(np.broadcast_to((np.arange(NB0, dtype=np.float32) * 128.0)[None, :], (128, NB0))),
        'pidx': np.arange(128, dtype=np.float32).reshape(128, 1),
    }


def hyena_consts():
    n = NLAT
    f32 = np.float32
    t = np.linspace(0.0, 1.0, n, dtype=f32)[:, None]
    ang = (f32(2.0 * math.pi / n) * np.arange(n, dtype=f32)[:, None]) * np.linspace(1e-4, 15, 16, dtype=f32)[None, :]
    zfeat = np.concatenate([t, np.cos(ang), -np.sin(ang)], -1).astype(f32)
    decay = np.abs(np.linspace(math.log(1e-2) / 1.5, math.log(1e-2) / 0.3, 2048, dtype=f32))
    tt = np.arange(n, dtype=np.float64)[:, None]
    ff = (np.arange(n, dtype=np.float64) + 0.5)[None, :]
    a = 2.0 * math.pi * tt * ff / (2 * n)
    C = np.cos(a).astype(f32); S = np.sin(a).astype(f32)
    def inv_tiles(M):
        return np.ascontiguousarray(M.T.reshape(16, 128, 8, 256).transpose(2, 1, 0, 3))
    return {
        'hy_zfeatT': np.ascontiguousarray(zfeat.T),
        'hy_tnorm': np.ascontiguousarray(np.broadcast_to(t[:, 0][None, :], (128, n))).astype(f32),
        'hy_ndecay': fm_vec(-decay),
        'dft_C': lhsT_tiles(C), 'dft_S': lhsT_tiles(S), 'dft_CT': inv_tiles(C), 'dft_nST': inv_tiles(-S),
    }


def odd_in_cols():
    q = np.arange(0, 2048); kk = np.arange(2048, 4096); v = np.arange(4096, 6144); hy = np.arange(6144, 12288)
    sw = np.concatenate([np.arange(32, 64), np.arange(0, 32)])
    def swp(base):
        return np.concatenate([base[g * 64:(g + 1) * 64][sw] for g in range(32)])
    return np.concatenate([q, swp(q), kk, swp(kk), v, hy])


def rope4(n=NLAT):
    r = rope_table(64, n)
    return np.ascontiguousarray(np.concatenate([r[0:64], r[0:64]], 0)), np.ascontiguousarray(np.concatenate([r[64:128], r[64:128]], 0))


LAM_INIT1 = 0.8 - 0.6 * math.exp(-0.3 * 1)


def odd_inputs(z):
    c4, s4 = rope4()
    d = {
        'inW1': lhsT_tiles(np.ascontiguousarray(z['odd_in_w'][0][:, odd_in_cols()])),
        'cos4': c4, 'sin4': s4,
        'dlam': np.ascontiguousarray(z['diff_lambda'][0].reshape(1, 256)),
        'subln': np.ascontiguousarray(z['diff_subln'][0].reshape(128, 1)),
        'hy_convw': np.ascontiguousarray(z['hy_conv_w'][0].reshape(3, 48, 128).transpose(2, 1, 0)),
        'hy_w1': z['hy_w1'][0], 'hy_b1': np.ascontiguousarray(z['hy_b1'][0].reshape(64, 1)),
        'hy_w2': z['hy_w2'][0], 'hy_b2': np.ascontiguousarray(z['hy_b2'][0].reshape(64, 1)),
        'hy_w3T': np.ascontiguousarray(z['hy_w3'][0].reshape(64, 64, 128).transpose(1, 0, 2)),
        'hy_skip': np.ascontiguousarray(z['hy_skip'][0].reshape(2, 16, 128).transpose(2, 0, 1)),
        'outW1': lhsT_tiles(z['odd_out_w'][0]),
    }
    d.update(hyena_consts())
    return d


def odd_mts(isc):
    return (list(range(32, 48)) + list(range(64, 80))) if isc else list(range(128))


def build_program(dbg=False):
    P = Prog(dbg=dbg)
    P.consts()
    hT0 = P.inp('hT0', [D, T])
    P.st_adaln(0)
    hlT0 = P.st_inproj(0, hT0, 61, lambda isc: list(range(61)))
    qnT, qpT, knT, vT, kpT = P.st_mla_prep(hlT0)
    catT = P.scratch('catT', [D, T])
    P.st_mla_attn(qnT, qpT, knT, vT, kpT, catT)
    P.st_sconv(hlT0, catT)
    hA = P.scratch('hA', [D, T])
    P.st_outproj(0, catT, hT0, hA)
    hB = P.scratch('hB', [D, T])
    P.st_moe(0, hA, hB, True)
    P.st_adaln(1)
    hlT1 = P.st_inproj(1, hB, 128, odd_mts)
    P.st_diff_attn(hlT1, catT, LAM_INIT1)
    P.st_hyena(hlT1, catT, 80 * 128)
    hC = P.scratch('hC', [D, T])
    P.st_outproj(1, catT, hB, hC, with_ctx=False)
    hD = P.scratch('hD', [D, T])
    P.st_moe(1, hC, hD, False)
    outT = P.scratch('outT', [D, NLAT], out=True)
    P.st_final(hD, outT)
    P.k.barrier()
    return P


def host_inputs(z, nb):
    shared = {'consts': host_consts()}
    for l in range(2):
        shared[f'adaW{l}'] = lhsT_tiles(z['ada_w'][l])
        shared[f'adab{l}'] = fm_vec(z['ada_b'][l])
        shared.update(moe_inputs(z, l))
    shared['inW0'] = lhsT_tiles(np.ascontiguousarray(z['mla_in_w'][0][:, IN0_COLS]))
    shared['uqW'] = lhsT_tiles(np.ascontiguousarray(z['mla_w_uq'][0][:, uq_cols()]))
    shared['ukvW'] = lhsT_tiles(z['mla_w_ukv'][0])
    shared['qgain'] = fm_vec(z['mla_q_norm'][0])
    shared['kvgain'] = fm_vec(z['mla_kv_norm'][0])
    shared['rope_mla'] = rope_table(64)
    shared['sconv_w'] = np.ascontiguousarray(z['sc_conv_w'][0].reshape(3, 16, 128).transpose(2, 1, 0))
    shared['outW0'] = lhsT_tiles(z['even_out_w'][0])
    shared.update(odd_inputs(z))
    shared['final_gain'] = fm_vec(z['final_norm'])
    maps = []
    for b in range(nb):
        m = dict(shared)
        m['hT0'] = np.ascontiguousarray(np.concatenate([z['x'][b].T, z['ctx'][b].T], axis=1))
        m['cT'] = np.ascontiguousarray(np.stack([fm_vec(z['c'][b]), fm_vec(z['c_ctx'])], axis=-1))
        maps.append(m)
    return maps


def kernel(**inputs):
    z = {k: np.asarray(v, dtype=np.float32) for k, v in inputs.items()}
    nb = z['x'].shape[0]
    P = build_program()
    maps = host_inputs(z, nb)
    maps = [{n: m[n] for n in P.I} for m in maps]
    res = run_bass_kernel_spmd(P.nc, maps, core_ids=list(range(nb)))
    out = np.stack([np.ascontiguousarray(r['outT'].T) for r in res.results], axis=0)
    return out.astype(np.float32)
```

```python
from contextlib import ExitStack
import math
import numpy as np
import concourse.bass as bass
import concourse.mybir as mybir
from concourse.bass_utils import run_bass_kernel_spmd

F32 = mybir.dt.float32
F32R = mybir.dt.float32r
I32 = mybir.dt.int32
BF16 = mybir.dt.bfloat16
ALU = mybir.AluOpType
AF = mybir.ActivationFunctionType
AX = mybir.AxisListType

RING = 8
D = 4096
KC = 32
NLAT = 2048
NCTX = 256
T = NLAT + NCTX
EPS = 1e-6
NE = 32
DE = 640
NB0 = 2 * ((T * 4) // 256 + NE)
NB1 = 2 * ((NLAT * 4) // 256 + NE)


class KB:
    def __init__(self, nc):
        self.nc = nc
        self.E = {'pe': nc.tensor, 'act': nc.scalar, 'dve': nc.vector, 'pool': nc.gpsimd, 'sp': nc.sync}
        self.sems = {}
        self.cnt = {}
        for e in self.E:
            self.sems[e] = nc.alloc_semaphore(name=f"sem_{e}")
            self.cnt[e] = 0
        self.rings = {}
        self.dcnt = {}
        for q in ('sp', 'act', 'pool'):
            self.rings[q] = [nc.alloc_semaphore(name=f"dq_{q}_{i}") for i in range(RING)]
            self.dcnt[q] = 0
        self.semobj = {}
        for e in self.E:
            self.semobj[('c', e)] = self.sems[e]
        for q in self.rings:
            for i, s in enumerate(self.rings[q]):
                self.semobj[('d', q, i)] = s
        self.waited = {e: {} for e in self.E}
        self.state = {}
        self.children = {}
        self.latest = {}
        self.ninst = 0

    def _st(self, key):
        s = self.state.get(key)
        if s is None:
            s = {'w': {}, 'r': {}}
            self.state[key] = s
            if '/' in key:
                self.children.setdefault(key.split('/')[0], set()).add(key)
        return s

    def _conf(self, key):
        if '/' in key:
            return [key, key.split('/')[0]]
        return [key] + list(self.children.get(key, ()))

    def _deps(self, reads, writes):
        deps = {}

        def add(d):
            for sid, v in d.items():
                if deps.get(sid, 0) < v:
                    deps[sid] = v
        for key in reads:
            for ck in self._conf(key):
                if ck in self.state:
                    add(self.state[ck]['w'])
        for key in writes:
            for ck in self._conf(key):
                if ck in self.state:
                    add(self.state[ck]['w'])
                    add(self.state[ck]['r'])
        return deps

    def _commit(self, ev, reads, writes):
        sid, v = ev
        for key in reads:
            s = self._st(key)
            s['r'][sid] = max(s['r'].get(sid, 0), v)
        for key in writes:
            s = self._st(key)
            s['w'] = {sid: v}
            s['r'] = {}
            if '/' not in key:
                for ck in list(self.children.get(key, ())):
                    self.state[ck] = {'w': {}, 'r': {}}

    def _wait(self, eng, deps):
        w = self.waited[eng]
        for sid, v in deps.items():
            if eng == 'pe' and sid == ('c', 'pe'):
                continue
            if w.get(sid, 0) >= v:
                continue
            self.E[eng].wait_ge(self.semobj[sid], v)
            w[sid] = v

    def op(self, eng, fn, reads=(), writes=()):
        deps = self._deps(reads, writes)
        self._wait(eng, deps)
        inst = fn(self.E[eng])
        self.cnt[eng] += 1
        inst.then_inc(self.sems[eng], 1)
        ev = (('c', eng), self.cnt[eng])
        self.latest[ev[0]] = ev[1]
        self._commit(ev, reads, writes)
        self.ninst += 1
        return inst

    def dma(self, q, fn, reads=(), writes=()):
        deps = self._deps(reads, writes)
        i = self.dcnt[q]
        slot = i % RING
        sid = ('d', q, slot)
        if i >= RING:
            deps[sid] = max(deps.get(sid, 0), 16 * (i // RING))
        self._wait(q, deps)
        inst = fn(self.E[q])
        inst.then_inc(self.rings[q][slot], 16)
        self.dcnt[q] += 1
        ev = (sid, 16 * (i // RING + 1))
        self.latest[sid] = ev[1]
        self._commit(ev, reads, writes)
        self.ninst += 1
        return inst

    def barrier(self):
        for e in self.E:
            self._wait(e, dict(self.latest))
        self.state = {}
        self.children = {}


def _dma(out, in_):
    return lambda e: e.dma_start(out=out, in_=in_)


def f32(ap):
    return ap.bitcast(F32)


TCH = [(0, 512, False), (512, 512, False), (1024, 512, False), (1536, 512, False), (2048, 256, True)]


class Prog:
    def __init__(self, dbg=False):
        self.nc = bass.Bass("TRN2", target_bir_lowering=False)
        self.k = KB(self.nc)
        self.dbg = dbg
        self.I = {}
        self.S = {}
        self.O = {}
        nc = self.nc
        self.ps = [nc.alloc_psum_tensor(f"psb{i}", [128, 512], F32).ap() for i in range(8)]
        self.gl = ExitStack()
        self.ident = self.gsb('ident', [128, 128])
        self.onesR = self.gsb('onesR', [128, 128], F32R)
        self.ones = self.gsb('ones', [128, 128])
        self.triu = self.gsb('triu', [128, 128])
        self.identR = self.gsb('identR', [128, 128], F32R)
        self.mod = [self.gsb(f'mod{l}', [128, 2, 192]) for l in range(2)]
        self.opsc = [self.gsb(f'opsc{l}', [128, 2, 2, 32]) for l in range(2)]
        self.did_const = False
        self.bregs = {}
        for nm, val in (('e', NE - 1), ('d', NE * DE - 1), ('w', NE * D - 1)):
            r = nc.gpsimd.alloc_register(f'bnd_{nm}')
            nc.gpsimd.reg_mov(r, val)
            self.bregs[nm] = r

    def sbf(self, es):
        self._stage = getattr(self, '_stage', 0) + 1
        st = self._stage
        return lambda n, s, dt=F32: es.enter_context(self.nc.sbuf_tensor(f's{st}_{n}', s, dt)).ap()

    def gsb(self, name, shape, dt=F32):
        return self.gl.enter_context(self.nc.sbuf_tensor(name, shape, dt)).ap()

    def inp(self, name, shape, dt=F32):
        ap = self.nc.dram_tensor(name, list(shape), dt, kind="ExternalInput").ap()
        self.I[name] = ap
        return ap

    def scratch(self, name, shape, dt=F32, out=False):
        kind = "ExternalOutput" if (self.dbg or out) else "Internal"
        ap = self.nc.dram_tensor(name, list(shape), dt, kind=kind).ap()
        self.S[name] = ap
        return ap

    def consts(self):
        k = self.k
        c = self.inp('consts', [128, 3, 128])
        k.dma('sp', _dma(self.ident, c[:, 0, :]), writes=['ident'])
        k.dma('sp', _dma(self.ones, c[:, 1, :]), writes=['ones'])
        k.dma('sp', _dma(self.triu, c[:, 2, :]), writes=['triu'])
        k.dma('pool', _dma(self.onesR, c[:, 1, :]), writes=['onesR'])
        k.dma('pool', _dma(self.identR, c[:, 0, :]), writes=['identR'])

    def st_adaln(self, l):
        nc, k = self.nc, self.k
        cT = self.I.get('cT') if 'cT' in self.I else self.inp('cT', [128, 32, 2])
        adaW = self.inp(f'adaW{l}', [192, 128, 32, 128])
        adab = self.inp(f'adab{l}', [128, 192])
        mod = self.mod[l]
        ps = self.ps[0]
        with ExitStack() as es:
            sb = self.sbf(es)
            c_sb = sb('a_c', [128, 32, 2])
            sc = sb('a_sc', [128, 32, 2], F32R)
            bT = sb('a_b', [128, 192])
            wb = [sb(f'a_w{i}', [128, 32, 128], F32R) for i in range(3)]
            k.dma('sp', _dma(c_sb, cT), writes=['a_c'])
            k.dma('sp', _dma(bT, adab), writes=['a_b'])
            k.op('act', lambda e: e.activation(out=sc, in_=c_sb, func=AF.Silu), reads=['a_c'], writes=['a_sc'])
            for mt in range(192):
                w = wb[mt % 3]
                wk = f'a_w{mt % 3}'
                k.dma('pool', _dma(w, adaW[mt]), writes=[wk])
                for c in range(32):
                    k.op('pe', lambda e: e.matmul(ps[:, 2 * mt:2 * mt + 2], lhsT=w[:, c, :], rhs=sc[:, c, :],
                                                  start=(c == 0), stop=(c == 31)),
                         reads=[wk, 'a_sc'], writes=['a_ps'])
            pv = ps[:, 0:384].rearrange("p (m j) -> p j m", j=2)
            for j in range(2):
                k.op('dve', lambda e: e.tensor_tensor(out=mod[:, j, :], in0=pv[:, j, :], in1=bT, op=ALU.add),
                     reads=['a_ps', 'a_b'], writes=[f'mod{l}'])
            for j in range(2):
                for wh in range(2):
                    s0 = 32 + 96 * wh
                    k.op('dve', lambda e: e.tensor_scalar(out=self.opsc[l][:, j, wh, :], in0=mod[:, j, s0:s0 + 32],
                                                          scalar1=1.0, scalar2=None, op0=ALU.add),
                         reads=[f'mod{l}'], writes=[f'opsc{l}'])
            k.barrier()

    def rstd_from_ps(self, psS, rstd, tn, n, pk, rk):
        k = self.k
        k.op('dve', lambda e: e.tensor_scalar(out=rstd[:, :tn], in0=psS[:, :tn], scalar1=1.0 / n, scalar2=EPS,
                                              op0=ALU.mult, op1=ALU.add), reads=[pk], writes=[rk])
        k.op('act', lambda e: e.activation(out=rstd[:, :tn], in_=rstd[:, :tn], func=AF.Sqrt), reads=[rk], writes=[rk])
        k.op('dve', lambda e: e.reciprocal(out=rstd[:, :tn], in_=rstd[:, :tn]), reads=[rk], writes=[rk])

    def load_modulate(self, sb_, src, t0, tn, scale_ap, shift_ap, tag):
        k = self.k
        xs = sb_['xs']
        hs = sb_['hs']
        srcv = src[:, t0:t0 + tn].rearrange("(c p) t -> c p t", p=128)
        psS = self.ps[7]
        n = [0]

        def ld(c):
            i = n[0] = n[0] + 1
            h = hs[i % 4]
            hk = f'hs{i % 4}'
            k.dma('sp', _dma(h[:, :tn], srcv[c]), writes=[hk])
            return h, hk
        for c in range(KC):
            h, hk = ld(c)
            sq = sb_['sq'][c % 2]
            sqk = f'sq{c % 2}'
            k.op('act', lambda e: e.activation(out=sq[:, :tn], in_=h[:, :tn], func=AF.Square),
                 reads=[hk], writes=[sqk])
            k.op('pe', lambda e: e.matmul(psS[:, :tn], lhsT=self.onesR, rhs=sq[:, :tn], start=(c == 0), stop=(c == KC - 1)),
                 reads=[sqk, 'onesR'], writes=['ps7'])
        rstd = sb_['rstd']
        self.rstd_from_ps(psS, rstd, tn, D, 'ps7', 'rstd')
        for c in range(KC):
            h, hk = ld(c)
            if shift_ap is not None:
                tmp = sb_['tmp'][c % 2]
                tk = f'tmp{c % 2}'
                k.op('dve', lambda e: e.scalar_tensor_tensor(out=tmp[:, :tn], in0=h[:, :tn], scalar=scale_ap[:, c:c + 1],
                                                             in1=rstd[:, :tn], op0=ALU.mult, op1=ALU.mult),
                     reads=[hk, 'rstd'], writes=[tk])
                k.op('act', lambda e: e.activation(out=xs[:, c, :tn], in_=tmp[:, :tn], func=AF.Identity,
                                                   bias=shift_ap[:, c:c + 1], scale=1.0),
                     reads=[tk], writes=[f'xs/{c}'])
            else:
                k.op('dve', lambda e: e.scalar_tensor_tensor(out=xs[:, c, :tn], in0=h[:, :tn], scalar=scale_ap[:, c:c + 1],
                                                             in1=rstd[:, :tn], op0=ALU.mult, op1=ALU.mult),
                     reads=[hk, 'rstd'], writes=[f'xs/{c}'])
        return xs

    def mod_bufs(self, sb, xdt=F32R):
        return {'xs': sb('xs', [128, 32, 512], xdt), 'hs': [sb(f'hs{i}', [128, 512]) for i in range(4)],
                'sq': [sb(f'sq{i}', [128, 512], F32R) for i in range(2)],
                'rstd': sb('rstd', [128, 512]), 'tmp': [sb(f'tmp{i}', [128, 512]) for i in range(2)]}

    def linear(self, es, xs, xkey, kc, wt, nmt, tn, epilogue, tag, mw_of=None, wdt=F32R):
        nc, k = self.nc, self.k
        if not hasattr(self, '_lw'):
            self._lw = {}
        key = (tag, kc)
        if key not in self._lw:
            self._lw[key] = [es.enter_context(nc.sbuf_tensor(f's{self._stage}_{tag}_w{i}', [128, kc, 128], wdt)).ap() for i in range(3)]
        wb = self._lw[key]
        for mt in range(nmt):
            mw = 128 if mw_of is None else mw_of(mt)
            i = self._lwc = getattr(self, '_lwc', 0) + 1
            w = wb[i % 3]
            wk = f'{tag}_w{i % 3}'
            k.dma('pool', _dma(w[:, :, :mw], wt[mt][:, :, :mw]), writes=[wk])
            pi = i % 2
            ps = self.ps[pi]
            for c in range(kc):
                k.op('pe', lambda e: e.matmul(ps[:mw, :tn], lhsT=w[:, c, :mw], rhs=xs[:, c, :tn], start=(c == 0), stop=(c == kc - 1)),
                     reads=[wk, xkey], writes=[f'ps{pi}'])
            epilogue(mt, ps, f'ps{pi}', mw)

    def st_inproj(self, l, src, nmt, chunks_mt):
        nc, k = self.nc, self.k
        inW = self.inp(f'inW{l}', [nmt, 128, 32, 128])
        hlT = self.scratch(f'hlT{l}', [nmt * 128, T])
        mod = self.mod[l]
        with ExitStack() as es:
            sb = self.sbf(es)
            sb_ = self.mod_bufs(sb, BF16)
            ost = [sb(f'ost{i}', [128, 512]) for i in range(4)]
            cnt = [0]
            self._lw = {}
            for (t0, tn, isc) in TCH:
                j = 1 if isc else 0
                xs = self.load_modulate(sb_, src, t0, tn, self.opsc[l][:, j, 0, :], mod[:, j, 0:32], 'ip')
                mts = chunks_mt(isc)

                def epi(mi, ps, pk, mw, t0=t0, tn=tn, mts=mts):
                    mt = mts[mi]
                    i = cnt[0] = cnt[0] + 1
                    o = ost[i % 4]
                    ok = f'ost{i % 4}'
                    if i % 2 == 0:
                        k.op('act', lambda e: e.activation(out=o[:, :tn], in_=ps[:, :tn], func=AF.Copy), reads=[pk], writes=[ok])
                    else:
                        k.op('dve', lambda e: e.tensor_copy(out=o[:, :tn], in_=ps[:, :tn]), reads=[pk], writes=[ok])
                    k.dma('sp', _dma(hlT[mt * 128:(mt + 1) * 128, t0:t0 + tn], o[:, :tn]), reads=[ok], writes=[f'hlT/{mt}_{t0}'])
                wts = [inW[mt] for mt in mts]
                self.linear(es, xs, 'xs', 32, wts, len(mts), tn, epi, 'ip', wdt=BF16)
            k.barrier()
        return hlT

    def norm_linear(self, es, sb, src_rows, kc, nfeat, gain, t0, tn, wt, nmt, epi, tag):
        nc, k = self.nc, self.k
        bufs = self._nl.get(tag)
        if bufs is None:
            bufs = self._nl[tag] = {'x': sb(f'{tag}_x', [128, kc, 512]), 'xr': sb(f'{tag}_xr', [128, kc, 512], F32R),
                                    'sq': [sb(f'{tag}_sq{i}', [128, 512], F32R) for i in range(2)], 'rstd': sb(f'{tag}_rstd', [128, 512])}
        x, xr, rstd = bufs['x'], bufs['xr'], bufs['rstd']
        k.dma('sp', _dma(x[:, :, :tn], src_rows[:, t0:t0 + tn].rearrange("(c p) t -> p c t", p=128)), writes=[f'{tag}_x'])
        psS = self.ps[7]
        for c in range(kc):
            sq = bufs['sq'][c % 2]
            sqk = f'{tag}_sq{c % 2}'
            k.op('act', lambda e: e.activation(out=sq[:, :tn], in_=x[:, c, :tn], func=AF.Square), reads=[f'{tag}_x'], writes=[sqk])
            k.op('pe', lambda e: e.matmul(psS[:, :tn], lhsT=self.onesR, rhs=sq[:, :tn], start=(c == 0), stop=(c == kc - 1)),
                 reads=[sqk, 'onesR'], writes=['ps7'])
        self.rstd_from_ps(psS, rstd, tn, nfeat, 'ps7', f'{tag}_rstd')
        for c in range(kc):
            k.op('dve', lambda e: e.scalar_tensor_tensor(out=xr[:, c, :tn], in0=x[:, c, :tn], scalar=gain[:, c:c + 1],
                                                         in1=rstd[:, :tn], op0=ALU.mult, op1=ALU.mult),
                 reads=[f'{tag}_x', f'{tag}_rstd'], writes=[f'{tag}_xr/{c}'])
        self.linear(es, xr, f'{tag}_xr', kc, wt, nmt, tn, epi, tag)

    def st_mla_prep(self, hlT):
        nc, k = self.nc, self.k
        uqW = self.inp('uqW', [32, 128, 8, 128])
        ukvW = self.inp('ukvW', [32, 128, 4, 128])
        qg = self.inp('qgain', [128, 8])
        kvg = self.inp('kvgain', [128, 4])
        ropeT = self.inp('rope_mla', [128, NLAT])
        qnT = self.scratch('qnT', [16, 128, T])
        qpT = self.scratch('qpT', [16, 64, T])
        knT = self.scratch('knT', [16, 128, T])
        vT = self.scratch('vT', [16, 128, T])
        kpT = self.scratch('kpT', [64, T])
        qscale = 192.0 ** -0.5
        with ExitStack() as es:
            sb = self.sbf(es)
            self._nl = {}
            self._lw = {}
            qg_sb = sb('qg', [128, 8]); kvg_sb = sb('kvg', [128, 4]); rope = sb('rope', [128, NLAT])
            k.dma('sp', _dma(qg_sb, qg), writes=['qg'])
            k.dma('sp', _dma(kvg_sb, kvg), writes=['kvg'])
            k.dma('sp', _dma(rope, ropeT), writes=['rope'])
            ost = [sb(f'ost{i}', [128, 512]) for i in range(4)]
            tt = [sb(f'tt{i}', [128, 512]) for i in range(2)]
            kpe = sb('kpe', [128, 512])
            cnt = [0]
            for (t0, tn, isc) in TCH:
                def nxt():
                    i = cnt[0] = cnt[0] + 1
                    return ost[i % 4], f'ost{i % 4}', i

                def rope_out(src_ap, srck, scale, t0=t0, tn=tn, isc=isc):
                    o, ok, i = nxt()
                    if isc:
                        k.op('act', lambda e: e.activation(out=o[0:64, :tn], in_=src_ap[0:64, :tn], func=AF.Copy, scale=scale),
                             reads=[srck], writes=[ok])
                    else:
                        t = tt[i % 2]
                        tk = f'tt{i % 2}'
                        k.op('dve', lambda e: e.scalar_tensor_tensor(out=t[:, :tn], in0=src_ap[:, :tn], scalar=scale, in1=rope[:, t0:t0 + tn],
                                                                     op0=ALU.mult, op1=ALU.mult), reads=[srck, 'rope'], writes=[tk])
                        k.op('act', lambda e: e.activation(out=o[64:128, :tn], in_=t[0:64, :tn], func=AF.Copy), reads=[tk], writes=[ok])
                        k.op('dve', lambda e: e.tensor_tensor(out=o[0:64, :tn], in0=o[64:128, :tn], in1=t[64:128, :tn], op=ALU.add),
                             reads=[tk, ok], writes=[ok])
                    return o, ok

                def epi_q(mt, ps, pk, mw, t0=t0, tn=tn):
                    h = mt // 2
                    if mt % 2 == 0:
                        o, ok, i = nxt()
                        k.op('act', lambda e: e.activation(out=o[:, :tn], in_=ps[:, :tn], func=AF.Copy, scale=qscale), reads=[pk], writes=[ok])
                        k.dma('sp', _dma(qnT[h, :, t0:t0 + tn], o[:, :tn]), reads=[ok], writes=[f'qnT/{h}_{t0}'])
                    else:
                        o, ok = rope_out(ps, pk, qscale)
                        k.dma('sp', _dma(qpT[h, :, t0:t0 + tn], o[0:64, :tn]), reads=[ok], writes=[f'qpT/{h}_{t0}'])
                self.norm_linear(es, sb, hlT[0:1024], 8, 1024, qg_sb, t0, tn, [uqW[i] for i in range(32)], 32, epi_q, 'uq')

                def epi_kv(mt, ps, pk, mw, t0=t0, tn=tn):
                    h = mt // 2
                    o, ok, i = nxt()
                    if i % 2 == 0:
                        k.op('act', lambda e: e.activation(out=o[:, :tn], in_=ps[:, :tn], func=AF.Copy), reads=[pk], writes=[ok])
                    else:
                        k.op('dve', lambda e: e.tensor_copy(out=o[:, :tn], in_=ps[:, :tn]), reads=[pk], writes=[ok])
                    dst = knT if mt % 2 == 0 else vT
                    k.dma('sp', _dma(dst[h, :, t0:t0 + tn], o[:, :tn]), reads=[ok], writes=[f'kv{mt % 2}/{h}_{t0}'])
                self.norm_linear(es, sb, hlT[1024:1536], 4, 512, kvg_sb, t0, tn, [ukvW[i] for i in range(32)], 32, epi_kv, 'ukv')
                k.dma('sp', _dma(kpe[:, :tn], hlT[1536:1664, t0:t0 + tn]), writes=['kpe'])
                o, ok = rope_out(kpe, 'kpe', 1.0)
                k.dma('sp', _dma(kpT[:, t0:t0 + tn], o[0:64, :tn]), reads=[ok], writes=[f'kpT/{t0}'])
            k.barrier()
        return qnT, qpT, knT, vT, kpT

    def st_mla_attn(self, qnT, qpT, knT, vT, kpT, catT, with_ctx_q=True):
        nc, k = self.nc, self.k
        with ExitStack() as es:
            sb = self.sbf(es)
            kp = sb('at_kp', [64, T], F32R)
            k.dma('pool', _dma(kp, kpT), writes=['at_kp'])
            kn = [sb(f'at_kn{i}', [128, T], F32R) for i in range(2)]
            qn = [sb(f'at_qn{i}', [128, T], F32R) for i in range(2)]
            qp = [sb(f'at_qp{i}', [64, T], F32R) for i in range(2)]
            vt = [sb(f'at_vt{i}', [128, T]) for i in range(2)]
            vm = [sb(f'at_vm{i}', [128, 18, 128], F32R) for i in range(2)]
            pT = [sb(f'at_p{i}', [128, 512], F32R) for i in range(3)]
            rden = [sb(f'at_rd{i}', [128, 512]) for i in range(2)]
            ob = [sb(f'at_o{i}', [128, 512]) for i in range(2)]
            pc = 0
            qcn = 0
            for h in range(16):
                b = h % 2
                k.dma('pool', _dma(kn[b], knT[h]), writes=[f'at_kn{b}'])
                k.dma('pool', _dma(qn[b], qnT[h]), writes=[f'at_qn{b}'])
                k.dma('pool', _dma(qp[b], qpT[h]), writes=[f'at_qp{b}'])
                k.dma('sp', _dma(vt[b], vT[h]), writes=[f'at_vt{b}'])
                for g in range(5):
                    n = min(4, 18 - 4 * g)
                    pst = self.ps[6]
                    for j in range(n):
                        kt = 4 * g + j
                        k.op('pe', lambda e: e.transpose(out=pst[:, j * 128:(j + 1) * 128], in_=vt[b][:, kt * 128:(kt + 1) * 128], identity=self.ident),
                             reads=[f'at_vt{b}', 'ident'], writes=['ps6'])
                    k.op('act', lambda e: e.activation(out=vm[b][:, 4 * g:4 * g + n, :], in_=pst[:, :n * 128].rearrange("p (j d) -> p j d", d=128), func=AF.Copy),
                         reads=['ps6'], writes=[f'at_vm{b}'])
                qcs = [(t0, tn, list(range(18))) for (t0, tn, isc) in TCH if not isc]
                if with_ctx_q:
                    qcs.append((NLAT, NCTX, [16, 17]))
                for (t0, tn, kts) in qcs:
                    qcn += 1
                    pn = self.ps[2 + qcn % 2]
                    pd = self.ps[4 + qcn % 2]
                    pnk, pdk = f'ps{2 + qcn % 2}', f'ps{4 + qcn % 2}'
                    def emit_s(kt, t0=t0, tn=tn, b=b):
                        nonlocal pc
                        pc += 1
                        pss = self.ps[pc % 2]
                        psk = f'ps{pc % 2}'
                        ks = slice(kt * 128, (kt + 1) * 128)
                        k.op('pe', lambda e: e.matmul(pss[:, :tn], lhsT=kn[b][:, ks], rhs=qn[b][:, t0:t0 + tn], start=True, stop=False),
                             reads=[f'at_kn{b}', f'at_qn{b}'], writes=[psk])
                        k.op('pe', lambda e: e.matmul(pss[:, :tn], lhsT=kp[:, ks], rhs=qp[b][:, t0:t0 + tn], start=False, stop=True),
                             reads=['at_kp', f'at_qp{b}'], writes=[psk])
                        return pss, psk, pc
                    cur = emit_s(kts[0])
                    for ii, kt in enumerate(kts):
                        nxt_ = emit_s(kts[ii + 1]) if ii + 1 < len(kts) else None
                        pss, psk, pci = cur
                        p = pT[pci % 3]
                        ppk = f'at_p{pci % 3}'
                        k.op('act', lambda e: e.activation(out=p[:, :tn], in_=pss[:, :tn], func=AF.Exp), reads=[psk], writes=[ppk])
                        k.op('pe', lambda e: e.matmul(pn[:, :tn], lhsT=vm[b][:, kt, :], rhs=p[:, :tn], start=(ii == 0), stop=(ii == len(kts) - 1)),
                             reads=[f'at_vm{b}', ppk], writes=[pnk])
                        k.op('pe', lambda e: e.matmul(pd[:, :tn], lhsT=self.onesR, rhs=p[:, :tn], start=(ii == 0), stop=(ii == len(kts) - 1)),
                             reads=['onesR', ppk], writes=[pdk])
                        cur = nxt_
                    rd = rden[qcn % 2]
                    o = ob[qcn % 2]
                    k.op('dve', lambda e: e.reciprocal(out=rd[:, :tn], in_=pd[:, :tn]), reads=[pdk], writes=[f'at_rd{qcn % 2}'])
                    k.op('dve', lambda e: e.tensor_tensor(out=o[:, :tn], in0=pn[:, :tn], in1=rd[:, :tn], op=ALU.mult),
                         reads=[pnk, f'at_rd{qcn % 2}'], writes=[f'at_o{qcn % 2}'])
                    k.dma('sp', _dma(catT[h * 128:(h + 1) * 128, t0:t0 + tn], o[:, :tn]), reads=[f'at_o{qcn % 2}'], writes=[f'catT/{h}_{t0}'])
            k.barrier()

    def st_sconv(self, hlT, catT, row0=1664):
        nc, k = self.nc, self.k
        cw = self.inp('sconv_w', [128, 16, 3])
        with ExitStack() as es:
            sb = self.sbf(es)
            cw_sb = sb('cw', [128, 16, 3])
            k.dma('sp', _dma(cw_sb, cw), writes=['cw'])
            bufs = [[sb(f'sc_{nm}{i}', [128, T]) for nm in ('gb', 'gc', 'hh', 'y')] for i in range(2)]
            segs = [(0, NLAT), (NLAT, T)]
            for c in range(16):
                i = c % 2
                gb, gc, hh, y = bufs[i]
                kk = [f'sc_{nm}{i}' for nm in ('gb', 'gc', 'hh', 'y')]
                for j, buf in enumerate((gb, gc, hh)):
                    r0 = row0 + j * 2048 + c * 128
                    k.dma('sp', _dma(buf, hlT[r0:r0 + 128, :]), writes=[kk[j]])
                k.op('dve', lambda e: e.tensor_tensor(out=gc, in0=gc, in1=hh, op=ALU.mult), reads=[kk[1], kk[2]], writes=[kk[1]])
                k.op('act', lambda e: e.activation(out=y, in_=gc, func=AF.Copy, scale=cw_sb[:, c, 1:2]), reads=[kk[1], 'cw'], writes=[kk[3]])
                for (a, bnd) in segs:
                    k.op('dve', lambda e: e.scalar_tensor_tensor(out=y[:, a + 1:bnd], in0=gc[:, a:bnd - 1], scalar=cw_sb[:, c, 0:1], in1=y[:, a + 1:bnd],
                                                                 op0=ALU.mult, op1=ALU.add), reads=[kk[1], kk[3], 'cw'], writes=[kk[3]])
                    k.op('dve', lambda e: e.scalar_tensor_tensor(out=y[:, a:bnd - 1], in0=gc[:, a + 1:bnd], scalar=cw_sb[:, c, 2:3], in1=y[:, a:bnd - 1],
                                                                 op0=ALU.mult, op1=ALU.add), reads=[kk[1], kk[3], 'cw'], writes=[kk[3]])
                k.op('pool', lambda e: e.tensor_tensor(out=y, in0=y, in1=gb, op=ALU.mult), reads=[kk[0], kk[3]], writes=[kk[3]])
                k.dma('sp', _dma(catT[2048 + c * 128:2048 + (c + 1) * 128, :], y), reads=[kk[3]], writes=[f'catT/s{c}'])
            k.barrier()

    def st_outproj(self, l, catT, hsrc, hdst, with_ctx=True):
        nc, k = self.nc, self.k
        outW = self.inp(f'outW{l}', [32, 128, 32, 128])
        mod = self.mod[l]
        with ExitStack() as es:
            sb = self.sbf(es)
            self._lw = {}
            xs = sb('op_xs', [128, 32, 512], BF16)
            ht = [sb(f'op_h{i}', [128, 512]) for i in range(3)]
            cnt = [0]
            for (t0, tn, isc) in TCH:
                if isc and not with_ctx:
                    continue
                j = 1 if isc else 0
                k.dma('pool', _dma(xs[:, :, :tn], catT[:, t0:t0 + tn].rearrange("(c p) t -> p c t", p=128)), writes=['op_xs'])

                def epi(mt, ps, pk, mw, t0=t0, tn=tn, j=j):
                    i = cnt[0] = cnt[0] + 1
                    h = ht[i % 3]
                    hk = f'op_h{i % 3}'
                    k.dma('sp', _dma(h[:, :tn], hsrc[mt * 128:(mt + 1) * 128, t0:t0 + tn]), writes=[hk])
                    k.op('dve', lambda e: e.scalar_tensor_tensor(out=h[:, :tn], in0=ps[:, :tn], scalar=mod[:, j, 64 + mt:65 + mt], in1=h[:, :tn],
                                                                 op0=ALU.mult, op1=ALU.add), reads=[pk, hk, f'mod{l}'], writes=[hk])
                    k.dma('sp', _dma(hdst[mt * 128:(mt + 1) * 128, t0:t0 + tn], h[:, :tn]), reads=[hk], writes=[f'hdst/{mt}_{t0}'])
                self.linear(es, xs, 'op_xs', 32, [outW[i] for i in range(32)], 32, tn, epi, 'op', wdt=BF16)
            k.barrier()

    def st_moe(self, l, hsrc, hdst, with_ctx):
        nc, k = self.nc, self.k
        mod = self.mod[l]
        chunks = [c for c in TCH if (with_ctx or not c[2])]
        ntok = sum(c[1] for c in chunks)
        NT = ntok // 128
        NP = (ntok * 4) // 256 + NE
        NB = 2 * NP
        rw = self.inp(f'routerW{l}', [128, 32, 32])
        rb = self.inp(f'routerb{l}', [1, 32])
        wgu = self.inp(f'moe_wgu{l}', [NE * D, 2 * DE])
        bgu = self.inp(f'moe_bgu{l}', [NE, 2 * DE])
        wdn = self.inp(f'moe_wdn{l}', [NE * DE, D])
        bdn = self.inp(f'moe_bdn{l}', [NE, D])
        pidx = self.I['pidx'] if 'pidx' in self.I else self.inp('pidx', [128, 1])
        blk128 = self.I['blk128'] if 'blk128' in self.I else self.inp('blk128', [128, NB0])
        HD = D // 2
        if 'u2' not in self.S:
            self.scratch('u2', [T, D])
            for i in range(2):
                self.scratch(f'xslots_{i}', [NB0 * 128, HD])
                self.scratch(f'yslots_{i}', [NB0 * 128, HD])
        u2 = self.S['u2']
        xsl = [self.S[f'xslots_{i}'] for i in range(2)]
        ysl = [self.S[f'yslots_{i}'] for i in range(2)]
        with ExitStack() as esg:
            gsb = self.sbf(esg)
            lg_all = gsb('lg_all', [128, NT, 32]); top8 = gsb('top8', [128, NT, 8]); rank_all = gsb('rank_all', [128, NT, 32])
            gate_all = gsb('gate_all', [128, NT, 4]); dest_f = gsb('dest_f', [128, NT * 4]); dest_i = gsb('dest_i', [128, NT * 4], I32)
            base = gsb('base', [128, 32]); pstart = gsb('pstart', [128, 32]); blk_i = gsb('blk_i', [128, NB], I32)
            widx = gsb('widx', [128, NB], I32); didx = gsb('didx', [128, NB], I32)
            with ExitStack() as es:
                sb = self.sbf(es)
                sb_ = self.mod_bufs(sb)
                rw_sb = sb('rw', [128, 32, 32], F32R); rb_sb = sb('rb', [1, 32], F32R)
                k.dma('pool', _dma(rw_sb, rw), writes=['rw'])
                k.dma('pool', _dma(rb_sb, rb), writes=['rb'])
                k.op('dve', lambda e: e.memset(base, 0.0), writes=['base'])
                mask = [sb(f'mask{i}', [128, 32]) for i in range(2)]
                sm = [sb(f'sm{i}', [128, 8]) for i in range(2)]
                u2tm = [sb(f'u2tm{i}', [128, D]) for i in range(2)]
                ti = 0
                trn = 0
                for (t0, tn, isc) in chunks:
                    j = 1 if isc else 0
                    xs = self.load_modulate(sb_, hsrc, t0, tn, self.opsc[l][:, j, 1, :], mod[:, j, 96:128], 'mo')
                    for tt in range(tn // 128):
                        tsl = slice(tt * 128, (tt + 1) * 128)
                        pr = self.ps[3 + ti % 2]
                        prk = f'ps{3 + ti % 2}'
                        for c in range(KC):
                            k.op('pe', lambda e: e.matmul(pr[:, 64:96], lhsT=xs[:, c, tsl], rhs=rw_sb[:, c, :], start=(c == 0), stop=False),
                                 reads=['xs', 'rw'], writes=[prk])
                        k.op('pe', lambda e: e.matmul(pr[:, 64:96], lhsT=self.onesR[0:1, :], rhs=rb_sb, start=False, stop=True),
                             reads=['onesR', 'rb'], writes=[prk])
                        lg = lg_all[:, ti, :]
                        k.op('act', lambda e: e.activation(out=lg, in_=pr[:, 64:96], func=AF.Copy), reads=[prk], writes=['lg_all'])
                        k.op('dve', lambda e: e.max(out=top8[:, ti, :], in_=lg), reads=['lg_all'], writes=['top8'])
                        m = mask[ti % 2]
                        mk = f'mask{ti % 2}'
                        k.op('dve', lambda e: e.tensor_scalar(out=m, in0=lg, scalar1=top8[:, ti, 3:4], scalar2=None, op0=ALU.is_ge),
                             reads=['lg_all', 'top8'], writes=[mk])
                        s_ = sm[ti % 2]
                        sk = f'sm{ti % 2}'
                        k.op('dve', lambda e: e.tensor_scalar(out=s_[:, 0:1], in0=top8[:, ti, 0:1], scalar1=-1.0, scalar2=None, op0=ALU.mult),
                             reads=['top8'], writes=[sk])
                        k.op('act', lambda e: e.activation(out=s_[:, 4:8], in_=top8[:, ti, 0:4], func=AF.Exp, bias=s_[:, 0:1], scale=1.0),
                             reads=['top8', sk], writes=[sk])
                        k.op('dve', lambda e: e.tensor_reduce(out=s_[:, 1:2], in_=s_[:, 4:8], axis=AX.X, op=ALU.add), reads=[sk], writes=[sk])
                        k.op('dve', lambda e: e.reciprocal(out=s_[:, 2:3], in_=s_[:, 1:2]), reads=[sk], writes=[sk])
                        k.op('dve', lambda e: e.tensor_scalar(out=gate_all[:, ti, :], in0=s_[:, 4:8], scalar1=s_[:, 2:3], scalar2=None, op0=ALU.mult),
                             reads=[sk], writes=['gate_all'])
                        k.op('pe', lambda e: e.matmul(pr[:, 0:32], lhsT=self.triu, rhs=m, start=True, stop=True), reads=['triu', mk], writes=[prk])
                        k.op('pe', lambda e: e.matmul(pr[:, 32:64], lhsT=self.ones, rhs=m, start=True, stop=True), reads=['ones', mk], writes=[prk])
                        k.op('dve', lambda e: e.tensor_tensor(out=rank_all[:, ti, :], in0=pr[:, 0:32], in1=base, op=ALU.add),
                             reads=[prk, 'base'], writes=['rank_all'])
                        k.op('dve', lambda e: e.tensor_tensor(out=base, in0=pr[:, 32:64], in1=base, op=ALU.add), reads=[prk, 'base'], writes=['base'])
                        ut = u2tm[ti % 2]
                        uk = f'u2tm{ti % 2}'
                        for g in range(8):
                            trn += 1
                            pt = self.ps[5 + trn % 2]
                            ptk = f'ps{5 + trn % 2}'
                            for jj in range(4):
                                c = 4 * g + jj
                                k.op('pe', lambda e: e.transpose(out=pt[:, jj * 128:(jj + 1) * 128].bitcast(F32R), in_=xs[:, c, tsl], identity=self.identR),
                                     reads=['xs', 'identR'], writes=[ptk])
                            if g % 2 == 0:
                                k.op('act', lambda e: e.activation(out=ut[:, g * 512:(g + 1) * 512], in_=pt, func=AF.Copy), reads=[ptk], writes=[uk])
                            else:
                                k.op('dve', lambda e: e.tensor_copy(out=ut[:, g * 512:(g + 1) * 512], in_=pt), reads=[ptk], writes=[uk])
                        k.dma('sp', _dma(u2[ti * 128:(ti + 1) * 128, :], ut), reads=[uk], writes=[f'u2/{ti}'])
                        ti += 1
                k.barrier()
            with ExitStack() as es:
                sb = self.sbf(es)
                t1 = sb('t1', [128, 32]); t2 = sb('t2', [128, 32]); padded = sb('padded', [128, 32])
                cs = [sb(f'cs{i}', [128, 32]) for i in range(2)]
                b128 = sb('b128', [128, NB]); acc = sb('acc', [128, NB])
                k.dma('sp', _dma(b128, blk128[:, :NB]), writes=['b128'])
                k.op('dve', lambda e: e.tensor_scalar(out=t1, in0=base, scalar1=255.0, scalar2=None, op0=ALU.add), reads=['base'], writes=['t1'])
                ti1 = sb('ti1', [128, 32], I32); ti2 = sb('ti2', [128, 32], I32)
                k.op('dve', lambda e: e.tensor_copy(out=ti1, in_=t1), reads=['t1'], writes=['ti1'])
                k.op('dve', lambda e: e.tensor_scalar(out=ti2, in0=ti1, scalar1=8, scalar2=8, op0=ALU.arith_shift_right, op1=ALU.logical_shift_left),
                     reads=['ti1'], writes=['ti2'])
                k.op('dve', lambda e: e.tensor_copy(out=padded, in_=ti2), reads=['ti2'], writes=['padded'])
                k.op('dve', lambda e: e.tensor_copy(out=cs[0], in_=padded), reads=['padded'], writes=['cs0'])
                cur = 0
                for sh in (1, 2, 4, 8, 16):
                    a, b_ = cs[cur], cs[1 - cur]
                    k.op('dve', lambda e: e.tensor_copy(out=b_[:, 0:sh], in_=a[:, 0:sh]), reads=[f'cs{cur}'], writes=[f'cs{1 - cur}'])
                    k.op('dve', lambda e: e.tensor_tensor(out=b_[:, sh:32], in0=a[:, sh:32], in1=a[:, 0:32 - sh], op=ALU.add),
                         reads=[f'cs{cur}'], writes=[f'cs{1 - cur}'])
                    cur = 1 - cur
                pend = cs[cur]
                pek = f'cs{cur}'
                k.op('dve', lambda e: e.tensor_tensor(out=pstart, in0=pend, in1=padded, op=ALU.subtract), reads=[pek, 'padded'], writes=['pstart'])
                k.op('dve', lambda e: e.memset(acc, 0.0), writes=['acc'])
                for ex in range(NE):
                    k.op('dve', lambda e: e.scalar_tensor_tensor(out=acc, in0=b128, scalar=pend[:, ex:ex + 1], in1=acc, op0=ALU.is_ge, op1=ALU.add),
                         reads=['b128', pek, 'acc'], writes=['acc'])
                k.op('dve', lambda e: e.tensor_scalar(out=acc, in0=acc, scalar1=float(NE - 1), scalar2=None, op0=ALU.min), reads=['acc'], writes=['acc'])
                unus = sb('unus', [128, NB])
                BIG = 4194304.0
                k.op('dve', lambda e: e.tensor_scalar(out=unus, in0=b128, scalar1=pend[:, NE - 1:NE], scalar2=BIG, op0=ALU.is_ge, op1=ALU.mult),
                     reads=['b128', pek], writes=['unus'])
                eix = sb('eix', [128, NB])
                k.op('dve', lambda e: e.tensor_tensor(out=eix, in0=acc, in1=unus, op=ALU.add), reads=['acc', 'unus'], writes=['eix'])
                k.op('dve', lambda e: e.tensor_copy(out=blk_i, in_=eix), reads=['eix'], writes=['blk_i'])
                pix = sb('pix', [128, 1]); acc2 = sb('acc2', [128, NB])
                k.dma('sp', _dma(pix, pidx), writes=['pix'])
                k.op('dve', lambda e: e.tensor_scalar(out=acc2, in0=acc, scalar1=float(D), scalar2=pix[:, 0:1], op0=ALU.mult, op1=ALU.add), reads=['acc', 'pix'], writes=['acc2'])
                k.op('dve', lambda e: e.tensor_tensor(out=acc2, in0=acc2, in1=unus, op=ALU.add), reads=['acc2', 'unus'], writes=['acc2'])
                k.op('dve', lambda e: e.tensor_copy(out=widx, in_=acc2), reads=['acc2'], writes=['widx'])
                k.op('dve', lambda e: e.tensor_scalar(out=acc2, in0=acc, scalar1=float(DE), scalar2=pix[:, 0:1], op0=ALU.mult, op1=ALU.add), reads=['acc', 'pix'], writes=['acc2'])
                k.op('dve', lambda e: e.tensor_tensor(out=acc2, in0=acc2, in1=unus, op=ALU.add), reads=['acc2', 'unus'], writes=['acc2'])
                k.op('dve', lambda e: e.tensor_copy(out=didx, in_=acc2), reads=['acc2'], writes=['didx'])
                dall = sb('dall', [128, 32]); prod = [sb(f'prod{i}', [128, 32]) for i in range(2)]
                for ti in range(NT):
                    k.op('dve', lambda e: e.tensor_tensor(out=dall, in0=rank_all[:, ti, :], in1=pstart, op=ALU.add),
                         reads=['rank_all', 'pstart'], writes=['dall'])
                    for k4 in range(4):
                        p_ = prod[k4 % 2]
                        k.op('dve', lambda e: e.scalar_tensor_tensor(out=p_, in0=lg_all[:, ti, :], scalar=top8[:, ti, k4:k4 + 1], in1=dall,
                                                                     op0=ALU.is_equal, op1=ALU.mult), reads=['lg_all', 'top8', 'dall'], writes=[f'prod{k4 % 2}'])
                        k.op('dve', lambda e: e.tensor_reduce(out=dest_f[:, ti * 4 + k4:ti * 4 + k4 + 1], in_=p_, axis=AX.X, op=ALU.add),
                             reads=[f'prod{k4 % 2}'], writes=['dest_f'])
                k.op('dve', lambda e: e.tensor_scalar(out=dest_f, in0=dest_f, scalar1=0.0, scalar2=float(NB * 128 - 1), op0=ALU.max, op1=ALU.min), reads=['dest_f'], writes=['dest_f'])
                k.op('dve', lambda e: e.tensor_copy(out=dest_i, in_=dest_f), reads=['dest_f'], writes=['dest_i'])
                if self.dbg:
                    dd = self.scratch(f'dbg_dest{l}', [128, NT * 4], I32)
                    k.dma('sp', _dma(dd, dest_i), reads=['dest_i'], writes=['dbgd'])
                    db = self.scratch(f'dbg_blk{l}', [128, NB], I32)
                    k.dma('sp', _dma(db, blk_i), reads=['blk_i'], writes=['dbgb'])
                    dg = self.scratch(f'dbg_gate{l}', [128, NT, 4])
                    k.dma('sp', _dma(dg, gate_all), reads=['gate_all'], writes=['dbgg'])
                xrow = [sb(f'xrow{i}', [128, D]) for i in range(2)]
                for ti in range(NT):
                    xr = xrow[ti % 2]
                    xk = f'xrow{ti % 2}'
                    k.dma('sp', _dma(xr, u2[ti * 128:(ti + 1) * 128, :]), writes=[xk])
                    for k4 in range(4):
                        col = ti * 4 + k4
                        for hf in range(2):
                            k.dma('pool', lambda e: e.indirect_dma_start(out=xsl[hf], out_offset=bass.IndirectOffsetOnAxis(ap=dest_i[:, col:col + 1], axis=0),
                                                                         in_=xr[:, hf * HD:(hf + 1) * HD], in_offset=None), reads=[xk, 'dest_i'], writes=[f'xsl{hf}/{col}'])
                k.barrier()
            with ExitStack() as es:
                sb = self.sbf(es)
                xb = sb('xb', [128, D])
                XT = [sb(f'XT{i}', [128, 32, 128], F32R) for i in range(2)]
                wg = [sb(f'wg{i}', [128, 2 * DE], F32R) for i in range(4)]
                bg = sb('bg', [128, 2 * DE])
                gu = sb('gu', [128, 2 * DE]); gm = sb('gm', [128, DE]); sg = sb('sg', [128, DE]); upc = sb('upc', [128, DE]); aa = sb('aa', [128, DE])
                aT = sb('aT', [128, 5, 128], F32R)
                wd = sb('wd', [128, 5, D], F32R)
                bd = sb('bd', [128, D])
                yb = [sb(f'yb{i}', [128, 512]) for i in range(3)]
                wcnt = 0
                ycnt = 0
                trn = 0

                def gather(out, src, idx_ap, eoff, keys_w, bound):
                    k.dma('pool', lambda e: e.indirect_dma_start(out=out, out_offset=None, in_=src,
                                                                 in_offset=bass.IndirectOffsetOnAxis(ap=idx_ap, axis=0), element_offset=eoff,
                                                                 bounds_check=bound, oob_is_err=False),
                          reads=['widx', 'didx', 'blk_i'], writes=keys_w)
                nts = [(0, 512), (512, 512), (1024, 256)]
                for pr in range(NP):
                    b0 = 2 * pr
                    for s_ in range(2):
                        blk = b0 + s_
                        for hf in range(2):
                            k.dma('sp', _dma(xb[:, hf * HD:(hf + 1) * HD], xsl[hf][blk * 128:(blk + 1) * 128, :]), writes=[f'xb/{hf}'])
                        for g in range(8):
                            trn += 1
                            pt = self.ps[6 + trn % 2]
                            ptk = f'ps{6 + trn % 2}'
                            for jj in range(4):
                                c = 4 * g + jj
                                k.op('pe', lambda e: e.transpose(out=pt[:, jj * 128:(jj + 1) * 128], in_=xb[:, c * 128:(c + 1) * 128], identity=self.ident),
                                     reads=['xb', 'ident'], writes=[ptk])
                            pv = pt.rearrange("p (j s) -> p j s", s=128)
                            if g % 2 == 0:
                                k.op('act', lambda e: e.activation(out=XT[s_][:, 4 * g:4 * g + 4, :], in_=pv, func=AF.Copy), reads=[ptk], writes=[f'XT{s_}/{g}'])
                            else:
                                k.op('dve', lambda e: e.tensor_copy(out=XT[s_][:, 4 * g:4 * g + 4, :], in_=pv), reads=[ptk], writes=[f'XT{s_}/{g}'])
                    gather(bg, bgu, blk_i[:, b0:b0 + 1], 0, ['bg'], self.bregs['e'])
                    gather(bd, bdn, blk_i[:, b0:b0 + 1], 0, ['bd'], self.bregs['e'])
                    for c in range(5):
                        gather(wd[:, c, :], wdn, didx[:, b0:b0 + 1], c * 128 * D, [f'wd/{c}'], self.bregs['d'])
                    for c in range(KC):
                        wcnt += 1
                        w_ = wg[wcnt % 4]
                        wk = f'wg{wcnt % 4}'
                        gather(w_, wgu, widx[:, b0:b0 + 1], c * 128 * 2 * DE, [wk], self.bregs['w'])
                        for s_ in range(2):
                            for nt, (n0, nw) in enumerate(nts):
                                pi = 3 * s_ + nt
                                k.op('pe', lambda e: e.matmul(self.ps[pi][:, :nw], lhsT=XT[s_][:, c, :], rhs=w_[:, n0:n0 + nw], start=(c == 0), stop=(c == KC - 1)),
                                     reads=[f'XT{s_}', wk], writes=[f'ps{pi}'])
                    for s_ in range(2):
                        blk = b0 + s_
                        for nt, (n0, nw) in enumerate(nts):
                            pi = 3 * s_ + nt
                            k.op('dve', lambda e: e.tensor_tensor(out=gu[:, n0:n0 + nw], in0=self.ps[pi][:, :nw], in1=bg[:, n0:n0 + nw], op=ALU.add),
                                 reads=[f'ps{pi}', 'bg'], writes=['gu'])
                        k.op('dve', lambda e: e.tensor_scalar(out=gm, in0=gu[:, 0:DE], scalar1=7.0, scalar2=None, op0=ALU.min), reads=['gu'], writes=['gm'])
                        k.op('act', lambda e: e.activation(out=sg, in_=gm, func=AF.Sigmoid, scale=1.702), reads=['gm'], writes=['sg'])
                        k.op('dve', lambda e: e.tensor_scalar(out=upc, in0=gu[:, DE:2 * DE], scalar1=-7.0, scalar2=7.0, op0=ALU.max, op1=ALU.min), reads=['gu'], writes=['upc'])
                        k.op('dve', lambda e: e.scalar_tensor_tensor(out=upc, in0=upc, scalar=1.0, in1=gm, op0=ALU.add, op1=ALU.mult), reads=['upc', 'gm'], writes=['upc'])
                        k.op('pool', lambda e: e.tensor_tensor(out=aa, in0=upc, in1=sg, op=ALU.mult), reads=['upc', 'sg'], writes=['aa'])
                        pa, pb = self.ps[6], self.ps[7]
                        for c in range(5):
                            dst = pa[:, c * 128:(c + 1) * 128] if c < 4 else pb[:, 0:128]
                            k.op('pe', lambda e: e.transpose(out=dst, in_=aa[:, c * 128:(c + 1) * 128], identity=self.ident),
                                 reads=['aa', 'ident'], writes=['ps6' if c < 4 else 'ps7'])
                        k.op('act', lambda e: e.activation(out=aT[:, 0:4, :], in_=pa.rearrange("p (j s) -> p j s", s=128), func=AF.Copy), reads=['ps6'], writes=['aT/0'])
                        k.op('dve', lambda e: e.tensor_copy(out=aT[:, 4, :], in_=pb[:, 0:128]), reads=['ps7'], writes=['aT/1'])
                        for nt in range(8):
                            pyi = 6 + nt % 2
                            py = self.ps[pyi]
                            pyk = f'ps{pyi}'
                            for c in range(5):
                                k.op('pe', lambda e: e.matmul(py, lhsT=aT[:, c, :], rhs=wd[:, c, nt * 512:(nt + 1) * 512], start=(c == 0), stop=(c == 4)),
                                     reads=['aT', 'wd'], writes=[pyk])
                            ycnt += 1
                            y_ = yb[ycnt % 3]
                            yk = f'yb{ycnt % 3}'
                            k.op('dve', lambda e: e.tensor_tensor(out=y_, in0=py, in1=bd[:, nt * 512:(nt + 1) * 512], op=ALU.add), reads=[pyk, 'bd'], writes=[yk])
                            k.dma('sp', _dma(ysl[nt // 4][blk * 128:(blk + 1) * 128, (nt % 4) * 512:(nt % 4 + 1) * 512], y_), reads=[yk], writes=[f'ysl/{blk}_{nt}'])
                k.barrier()
            with ExitStack() as es:
                sb = self.sbf(es)
                yr = [sb(f'yr{i}', [128, D]) for i in range(2)]
                accb = [sb(f'accb{i}', [128, D]) for i in range(2)]
                hb = [sb(f'hb{i}', [128, 32, 128]) for i in range(2)]
                yc = 0
                trn = 0
                t_of = []
                for (t0, tn, isc) in chunks:
                    for tt in range(tn // 128):
                        t_of.append((t0 + tt * 128, 1 if isc else 0))
                for ti in range(NT):
                    tok0, j = t_of[ti]
                    a_ = accb[ti % 2]
                    ak = f'accb{ti % 2}'
                    for k4 in range(4):
                        yc += 1
                        y_ = yr[yc % 2]
                        yk = f'yr{yc % 2}'
                        col = ti * 4 + k4
                        for hf in range(2):
                            k.dma('pool', lambda e: e.indirect_dma_start(out=y_[:, hf * HD:(hf + 1) * HD], out_offset=None, in_=ysl[hf],
                                                                         in_offset=bass.IndirectOffsetOnAxis(ap=dest_i[:, col:col + 1], axis=0)),
                                  reads=['dest_i'], writes=[f'{yk}/{hf}'])
                        if k4 == 0:
                            k.op('dve', lambda e: e.tensor_scalar(out=a_, in0=y_, scalar1=gate_all[:, ti, 0:1], scalar2=None, op0=ALU.mult),
                                 reads=[yk, 'gate_all'], writes=[ak])
                        else:
                            eng = 'dve'
                            k.op(eng, lambda e: e.scalar_tensor_tensor(out=a_, in0=y_, scalar=gate_all[:, ti, k4:k4 + 1], in1=a_, op0=ALU.mult, op1=ALU.add),
                                 reads=[yk, 'gate_all', ak], writes=[ak])
                    h_ = hb[ti % 2]
                    hk = f'hb{ti % 2}'
                    k.dma('sp', _dma(h_, hsrc[:, tok0:tok0 + 128].rearrange("(c p) t -> p c t", p=128)), writes=[hk])
                    for g in range(8):
                        trn += 1
                        pt = self.ps[trn % 2]
                        ptk = f'ps{trn % 2}'
                        for jj in range(4):
                            c = 4 * g + jj
                            k.op('pe', lambda e: e.transpose(out=pt[:, jj * 128:(jj + 1) * 128], in_=a_[:, c * 128:(c + 1) * 128], identity=self.ident),
                                 reads=[ak, 'ident'], writes=[ptk])
                        for jj in range(4):
                            c = 4 * g + jj
                            k.op('dve', lambda e: e.scalar_tensor_tensor(out=h_[:, c, :], in0=pt[:, jj * 128:(jj + 1) * 128], scalar=mod[:, j, 160 + c:161 + c],
                                                                         in1=h_[:, c, :], op0=ALU.mult, op1=ALU.add), reads=[ptk, hk, f'mod{l}'], writes=[hk])
                    k.dma('sp', _dma(hdst[:, tok0:tok0 + 128].rearrange("(c p) t -> p c t", p=128), h_), reads=[hk], writes=[f'hdst/{ti}'])
                k.barrier()

    def st_diff_attn(self, hlT, catT, lam_init):
        nc, k = self.nc, self.k
        cos4 = self.inp('cos4', [128, NLAT]); sin4 = self.inp('sin4', [128, NLAT])
        dlam = self.inp('dlam', [1, 256]); subln = self.inp('subln', [128, 1])
        sc = 64.0 ** -0.5
        with ExitStack() as es:
            sb = self.sbf(es)
            c4 = sb('c4', [128, NLAT]); s4 = sb('s4', [128, NLAT])
            k.dma('sp', _dma(c4, cos4), writes=['c4']); k.dma('sp', _dma(s4, sin4), writes=['s4'])
            dl = sb('dl', [1, 256]); sl = sb('sl', [128, 1]); g2 = sb('g2', [128, 1]); nlam = sb('nlam', [128, 1])
            lt = sb('lt', [1, 128]); ls = sb('ls', [1, 8])
            k.dma('sp', _dma(dl, dlam), writes=['dl']); k.dma('sp', _dma(sl, subln), writes=['sl'])
            k.op('dve', lambda e: e.tensor_scalar(out=g2, in0=sl, scalar1=1.0 - lam_init, scalar2=None, op0=ALU.mult), reads=['sl'], writes=['g2'])
            k.op('dve', lambda e: e.tensor_tensor(out=lt[:, 0:64], in0=dl[:, 0:64], in1=dl[:, 64:128], op=ALU.mult), reads=['dl'], writes=['lt'])
            k.op('dve', lambda e: e.tensor_tensor(out=lt[:, 64:128], in0=dl[:, 128:192], in1=dl[:, 192:256], op=ALU.mult), reads=['dl'], writes=['lt'])
            k.op('dve', lambda e: e.tensor_reduce(out=ls[:, 0:1], in_=lt[:, 0:64], axis=AX.X, op=ALU.add), reads=['lt'], writes=['ls'])
            k.op('dve', lambda e: e.tensor_reduce(out=ls[:, 1:2], in_=lt[:, 64:128], axis=AX.X, op=ALU.add), reads=['lt'], writes=['ls'])
            k.op('act', lambda e: e.activation(out=ls[:, 2:4], in_=ls[:, 0:2], func=AF.Exp), reads=['ls'], writes=['ls'])
            k.op('dve', lambda e: e.tensor_tensor(out=ls[:, 4:5], in0=ls[:, 3:4], in1=ls[:, 2:3], op=ALU.subtract), reads=['ls'], writes=['ls'])
            k.op('dve', lambda e: e.tensor_scalar(out=ls[:, 5:6], in0=ls[:, 4:5], scalar1=-lam_init, scalar2=None, op0=ALU.add), reads=['ls'], writes=['ls'])
            k.op('pe', lambda e: e.matmul(self.ps[7][:, 0:1], lhsT=self.ones[0:1, :], rhs=ls[:, 5:6], start=True, stop=True), reads=['ones', 'ls'], writes=['ps7'])
            k.op('dve', lambda e: e.tensor_copy(out=nlam, in_=self.ps[7][:, 0:1]), reads=['ps7'], writes=['nlam'])
            raw = {n: sb(f'raw_{n}', [128, T]) for n in ('q', 'qs', 'k', 'ks', 'v')}
            t1 = sb('t1', [128, NLAT]); t2 = sb('t2', [128, NLAT])
            qr = [sb(f'qr{i}', [128, NLAT], F32R) for i in range(2)]
            kr = [sb(f'kr{i}', [128, T], F32R) for i in range(2)]
            vm = [sb(f'vm{i}', [128, 18, 128], F32R) for i in range(2)]
            pT = [sb(f'p{i}', [128, 512], F32R) for i in range(3)]
            rd = sb('rd', [128, 512]); o0 = sb('o0', [128, 512]); o1 = sb('o1', [128, 512]); sq = sb('sq', [128, 512], F32R)
            rstd = sb('rstd', [128, 512]); oo = [sb(f'oo{i}', [128, 512]) for i in range(2)]
            pc = 0
            qcn = 0
            for h in range(16):
                b = h % 2
                for n, mt, w in (('q', h, NLAT), ('qs', 16 + h, NLAT), ('k', 32 + h, T), ('ks', 48 + h, NLAT), ('v', 64 + h, T)):
                    k.dma('sp', _dma(raw[n][:, :w], hlT[mt * 128:(mt + 1) * 128, 0:w]), writes=[f'raw_{n}'])
                for (x, xsw, dst, dk) in ((raw['q'], raw['qs'], qr[b], f'qr{b}'), (raw['k'], raw['ks'], kr[b], f'kr{b}')):
                    xk = 'raw_q' if x is raw['q'] else 'raw_k'
                    xsk = 'raw_qs' if x is raw['q'] else 'raw_ks'
                    k.op('dve', lambda e: e.tensor_tensor(out=t1, in0=x[:, :NLAT], in1=c4, op=ALU.mult), reads=[xk, 'c4'], writes=['t1'])
                    k.op('pool', lambda e: e.tensor_tensor(out=t2, in0=xsw[:, :NLAT], in1=s4, op=ALU.mult), reads=[xsk, 's4'], writes=['t2'])
                    k.op('dve', lambda e: e.tensor_tensor(out=dst[:, :NLAT], in0=t1, in1=t2, op=ALU.add), reads=['t1', 't2'], writes=[dk])
                k.op('act', lambda e: e.activation(out=kr[b][:, NLAT:T], in_=raw['k'][:, NLAT:T], func=AF.Copy), reads=['raw_k'], writes=[f'kr{b}'])
                for g in range(5):
                    n = min(4, 18 - 4 * g)
                    pst = self.ps[6]
                    for j in range(n):
                        kt = 4 * g + j
                        k.op('pe', lambda e: e.transpose(out=pst[:, j * 128:(j + 1) * 128], in_=raw['v'][:, kt * 128:(kt + 1) * 128], identity=self.ident),
                             reads=['raw_v', 'ident'], writes=['ps6'])
                    k.op('act', lambda e: e.activation(out=vm[b][:, 4 * g:4 * g + n, :], in_=pst[:, :n * 128].rearrange("p (j d) -> p j d", d=128), func=AF.Copy),
                         reads=['ps6'], writes=[f'vm{b}'])
                for (t0, tn, isc) in TCH:
                    if isc:
                        continue
                    qcn += 1
                    for c in range(2):
                        pn = self.ps[2 + c]; pd = self.ps[4 + c]
                        rs = slice(64 * c, 64 * c + 64)
                        def emit_s(kt, t0=t0, tn=tn, b=b, rs=rs):
                            nonlocal pc
                            pc += 1
                            pss = self.ps[pc % 2]; psk = f'ps{pc % 2}'
                            ks = slice(kt * 128, (kt + 1) * 128)
                            k.op('pe', lambda e: e.matmul(pss[:, :tn], lhsT=kr[b][rs, ks], rhs=qr[b][rs, t0:t0 + tn], start=True, stop=True),
                                 reads=[f'kr{b}', f'qr{b}'], writes=[psk])
                            return pss, psk, pc
                        cur = emit_s(0)
                        for kt in range(18):
                            nxt_ = emit_s(kt + 1) if kt + 1 < 18 else None
                            pss, psk, pci = cur
                            p = pT[pci % 3]; ppk = f'p{pci % 3}'
                            k.op('act', lambda e: e.activation(out=p[:, :tn], in_=pss[:, :tn], func=AF.Exp, scale=sc), reads=[psk], writes=[ppk])
                            k.op('pe', lambda e: e.matmul(pn[:, :tn], lhsT=vm[b][:, kt, :], rhs=p[:, :tn], start=(kt == 0), stop=(kt == 17)),
                                 reads=[f'vm{b}', ppk], writes=[f'ps{2 + c}'])
                            k.op('pe', lambda e: e.matmul(pd[:, :tn], lhsT=self.onesR, rhs=p[:, :tn], start=(kt == 0), stop=(kt == 17)),
                                 reads=['onesR', ppk], writes=[f'ps{4 + c}'])
                            cur = nxt_
                        oc = o0 if c == 0 else o1
                        k.op('dve', lambda e: e.reciprocal(out=rd[:, :tn], in_=pd[:, :tn]), reads=[f'ps{4 + c}'], writes=['rd'])
                        k.op('dve', lambda e: e.tensor_tensor(out=oc[:, :tn], in0=pn[:, :tn], in1=rd[:, :tn], op=ALU.mult), reads=[f'ps{2 + c}', 'rd'], writes=[f'o{c}'])
                    k.op('dve', lambda e: e.scalar_tensor_tensor(out=o0[:, :tn], in0=o1[:, :tn], scalar=nlam[:, 0:1], in1=o0[:, :tn], op0=ALU.mult, op1=ALU.add),
                         reads=['o0', 'o1', 'nlam'], writes=['o0'])
                    k.op('act', lambda e: e.activation(out=sq[:, :tn], in_=o0[:, :tn], func=AF.Square), reads=['o0'], writes=['sq'])
                    k.op('pe', lambda e: e.matmul(self.ps[7][:, :tn], lhsT=self.onesR, rhs=sq[:, :tn], start=True, stop=True), reads=['onesR', 'sq'], writes=['ps7'])
                    self.rstd_from_ps(self.ps[7], rstd, tn, 128, 'ps7', 'rstd')
                    ob = oo[qcn % 2]
                    k.op('dve', lambda e: e.scalar_tensor_tensor(out=ob[:, :tn], in0=o0[:, :tn], scalar=g2[:, 0:1], in1=rstd[:, :tn], op0=ALU.mult, op1=ALU.mult),
                         reads=['o0', 'g2', 'rstd'], writes=[f'oo{qcn % 2}'])
                    k.dma('sp', _dma(catT[h * 128:(h + 1) * 128, t0:t0 + tn], ob[:, :tn]), reads=[f'oo{qcn % 2}'], writes=[f'catT/{h}_{t0}'])
            k.barrier()

    def st_hyena(self, hlT, catT, row0):
        nc, k = self.nc, self.k
        N2 = 2 * NLAT
        hcw = self.inp('hy_convw', [128, 48, 3])
        zf = self.inp('hy_zfeatT', [33, NLAT]); w1 = self.inp('hy_w1', [33, 64]); b1 = self.inp('hy_b1', [64, 1])
        w2 = self.inp('hy_w2', [64, 64]); b2 = self.inp('hy_b2', [64, 1]); w3 = self.inp('hy_w3T', [64, 64, 128])
        tnb = self.inp('hy_tnorm', [128, NLAT]); ndec = self.inp('hy_ndecay', [128, 16]); skip = self.inp('hy_skip', [128, 2, 16])
        Ct = self.inp('dft_C', [16, 128, 16, 128]); St = self.inp('dft_S', [16, 128, 16, 128])
        CTt = self.inp('dft_CT', [8, 128, 16, 256]); nSTt = self.inp('dft_nST', [8, 128, 16, 256])
        hyc = self.scratch('hyc', [6144, NLAT])
        edn = self.scratch('hy_edn', [2, 2, 2048, NLAT])
        hz = self.scratch('hy_z1', [2048, NLAT])
        with ExitStack() as es:
            sb = self.sbf(es)
            cw = sb('cw', [128, 48, 3])
            k.dma('sp', _dma(cw, hcw), writes=['cw'])
            pb = [sb(f'p{i}', [128, NLAT]) for i in range(2)]; yb = [sb(f'y{i}', [128, NLAT]) for i in range(2)]
            for c in range(48):
                i = c % 2
                p, y = pb[i], yb[i]
                pk, yk = f'p{i}', f'y{i}'
                k.dma('sp', _dma(p, hlT[row0 + c * 128:row0 + (c + 1) * 128, 0:NLAT]), writes=[pk])
                k.op('act', lambda e: e.activation(out=y, in_=p, func=AF.Copy, scale=cw[:, c, 1:2]), reads=[pk, 'cw'], writes=[yk])
                k.op('dve', lambda e: e.scalar_tensor_tensor(out=y[:, 1:NLAT], in0=p[:, 0:NLAT - 1], scalar=cw[:, c, 0:1], in1=y[:, 1:NLAT], op0=ALU.mult, op1=ALU.add),
                     reads=[pk, yk, 'cw'], writes=[yk])
                k.op('dve', lambda e: e.scalar_tensor_tensor(out=y[:, 0:NLAT - 1], in0=p[:, 1:NLAT], scalar=cw[:, c, 2:3], in1=y[:, 0:NLAT - 1], op0=ALU.mult, op1=ALU.add),
                     reads=[pk, yk, 'cw'], writes=[yk])
                k.dma('sp', _dma(hyc[c * 128:(c + 1) * 128, :], y), reads=[yk], writes=[f'hyc/{c}'])
            k.barrier()
        with ExitStack() as es:
            sb = self.sbf(es)
            zfr = sb('zfr', [33, NLAT], F32R); w1r = sb('w1r', [33, 64], F32R); w2r = sb('w2r', [64, 64], F32R)
            b1s = sb('b1s', [64, 1]); b2s = sb('b2s', [64, 1])
            w3r = [sb(f'w3r{i}', [64, 128], F32R) for i in range(4)]
            tn_sb = sb('tn_sb', [128, NLAT]); nd = sb('nd', [128, 16])
            for dst, src, q, kk in ((zfr, zf, 'pool', 'zfr'), (w1r, w1, 'pool', 'w1r'), (w2r, w2, 'pool', 'w2r'), (b1s, b1, 'sp', 'b1s'), (b2s, b2, 'sp', 'b2s'),
                                    (tn_sb, tnb, 'sp', 'tn_sb'), (nd, ndec, 'sp', 'nd')):
                k.dma(q, _dma(dst, src), writes=[kk])
            hid = [sb(f'hid{i}', [64, NLAT], F32R) for i in range(2)]
            ya = sb('ya', [64, 512]); yq = sb('yq', [64, 512]); yi = sb('yi', [64, 512], I32); ym = sb('ym', [64, 512])
            TWO_PI = 2.0 * math.pi

            def sin_layer(wr, wk, src, srck, bs, bk, dst, dstk):
                for q4 in range(4):
                    ts = slice(q4 * 512, (q4 + 1) * 512)
                    ps = self.ps[q4 % 2]; pk = f'ps{q4 % 2}'
                    k.op('pe', lambda e: e.matmul(ps[0:64, :], lhsT=wr, rhs=src[:, ts], start=True, stop=True), reads=[wk, srck], writes=[pk])
                    k.op('dve', lambda e: e.tensor_scalar(out=ya, in0=ps[0:64, :], scalar1=bs[:, 0:1], scalar2=None, op0=ALU.add), reads=[pk, bk], writes=['ya'])
                    k.op('dve', lambda e: e.tensor_scalar(out=yq, in0=ya, scalar1=1.0 / TWO_PI, scalar2=None, op0=ALU.mult), reads=['ya'], writes=['yq'])
                    k.op('dve', lambda e: e.tensor_copy(out=yi, in_=yq), reads=['yq'], writes=['yi'])
                    k.op('dve', lambda e: e.tensor_copy(out=yq, in_=yi), reads=['yi'], writes=['yq'])
                    k.op('dve', lambda e: e.scalar_tensor_tensor(out=ya, in0=yq, scalar=-TWO_PI, in1=ya, op0=ALU.mult, op1=ALU.add), reads=['yq', 'ya'], writes=['ya'])
                    k.op('dve', lambda e: e.tensor_scalar(out=ym, in0=ya, scalar1=math.pi, scalar2=None, op0=ALU.is_gt), reads=['ya'], writes=['ym'])
                    k.op('dve', lambda e: e.scalar_tensor_tensor(out=ya, in0=ym, scalar=-TWO_PI, in1=ya, op0=ALU.mult, op1=ALU.add), reads=['ym', 'ya'], writes=['ya'])
                    k.op('dve', lambda e: e.tensor_scalar(out=ym, in0=ya, scalar1=-math.pi, scalar2=None, op0=ALU.is_lt), reads=['ya'], writes=['ym'])
                    k.op('dve', lambda e: e.scalar_tensor_tensor(out=ya, in0=ym, scalar=TWO_PI, in1=ya, op0=ALU.mult, op1=ALU.add), reads=['ym', 'ya'], writes=['ya'])
                    k.op('dve', lambda e: e.tensor_scalar(out=ya, in0=ya, scalar1=-3.1415925, scalar2=3.1415925, op0=ALU.max, op1=ALU.min), reads=['ya'], writes=['ya'])
                    k.op('act', lambda e: e.activation(out=dst[:, ts], in_=ya, func=AF.Sin), reads=['ya'], writes=[dstk])
            sin_layer(w1r, 'w1r', zfr, 'zfr', b1s, 'b1s', hid[0], 'hid0')
            sin_layer(w2r, 'w2r', hid[0], 'hid0', b2s, 'b2s', hid[1], 'hid1')
            h2 = hid[1]
            win = sb('win', [128, NLAT]); fw = sb('fw', [128, NLAT]); bw = sb('bw', [128, NLAT]); ab = sb('ab', [128, NLAT])
            ee = [sb(f'ee{i}', [128, NLAT]) for i in range(2)]; dd = [sb(f'dd{i}', [128, NLAT]) for i in range(2)]
            l1 = sb('l1', [128, 4])
            wc = 0
            it = 0
            for cc in range(16):
                k.op('act', lambda e: e.activation(out=win, in_=tn_sb, func=AF.Exp, scale=nd[:, cc:cc + 1]), reads=['tn_sb', 'nd'], writes=['win'])
                k.op('dve', lambda e: e.tensor_scalar(out=win, in0=win, scalar1=0.05, scalar2=None, op0=ALU.add), reads=['win'], writes=['win'])
                for o in range(2):
                    it += 1
                    for di, dst, dk in ((0, fw, 'fw'), (1, bw, 'bw')):
                        mt = o * 32 + di * 16 + cc
                        wc += 1
                        wr = w3r[wc % 4]; wk = f'w3r{wc % 4}'
                        k.dma('pool', _dma(wr, w3[mt]), writes=[wk])
                        for q4 in range(4):
                            ts = slice(q4 * 512, (q4 + 1) * 512)
                            ps = self.ps[2 + q4 % 2]; pk = f'ps{2 + q4 % 2}'
                            k.op('pe', lambda e: e.matmul(ps, lhsT=wr, rhs=h2[:, ts], start=True, stop=True), reads=[wk, 'hid1'], writes=[pk])
                            k.op('dve', lambda e: e.tensor_tensor(out=dst[:, ts], in0=ps, in1=win[:, ts], op=ALU.mult), reads=[pk, 'win'], writes=[dk])
                    e_, d_ = ee[it % 2], dd[it % 2]
                    ek, dk = f'ee{it % 2}', f'dd{it % 2}'
                    k.op('dve', lambda e: e.tensor_tensor(out=e_, in0=fw, in1=bw, op=ALU.add), reads=['fw', 'bw'], writes=[ek])
                    k.op('pool', lambda e: e.tensor_tensor(out=d_, in0=bw, in1=fw, op=ALU.subtract), reads=['fw', 'bw'], writes=[dk])
                    k.op('act', lambda e: e.activation(out=ab[:, 1:NLAT], in_=fw[:, 1:NLAT], func=AF.Abs, accum_out=l1[:, 0:1]), reads=['fw'], writes=['ab', 'l1'])
                    k.op('act', lambda e: e.activation(out=ab[:, 1:NLAT], in_=bw[:, 1:NLAT], func=AF.Abs, accum_out=l1[:, 1:2]), reads=['bw', 'ab'], writes=['ab', 'l1'])
                    k.op('act', lambda e: e.activation(out=l1[:, 2:3], in_=e_[:, 0:1], func=AF.Abs), reads=[ek, 'l1'], writes=['l1'])
                    k.op('dve', lambda e: e.tensor_tensor(out=l1[:, 0:1], in0=l1[:, 0:1], in1=l1[:, 1:2], op=ALU.add), reads=['l1'], writes=['l1'])
                    k.op('dve', lambda e: e.tensor_tensor(out=l1[:, 0:1], in0=l1[:, 0:1], in1=l1[:, 2:3], op=ALU.add), reads=['l1'], writes=['l1'])
                    k.op('dve', lambda e: e.reciprocal(out=l1[:, 3:4], in_=l1[:, 0:1]), reads=['l1'], writes=['l1'])
                    k.op('dve', lambda e: e.tensor_scalar(out=e_, in0=e_, scalar1=l1[:, 3:4], scalar2=None, op0=ALU.mult), reads=[ek, 'l1'], writes=[ek])
                    k.op('dve', lambda e: e.tensor_scalar(out=d_, in0=d_, scalar1=l1[:, 3:4], scalar2=None, op0=ALU.mult), reads=[dk, 'l1'], writes=[dk])
                    k.dma('sp', _dma(edn[o, 0, cc * 128:(cc + 1) * 128, :], e_), reads=[ek], writes=[f'edn/{o}_0_{cc}'])
                    k.dma('sp', _dma(edn[o, 1, cc * 128:(cc + 1) * 128, :], d_), reads=[dk], writes=[f'edn/{o}_1_{cc}'])
            k.barrier()
        for o in range(2):
            zsrc = hyc if o == 0 else hz
            gate0 = 2048 * (o + 1)
            with ExitStack() as es:
                sb = self.sbf(es)
                sk = sb('sk', [128, 2, 16])
                k.dma('sp', _dma(sk, skip), writes=['sk'])
                zf_ = [sb(f'zf{i}', [128, NLAT]) for i in range(2)]
                ld = [sb(f'ld{i}', [128, NLAT]) for i in range(2)]
                ztm = sb('ztm', [128, 16, 256], F32R); etm = sb('etm', [128, 16, 256], F32R); dtm = sb('dtm', [128, 16, 256], F32R)
                Yre = sb('Yre', [128, 16, 256], F32R); Yim = sb('Yim', [128, 16, 256], F32R)
                Cb = [sb(f'Cb{i}', [128, 16, 128], F32R) for i in range(2)]; Sb = [sb(f'Sb{i}', [128, 16, 128], F32R) for i in range(2)]
                CTb = [sb(f'CTb{i}', [128, 16, 256], F32R) for i in range(1)]; STb = [sb(f'STb{i}', [128, 16, 256], F32R) for i in range(1)]
                hre = sb('hre', [128, 256]); him = sb('him', [128, 256]); ta = sb('ta', [128, 256]); tb = sb('tb', [128, 256])
                u1 = [sb(f'u1{i}', [128, 256]) for i in range(2)]; gt = [sb(f'gt{i}', [128, 256]) for i in range(2)]
                trn = 0
                fcn = 0
                tqn = 0
                for cq in range(8):
                    for half in range(2):
                        ch0 = cq * 256 + half * 128
                        k.dma('sp', _dma(zf_[half], zsrc[ch0:ch0 + 128, :]), writes=[f'zf{half}'])
                        for (src_ap, srck, dst_tm, dstk) in ((zf_[half], f'zf{half}', ztm, 'ztm'), (None, 'e', etm, 'etm'), (None, 'd', dtm, 'dtm')):
                            if src_ap is None:
                                li = 0 if srck == 'e' else 1
                                src_ap = ld[li]
                                k.dma('sp', _dma(src_ap, edn[o, li, ch0:ch0 + 128, :]), writes=[f'ld{li}'])
                                srck = f'ld{li}'
                            for g in range(4):
                                trn += 1
                                pt = self.ps[6 + trn % 2]; ptk = f'ps{6 + trn % 2}'
                                for jj in range(4):
                                    tc = 4 * g + jj
                                    k.op('pe', lambda e: e.transpose(out=pt[:, jj * 128:(jj + 1) * 128], in_=src_ap[:, tc * 128:(tc + 1) * 128], identity=self.ident),
                                         reads=[srck, 'ident'], writes=[ptk])
                                k.op('act' if trn % 2 else 'dve',
                                     (lambda e: e.activation(out=dst_tm[:, 4 * g:4 * g + 4, half * 128:(half + 1) * 128], in_=pt.rearrange("p (j s) -> p j s", s=128), func=AF.Copy)) if trn % 2 else
                                     (lambda e: e.tensor_copy(out=dst_tm[:, 4 * g:4 * g + 4, half * 128:(half + 1) * 128], in_=pt.rearrange("p (j s) -> p j s", s=128))),
                                     reads=[ptk], writes=[f'{dstk}/{half}_{g}'])
                    for ft in range(16):
                        fcn += 1
                        C_, S_ = Cb[fcn % 2], Sb[fcn % 2]
                        ck, skk = f'Cb{fcn % 2}', f'Sb{fcn % 2}'
                        k.dma('pool', _dma(C_, Ct[ft]), writes=[ck])
                        k.dma('pool', _dma(S_, St[ft]), writes=[skk])
                        z0, z1 = (0, 1) if ft % 2 == 0 else (4, 5)
                        for (pi, W_, wk_, X_, xk_) in ((z0, C_, ck, ztm, 'ztm'), (z1, S_, skk, ztm, 'ztm'), (2, C_, ck, etm, 'etm'), (3, S_, skk, dtm, 'dtm')):
                            for tc in range(16):
                                k.op('pe', lambda e: e.matmul(self.ps[pi][:, :256], lhsT=W_[:, tc, :], rhs=X_[:, tc, :], start=(tc == 0), stop=(tc == 15)),
                                     reads=[wk_, xk_], writes=[f'ps{pi}'])
                        zre, zs = self.ps[z0][:, :256], self.ps[z1][:, :256]
                        zrk, zsk = f'ps{z0}', f'ps{z1}'
                        k.op('act', lambda e: e.activation(out=hre, in_=self.ps[2][:, :256], func=AF.Copy), reads=['ps2'], writes=['hre'])
                        k.op('act', lambda e: e.activation(out=him, in_=self.ps[3][:, :256], func=AF.Copy), reads=['ps3'], writes=['him'])
                        k.op('dve', lambda e: e.tensor_tensor(out=ta, in0=zre, in1=hre, op=ALU.mult), reads=[zrk, 'hre'], writes=['ta'])
                        k.op('dve', lambda e: e.tensor_tensor(out=tb, in0=zs, in1=him, op=ALU.mult), reads=[zsk, 'him'], writes=['tb'])
                        k.op('pool', lambda e: e.tensor_tensor(out=Yre[:, ft, :], in0=ta, in1=tb, op=ALU.add), reads=['ta', 'tb'], writes=[f'Yre/{ft}'])
                        k.op('dve', lambda e: e.tensor_tensor(out=ta, in0=zre, in1=him, op=ALU.mult), reads=[zrk, 'him', f'Yre/{ft}'], writes=['ta'])
                        k.op('dve', lambda e: e.tensor_tensor(out=tb, in0=zs, in1=hre, op=ALU.mult), reads=[zsk, 'hre', f'Yre/{ft}'], writes=['tb'])
                        k.op('pool', lambda e: e.tensor_tensor(out=Yim[:, ft, :], in0=ta, in1=tb, op=ALU.subtract), reads=['ta', 'tb'], writes=[f'Yim/{ft}'])
                    for tq in range(8):
                        tqn += 1
                        CT_, ST_ = (CTb[0], STb[0]) if tqn % 2 else (etm, dtm)
                        ctk, stk = ('CTb0', 'STb0') if tqn % 2 else ('etm', 'dtm')
                        k.dma('pool', _dma(CT_, CTt[tq]), writes=[ctk])
                        k.dma('pool', _dma(ST_, nSTt[tq]), writes=[stk])
                        tsl = slice(tq * 256, (tq + 1) * 256)
                        for half in range(2):
                            ch0 = cq * 256 + half * 128
                            cchunk = ch0 // 128
                            py = self.ps[4 + half]; pyk = f'ps{4 + half}'
                            cs_ = slice(half * 128, (half + 1) * 128)
                            for fc in range(16):
                                k.op('pe', lambda e: e.matmul(py[:, :256], lhsT=Yre[:, fc, cs_], rhs=CT_[:, fc, :], start=(fc == 0), stop=False),
                                     reads=['Yre', ctk], writes=[pyk])
                            for fc in range(16):
                                k.op('pe', lambda e: e.matmul(py[:, :256], lhsT=Yim[:, fc, cs_], rhs=ST_[:, fc, :], start=False, stop=(fc == 15)),
                                     reads=['Yim', stk], writes=[pyk])
                            u_ = u1[half]; g_ = gt[half]
                            k.dma('sp', _dma(g_, hyc[gate0 + ch0:gate0 + ch0 + 128, tsl]), writes=[f'gt{half}'])
                            k.op('act', lambda e: e.activation(out=u_, in_=zf_[half][:, tsl], func=AF.Copy, scale=sk[:, o, cchunk:cchunk + 1]),
                                 reads=[f'zf{half}', 'sk'], writes=[f'u1{half}'])
                            k.op('dve', lambda e: e.scalar_tensor_tensor(out=u_, in0=py[:, :256], scalar=2.0 / N2, in1=u_, op0=ALU.mult, op1=ALU.add),
                                 reads=[pyk, f'u1{half}'], writes=[f'u1{half}'])
                            k.op('pool', lambda e: e.tensor_tensor(out=u_, in0=u_, in1=g_, op=ALU.mult), reads=[f'u1{half}', f'gt{half}'], writes=[f'u1{half}'])
                            if o == 0:
                                k.dma('sp', _dma(hz[ch0:ch0 + 128, tsl], u_), reads=[f'u1{half}'], writes=[f'hz/{ch0}_{tq}'])
                            else:
                                k.dma('sp', _dma(catT[2048 + ch0:2048 + ch0 + 128, tsl], u_), reads=[f'u1{half}'], writes=[f'catT/y{ch0}_{tq}'])
                k.barrier()

    def st_final(self, hsrc, outT):
        nc, k = self.nc, self.k
        fn = self.inp('final_gain', [128, 32])
        with ExitStack() as es:
            sb = self.sbf(es)
            sb_ = self.mod_bufs(sb)
            fg = sb('fg', [128, 32])
            k.dma('sp', _dma(fg, fn), writes=['fg'])
            xo = sb_['xs'].bitcast(F32)
            for (t0, tn, isc) in TCH:
                if isc:
                    continue
                self.load_modulate(sb_, hsrc, t0, tn, fg, None, 'fin')
                k.dma('sp', _dma(outT[:, t0:t0 + tn].rearrange("(c p) t -> p c t", p=128), xo[:, :, :tn]), reads=['xs'], writes=[f'outT/{t0}'])
            k.barrier()


def lhsT_tiles(W, kc=None):
    K, M = W.shape
    assert K % 128 == 0 and M % 128 == 0
    return np.ascontiguousarray(W.reshape(K // 128, 128, M // 128, 128).transpose(2, 1, 0, 3))


def fm_vec(v):
    return np.ascontiguousarray(v.reshape(-1, 128).T)


def host_consts():
    c = np.zeros((128, 3, 128), np.float32)
    c[:, 0, :] = np.eye(128, dtype=np.float32)
    c[:, 1, :] = 1.0
    c[:, 2, :] = np.triu(np.ones((128, 128), np.float32), 1)
    return c


SW64 = np.concatenate([np.arange(32, 64), np.arange(0, 32)])
IN0_COLS = np.concatenate([np.arange(0, 1600), 1536 + SW64, np.arange(1600, 7744)])


def rope_table(rot_dim, n=NLAT, grid_w=64, theta=10000.0):
    rows = n // grid_w
    row = np.repeat(np.arange(rows), grid_w).astype(np.float32)
    col = np.tile(np.arange(grid_w), rows).astype(np.float32)
    quarter = rot_dim // 4
    inv = (1.0 / (theta ** (np.arange(quarter, dtype=np.float32) / quarter))).astype(np.float32)
    ang = np.concatenate([row[:, None] * inv, col[:, None] * inv], -1).astype(np.float32)
    cs, sn = np.cos(ang).T, np.sin(ang).T
    return np.ascontiguousarray(np.concatenate([cs, cs, -sn, sn], 0).astype(np.float32))


def uq_cols():
    cols = []
    for h in range(16):
        b = h * 192
        cols += [np.arange(b, b + 128), b + 128 + np.arange(64), b + 128 + SW64]
    return np.concatenate(cols)


def moe_inputs(z, l):
    return {
        f'routerW{l}': np.ascontiguousarray(z['router_w'][l].reshape(32, 128, 32).transpose(1, 0, 2)),
        f'routerb{l}': np.ascontiguousarray(z['router_b'][l].reshape(1, 32)),
        f'moe_wgu{l}': z['moe_w_gu'][l].reshape(NE * D, 2 * DE),
        f'moe_bgu{l}': z['moe_b_gu'][l],
        f'moe_wdn{l}': z['moe_w_down'][l].reshape(NE * DE, D),
        f'moe_bdn{l}': z['moe_b_down'][l],
        'blk128': np.ascontiguousarray(np.broadcast_to((np.arange(NB0, dtype=np.float32) * 128.0)[None, :], (128, NB0))),
        'pidx': np.arange(128, dtype=np.float32).reshape(128, 1),
    }


def hyena_consts():
    n = NLAT
    f32 = np.float32
    t = np.linspace(0.0, 1.0, n, dtype=f32)[:, None]
    ang = (f32(2.0 * math.pi / n) * np.arange(n, dtype=f32)[:, None]) * np.linspace(1e-4, 15, 16, dtype=f32)[None, :]
    zfeat = np.concatenate([t, np.cos(ang), -np.sin(ang)], -1).astype(f32)
    decay = np.abs(np.linspace(math.log(1e-2) / 1.5, math.log(1e-2) / 0.3, 2048, dtype=f32))
    tt = np.arange(n, dtype=np.float64)[:, None]
    ff = (np.arange(n, dtype=np.float64) + 0.5)[None, :]
    a = 2.0 * math.pi * tt * ff / (2 * n)
    C = np.cos(a).astype(f32); S = np.sin(a).astype(f32)
    def inv_tiles(M):
        return np.ascontiguousarray(M.T.reshape(16, 128, 8, 256).transpose(2, 1, 0, 3))
    return {
        'hy_zfeatT': np.ascontiguousarray(zfeat.T),
        'hy_tnorm': np.ascontiguousarray(np.broadcast_to(t[:, 0][None, :], (128, n))).astype(f32),
        'hy_ndecay': fm_vec(-decay),
        'dft_C': lhsT_tiles(C), 'dft_S': lhsT_tiles(S), 'dft_CT': inv_tiles(C), 'dft_nST': inv_tiles(-S),
    }


def odd_in_cols():
    q = np.arange(0, 2048); kk = np.arange(2048, 4096); v = np.arange(4096, 6144); hy = np.arange(6144, 12288)
    sw = np.concatenate([np.arange(32, 64), np.arange(0, 32)])
    def swp(base):
        return np.concatenate([base[g * 64:(g + 1) * 64][sw] for g in range(32)])
    return np.concatenate([q, swp(q), kk, swp(kk), v, hy])


def rope4(n=NLAT):
    r = rope_table(64, n)
    return np.ascontiguousarray(np.concatenate([r[0:64], r[0:64]], 0)), np.ascontiguousarray(np.concatenate([r[64:128], r[64:128]], 0))


LAM_INIT1 = 0.8 - 0.6 * math.exp(-0.3 * 1)


def odd_inputs(z):
    c4, s4 = rope4()
    d = {
        'inW1': lhsT_tiles(np.ascontiguousarray(z['odd_in_w'][0][:, odd_in_cols()])),
        'cos4': c4, 'sin4': s4,
        'dlam': np.ascontiguousarray(z['diff_lambda'][0].reshape(1, 256)),
        'subln': np.ascontiguousarray(z['diff_subln'][0].reshape(128, 1)),
        'hy_convw': np.ascontiguousarray(z['hy_conv_w'][0].reshape(3, 48, 128).transpose(2, 1, 0)),
        'hy_w1': z['hy_w1'][0], 'hy_b1': np.ascontiguousarray(z['hy_b1'][0].reshape(64, 1)),
        'hy_w2': z['hy_w2'][0], 'hy_b2': np.ascontiguousarray(z['hy_b2'][0].reshape(64, 1)),
        'hy_w3T': np.ascontiguousarray(z['hy_w3'][0].reshape(64, 64, 128).transpose(1, 0, 2)),
        'hy_skip': np.ascontiguousarray(z['hy_skip'][0].reshape(2, 16, 128).transpose(2, 0, 1)),
        'outW1': lhsT_tiles(z['odd_out_w'][0]),
    }
    d.update(hyena_consts())
    return d


def odd_mts(isc):
    return (list(range(32, 48)) + list(range(64, 80))) if isc else list(range(128))


def build_program(dbg=False):
    P = Prog(dbg=dbg)
    P.consts()
    hT0 = P.inp('hT0', [D, T])
    P.st_adaln(0)
    hlT0 = P.st_inproj(0, hT0, 61, lambda isc: list(range(61)))
    qnT, qpT, knT, vT, kpT = P.st_mla_prep(hlT0)
    catT = P.scratch('catT', [D, T])
    P.st_mla_attn(qnT, qpT, knT, vT, kpT, catT)
    P.st_sconv(hlT0, catT)
    hA = P.scratch('hA', [D, T])
    P.st_outproj(0, catT, hT0, hA)
    hB = P.scratch('hB', [D, T])
    P.st_moe(0, hA, hB, True)
    P.st_adaln(1)
    hlT1 = P.st_inproj(1, hB, 128, odd_mts)
    P.st_diff_attn(hlT1, catT, LAM_INIT1)
    P.st_hyena(hlT1, catT, 80 * 128)
    hC = P.scratch('hC', [D, T])
    P.st_outproj(1, catT, hB, hC, with_ctx=False)
    hD = P.scratch('hD', [D, T])
    P.st_moe(1, hC, hD, False)
    outT = P.scratch('outT', [D, NLAT], out=True)
    P.st_final(hD, outT)
    P.k.barrier()
    return P


def host_inputs(z, nb):
    shared = {'consts': host_consts()}
    for l in range(2):
        shared[f'adaW{l}'] = lhsT_tiles(z['ada_w'][l])
        shared[f'adab{l}'] = fm_vec(z['ada_b'][l])
        shared.update(moe_inputs(z, l))
    shared['inW0'] = lhsT_tiles(np.ascontiguousarray(z['mla_in_w'][0][:, IN0_COLS]))
    shared['uqW'] = lhsT_tiles(np.ascontiguousarray(z['mla_w_uq'][0][:, uq_cols()]))
    shared['ukvW'] = lhsT_tiles(z['mla_w_ukv'][0])
    shared['qgain'] = fm_vec(z['mla_q_norm'][0])
    shared['kvgain'] = fm_vec(z['mla_kv_norm'][0])
    shared['rope_mla'] = rope_table(64)
    shared['sconv_w'] = np.ascontiguousarray(z['sc_conv_w'][0].reshape(3, 16, 128).transpose(2, 1, 0))
    shared['outW0'] = lhsT_tiles(z['even_out_w'][0])
    shared.update(odd_inputs(z))
    shared['final_gain'] = fm_vec(z['final_norm'])
    maps = []
    for b in range(nb):
        m = dict(shared)
        m['hT0'] = np.ascontiguousarray(np.concatenate([z['x'][b].T, z['ctx'][b].T], axis=1))
        m['cT'] = np.ascontiguousarray(np.stack([fm_vec(z['c'][b]), fm_vec(z['c_ctx'])], axis=-1))
        maps.append(m)
    return maps


def kernel(**inputs):
    z = {k: np.asarray(v, dtype=np.float32) for k, v in inputs.items()}
    nb = z['x'].shape[0]
    P = build_program()
    maps = host_inputs(z, nb)
    maps = [{n: m[n] for n in P.I} for m in maps]
    res = run_bass_kernel_spmd(P.nc, maps, core_ids=list(range(nb)))
    out = np.stack([np.ascontiguousarray(r['outT'].T) for r in res.results], axis=0)
    return out.astype(np.float32)
```
